# Optimizing a Trainium2 kernel written in Bass

```python
import jax
import jax.numpy as jnp
from jax import lax
import numpy as np

D_MODEL = 2048
BATCH = 16
SEQ = 2048
DEPTH = 4

D_MIX = D_MODEL
HEAD_DIM = 128
D_MLSTM = D_MIX // 2
D_FOX = D_MIX // 4
D_CONV = D_MIX - D_MLSTM - D_FOX
MLSTM_HEADS = D_MLSTM // HEAD_DIM
FOX_HEADS = D_FOX // HEAD_DIM
MLSTM_CONV_WIDTH = 4
CONV_WIDTH = 31
D_FF = 4 * D_MODEL
MLSTM_CHUNK = 128
FOX_Q_BLOCK = 128
EPS = 1e-6
IN_SPLITS = (D_MLSTM, D_MLSTM, D_MLSTM, D_MLSTM, MLSTM_HEADS, MLSTM_HEADS,
             D_FOX, D_FOX, D_FOX, FOX_HEADS, D_CONV, D_CONV)
D_IN = 4 * D_MLSTM + 2 * MLSTM_HEADS + 3 * D_FOX + FOX_HEADS + 2 * D_CONV
MLSTM_F_OFF = 4 * D_MLSTM + MLSTM_HEADS
FOX_F_OFF = 4 * D_MLSTM + 2 * MLSTM_HEADS + 3 * D_FOX

kernel_name = "hybrid_mlstm_fox_conformer_trunk"


def rmsnorm(x, g):
    xf = x.astype(jnp.float32)
    y = xf * lax.rsqrt(jnp.mean(xf * xf, axis=-1, keepdims=True) + EPS)
    return (y * g.astype(jnp.float32)).astype(x.dtype)


def layernorm(x, g, b):
    xf = x.astype(jnp.float32)
    mu = jnp.mean(xf, axis=-1, keepdims=True)
    xc = xf - mu
    y = xc * lax.rsqrt(jnp.mean(xc * xc, axis=-1, keepdims=True) + EPS)
    return (y * g.astype(jnp.float32) + b.astype(jnp.float32)).astype(x.dtype)


def causal_depthwise_conv(x, w, b):
    width = w.shape[0]
    y = lax.conv_general_dilated(
        x, w[:, None, :].astype(x.dtype), window_strides=(1,),
        padding=[(width - 1, 0)], dimension_numbers=('NWC', 'WIO', 'NWC'),
        feature_group_count=x.shape[-1])
    return y + b.astype(x.dtype)


def mlstm_chunkwise(q, k, v, i_pre, f_pre):
    B, S, H, Dh = q.shape
    L = MLSTM_CHUNK
    NC = S // L

    def chunks(a):
        a = a.reshape((B, NC, L, H) + a.shape[3:])
        return jnp.moveaxis(a, (1, 3), (0, 2))

    qc = chunks(q)
    kc = chunks(k * (Dh ** -0.5))
    vc = chunks(v)
    logf = chunks(jax.nn.log_sigmoid(f_pre.astype(jnp.float32)))
    ig = chunks(i_pre.astype(jnp.float32))
    bcum = jnp.cumsum(logf, axis=-1)
    mask = jnp.tril(jnp.ones((L, L), dtype=bool))

    def step(carry, inp):
        C, n, m = carry
        qb, kb, vb, bb, ib = inp
        d_log = bb[..., :, None] - bb[..., None, :] + ib[..., None, :]
        d_log = jnp.where(mask, d_log, -jnp.inf)
        inter = bb + m[..., None]
        m_t = jnp.maximum(inter, jnp.max(d_log, axis=-1))
        w_intra = jnp.exp(d_log - m_t[..., None])
        w_inter = jnp.exp(inter - m_t)
        s = jnp.einsum('bhld,bhsd->bhls', qb, kb) * w_intra
        num = (w_inter[..., None] * jnp.einsum('bhld,bhde->bhle', qb, C)
               + jnp.einsum('bhls,bhse->bhle', s, vb))
        den = w_inter * jnp.einsum('bhld,bhd->bhl', qb, n) + jnp.sum(s, axis=-1)
        h = num / jnp.maximum(jnp.abs(den), jnp.exp(-m_t))[..., None]
        b_last = bb[..., -1]
        g = b_last[..., None] - bb + ib
        m_new = jnp.maximum(b_last + m, jnp.max(g, axis=-1))
        w_k = jnp.exp(g - m_new[..., None])
        decay = jnp.exp(b_last + m - m_new)
        C_new = decay[..., None, None] * C + jnp.einsum('bhs,bhsd,bhse->bhde', w_k, kb, vb)
        n_new = decay[..., None] * n + jnp.einsum('bhs,bhsd->bhd', w_k, kb)
        return (C_new, n_new, m_new), h

    init = (jnp.zeros((B, H, Dh, Dh), jnp.float32),
            jnp.zeros((B, H, Dh), jnp.float32),
            jnp.zeros((B, H), jnp.float32))
    _, h = lax.scan(step, init, (qc, kc, vc, bcum, ig))
    h = jnp.moveaxis(h, (0, 2), (1, 3)).reshape(B, S, H, Dh)
    return h


def forgetting_attention(q, k, v, f_pre):
    B, S, H, Dh = q.shape
    q, k, v = (jnp.swapaxes(a, 1, 2) for a in (q, k, v))
    F = jnp.swapaxes(jnp.cumsum(jax.nn.log_sigmoid(f_pre.astype(jnp.float32)), axis=1), 1, 2)
    scale = Dh ** -0.5
    outs = []
    for blk in range(S // FOX_Q_BLOCK):
        lo = blk * FOX_Q_BLOCK
        hi = lo + FOX_Q_BLOCK
        qb = q[:, :, lo:hi]
        kb = k[:, :, :hi]
        vb = v[:, :, :hi]
        logits = (jnp.einsum('bhqd,bhkd->bhqk', qb, kb).astype(jnp.float32) * scale
                  + F[:, :, lo:hi, None] - F[:, :, None, :hi])
        causal = (lo + jnp.arange(FOX_Q_BLOCK))[:, None] >= jnp.arange(hi)[None, :]
        p = jax.nn.softmax(jnp.where(causal, logits, -jnp.inf), axis=-1)
        outs.append(jnp.einsum('bhqk,bhkd->bhqd', p.astype(vb.dtype), vb))
    o = jnp.concatenate(outs, axis=2)
    return jnp.swapaxes(o, 1, 2).reshape(B, S, H * Dh)


def conformer_conv(u, g, w_dw, b_dw, ln_g, ln_b):
    y = u * jax.nn.sigmoid(g)
    y = causal_depthwise_conv(y, w_dw, b_dw)
    y = layernorm(y, ln_g, ln_b)
    return jax.nn.silu(y)


def hybrid_layer(x, norm_mix, w_in, b_in, mlstm_conv_w, mlstm_conv_b, mlstm_head_norm,
                 fox_head_norm, conv_dw_w, conv_dw_b, conv_ln_g, conv_ln_b, w_out,
                 norm_ffn, w_up, w_down):
    B, S, _ = x.shape
    h = rmsnorm(x, norm_mix)
    proj = h @ w_in + b_in
    split_points = np.cumsum(IN_SPLITS)[:-1].tolist()
    (q_m, k_m, v_m, o_m, i_m, f_m, q_a, k_a, v_a, f_a, u_c, g_c) = jnp.split(proj, split_points, axis=-1)

    qk = jax.nn.silu(causal_depthwise_conv(jnp.concatenate([q_m, k_m], axis=-1), mlstm_conv_w, mlstm_conv_b))
    q_m, k_m = jnp.split(qk, 2, axis=-1)
    heads_m = lambda a: a.reshape(B, S, MLSTM_HEADS, HEAD_DIM)
    hm = mlstm_chunkwise(heads_m(q_m), heads_m(k_m), heads_m(v_m), i_m, f_m).astype(x.dtype)
    y_m = rmsnorm(hm, mlstm_head_norm).reshape(B, S, D_MLSTM) * jax.nn.sigmoid(o_m)

    heads_a = lambda a: a.reshape(B, S, FOX_HEADS, HEAD_DIM)
    ha = forgetting_attention(heads_a(q_a), heads_a(k_a), heads_a(v_a), f_a)
    y_a = rmsnorm(heads_a(ha), fox_head_norm).reshape(B, S, D_FOX)

    y_c = conformer_conv(u_c, g_c, conv_dw_w, conv_dw_b, conv_ln_g, conv_ln_b)

    x = x + jnp.concatenate([y_m, y_a, y_c], axis=-1) @ w_out
    h2 = rmsnorm(x, norm_ffn)
    x = x + jnp.square(jax.nn.relu(h2 @ w_up)) @ w_down
    return x


def setup_inputs(seed: int = 0) -> dict:
    key = jax.random.key(seed)
    ks = jax.random.split(key, 17)
    f32 = jnp.float32

    def nrm(k, shape, scale):
        return jax.random.normal(k, shape, f32) * scale

    def gain(k, shape):
        return 1.0 + nrm(k, shape, 0.02)

    x = nrm(ks[0], (BATCH, SEQ, D_MODEL), 1.0)
    norm_mix = gain(ks[1], (DEPTH, D_MODEL))
    w_in = nrm(ks[2], (DEPTH, D_MODEL, D_IN), D_MODEL ** -0.5)
    b_in = nrm(ks[3], (DEPTH, D_IN), 0.02)
    b_in = b_in.at[:, MLSTM_F_OFF:MLSTM_F_OFF + MLSTM_HEADS].add(jnp.linspace(3.0, 6.0, MLSTM_HEADS))
    b_in = b_in.at[:, FOX_F_OFF:FOX_F_OFF + FOX_HEADS].add(jnp.linspace(1.0, 4.0, FOX_HEADS))
    mlstm_conv_w = nrm(ks[4], (DEPTH, MLSTM_CONV_WIDTH, 2 * D_MLSTM), MLSTM_CONV_WIDTH ** -0.5)
    mlstm_conv_b = nrm(ks[5], (DEPTH, 2 * D_MLSTM), 0.02)
    mlstm_head_norm = gain(ks[6], (DEPTH, MLSTM_HEADS, HEAD_DIM))
    fox_head_norm = gain(ks[7], (DEPTH, FOX_HEADS, HEAD_DIM))
    conv_dw_w = nrm(ks[8], (DEPTH, CONV_WIDTH, D_CONV), CONV_WIDTH ** -0.5)
    conv_dw_b = nrm(ks[9], (DEPTH, D_CONV), 0.02)
    conv_ln_g = gain(ks[10], (DEPTH, D_CONV))
    conv_ln_b = nrm(ks[11], (DEPTH, D_CONV), 0.02)
    w_out = nrm(ks[12], (DEPTH, D_MIX, D_MODEL), D_MIX ** -0.5)
    norm_ffn = gain(ks[13], (DEPTH, D_MODEL))
    w_up = nrm(ks[14], (DEPTH, D_MODEL, D_FF), D_MODEL ** -0.5)
    w_down = nrm(ks[15], (DEPTH, D_FF, D_MODEL), D_FF ** -0.5)
    final_norm = gain(ks[16], (D_MODEL,))
    return {"x": x, "norm_mix": norm_mix, "w_in": w_in, "b_in": b_in,
            "mlstm_conv_w": mlstm_conv_w, "mlstm_conv_b": mlstm_conv_b,
            "mlstm_head_norm": mlstm_head_norm, "fox_head_norm": fox_head_norm,
            "conv_dw_w": conv_dw_w, "conv_dw_b": conv_dw_b,
            "conv_ln_g": conv_ln_g, "conv_ln_b": conv_ln_b, "w_out": w_out,
            "norm_ffn": norm_ffn, "w_up": w_up, "w_down": w_down,
            "final_norm": final_norm}


def reference(x, norm_mix, w_in, b_in, mlstm_conv_w, mlstm_conv_b, mlstm_head_norm,
              fox_head_norm, conv_dw_w, conv_dw_b, conv_ln_g, conv_ln_b, w_out,
              norm_ffn, w_up, w_down, final_norm):
    for l in range(DEPTH):
        x = hybrid_layer(x, norm_mix[l], w_in[l], b_in[l], mlstm_conv_w[l], mlstm_conv_b[l],
                         mlstm_head_norm[l], fox_head_norm[l], conv_dw_w[l], conv_dw_b[l],
                         conv_ln_g[l], conv_ln_b[l], w_out[l], norm_ffn[l], w_up[l], w_down[l])
    return rmsnorm(x, final_norm)
```

```python
import math
from contextlib import ExitStack
import numpy as np
import concourse.bass as bass
import concourse.mybir as mybir
from concourse.bass_utils import run_bass_kernel_spmd

F32 = mybir.dt.float32
BF16 = mybir.dt.bfloat16
AF = mybir.ActivationFunctionType
ALU = mybir.AluOpType
AX = mybir.AxisListType
EPS = 1e-6
ENGS = ("sync", "scalar", "vector", "gpsimd", "tensor")
NSLOT = 16


class Res:
    __slots__ = ("name", "w", "r", "excl")

    def __init__(self, name, excl=False):
        self.name = name
        self.w = None
        self.r = {}
        self.excl = excl


class Op:
    __slots__ = ("eng", "fn", "deps", "sig", "rank", "is_dma", "slot", "val", "ndma", "calls")


class _Tok:
    def __init__(self, rec, idx):
        self.rec, self.idx = rec, idx

    def then_inc(self, sem, n):
        self.rec.calls[self.idx][3] = n
        return self


class _RecEng:
    def __init__(self):
        self.calls = []

    def __getattr__(self, name):
        def f(*a, **k):
            self.calls.append([name, a, k, None])
            return _Tok(self, len(self.calls) - 1)
        return f


class Prog:
    def __init__(self, nc):
        self.nc = nc
        self.ops = []
        self.q = {e: [] for e in ENGS}
        self.dma_cnt = {"sync": 0, "gpsimd": 0}
        self.slot_total = {}
        self.slot_last = {}
        self.out_ops = []
        self.cur_chain = None

    def _key(self, oid):
        op = self.ops[oid]
        return ("dma", oid) if op.is_dma else op.eng

    def _rec(self, eng, fn, reads, writes, is_dma=False, ndma=1, is_output=False, after=()):
        rec = _RecEng()
        if fn is not None:
            if is_dma:
                fn(rec, None)
            else:
                fn(rec)
        ent = (eng, rec.calls, list(reads), list(writes), is_dma, ndma, is_output, tuple(after))
        if self.cur_chain is not None:
            self.cur_chain.append(ent)
            return None
        return self.commit(ent)

    def commit(self, ent):
        eng, calls, reads, writes, is_dma, ndma, is_output, after = ent
        if any(r.excl for r in reads):
            writes = list(writes) + [r for r in reads if r.excl]
            reads = [r for r in reads if not r.excl]
        deps = set(after)
        for r in reads:
            if r.w is not None:
                deps.add(r.w)
        for r in writes:
            if r.w is not None:
                deps.add(r.w)
            deps.update(r.r.values())
        op = Op()
        op.eng, op.fn, op.sig, op.rank, op.is_dma, op.ndma = eng, True, False, 0, is_dma, ndma
        op.slot = op.val = None
        oid = len(self.ops)
        if is_dma:
            j = self.dma_cnt[eng]
            self.dma_cnt[eng] = j + 1
            slot = j % NSLOT
            op.slot = slot
            tot = self.slot_total.get((eng, slot), 0) + 16 * ndma
            self.slot_total[(eng, slot)] = tot
            op.val = tot
            prev = self.slot_last.get((eng, slot))
            if prev is not None:
                deps.add(prev)
            self.slot_last[(eng, slot)] = oid
        if eng == "tensor":
            deps = {d for d in deps if not (self.ops[d].eng == "tensor" and not self.ops[d].is_dma)}
        op.deps = deps
        op.calls = calls
        self.ops.append(op)
        self.q[eng].append(oid)
        key = ("dma", oid) if is_dma else eng
        for r in reads:
            r.r[key] = oid
        for r in writes:
            r.w = oid
            r.r = {}
        if is_output:
            self.out_ops.append(oid)
        return oid

    def op(self, eng, fn, reads=(), writes=()):
        return self._rec(eng, fn, reads, writes)

    def dma(self, queue, fn, reads=(), writes=(), ndma=1, is_output=False, after=()):
        return self._rec(queue, fn, reads, writes, is_dma=True, ndma=ndma, is_output=is_output, after=after)

    def run_chains(self, chains, weights):
        lists = []
        for fn in chains:
            self.cur_chain = []
            fn()
            lists.append(self.cur_chain)
            self.cur_chain = None
        idx = [0] * len(lists)
        while any(idx[i] < len(lists[i]) for i in range(len(lists))):
            for i, L in enumerate(lists):
                for _ in range(weights[i]):
                    if idx[i] < len(L):
                        self.commit(L[idx[i]])
                        idx[i] += 1

    def fence(self, frm, to):
        evs = {}
        for fr in frm:
            ids = list(fr.r.values())
            if fr.w is not None:
                ids.append(fr.w)
            for oid in ids:
                k = self._key(oid)
                if evs.get(k, -1) < oid:
                    evs[k] = oid
        for t in to:
            for k, v in evs.items():
                if t.r.get(k, -1) < v:
                    t.r[k] = v

    def emit(self):
        nc = self.nc
        ops = self.ops
        fin = Op()
        fin.eng, fin.fn, fin.sig, fin.rank, fin.is_dma, fin.ndma = "sync", None, False, 0, False, 0
        fin.slot = fin.val = None
        fin.deps = set(self.out_ops)
        fin.calls = []
        ops.append(fin)
        self.q["sync"].append(len(ops) - 1)
        for op in ops:
            for d in op.deps:
                if not ops[d].is_dma:
                    ops[d].sig = True
        for e in ENGS:
            r = 0
            for oid in self.q[e]:
                op = ops[oid]
                if (not op.is_dma) and op.sig:
                    r += 1
                    op.rank = r
        self.nwaits = 0
        with ExitStack() as st:
            esem = {e: st.enter_context(nc.semaphore("es_" + e)) for e in ENGS}
            dsem = {}
            for qn in ("sync", "gpsimd"):
                for i in range(NSLOT):
                    dsem[(qn, i)] = st.enter_context(nc.semaphore("ds_%s_%d" % (qn, i)))
            block = st.enter_context(nc.Block())

            def run(ename, e):
                known = {}
                for oid in self.q[ename]:
                    op = ops[oid]
                    waits = {}
                    for d in op.deps:
                        x = ops[d]
                        if x.is_dma:
                            key, val = ("d", x.eng, x.slot), x.val
                        else:
                            key, val = ("e", x.eng), x.rank
                        if waits.get(key, 0) < val:
                            waits[key] = val
                    for key, val in waits.items():
                        if known.get(key, 0) >= val:
                            continue
                        sem = dsem[(key[1], key[2])] if key[0] == "d" else esem[key[1]]
                        e.wait_ge(sem, val)
                        self.nwaits += 1
                        known[key] = val
                    if op.fn is None:
                        continue
                    if op.is_dma:
                        for (name, a, k, n) in op.calls:
                            getattr(e, name)(*a, **k).then_inc(dsem[(ename, op.slot)], 16)
                    else:
                        ins = None
                        for (name, a, k, n) in op.calls:
                            ins = getattr(e, name)(*a, **k)
                        if op.sig:
                            ins.then_inc(esem[ename], 1)

            @block.sync
            def _(e):
                run("sync", e)

            @block.scalar
            def _(e):
                run("scalar", e)

            @block.vector
            def _(e):
                run("vector", e)

            @block.gpsimd
            def _(e):
                run("gpsimd", e)

            @block.tensor
            def _(e):
                run("tensor", e)


class Cfg:
    def __init__(self, D=2048, S=2048, NL=4, NSEQ=2, HG=4):
        self.D, self.S, self.NL, self.NSEQ = D, S, NL, NSEQ
        self.KC = D // 128
        self.DM = D // 2
        self.HM = self.DM // 128
        self.DF = D // 4
        self.HA = self.DF // 128
        self.DC = D - self.DM - self.DF
        self.CC = self.DC // 128
        self.DFF = 4 * D
        self.HG = min(HG, self.HM)
        self.NGM = self.HM // self.HG
        self.TT = 512
        self.NT = S // self.TT
        self.NB = 4
        self.NBLK = S // 128
        DM, HM, DF, HA, DC = self.DM, self.HM, self.DF, self.HA, self.DC
        self.DIN = 4 * DM + 2 * HM + 3 * DF + HA + 2 * DC
        self.c_qm, self.c_km, self.c_vm, self.c_om = 0, DM, 2 * DM, 3 * DM
        self.c_im, self.c_fm = 4 * DM, 4 * DM + HM
        self.c_qa = 4 * DM + 2 * HM
        self.c_ka = self.c_qa + DF
        self.c_va = self.c_qa + 2 * DF
        self.c_fa = self.c_qa + 3 * DF
        self.c_uc = self.c_fa + HA
        self.c_gc = self.c_uc + DC
        self.NG = 2 * HM + HA
        self.NF = HM + HA
        self.NBO = D // 256
        blks = []
        for i in range(DM // 256):
            blks.append(("vm", i, self.c_vm + i * 256))
        for i in range(DF // 256):
            blks.append(("va", i, self.c_va + i * 256))
        for i in range(DF // 256):
            blks.append(("qa", i, self.c_qa + i * 256))
        for i in range(DF // 256):
            blks.append(("ka", i, self.c_ka + i * 256))
        for i in range(DC // 256):
            blks.append(("gc", i, self.c_gc + i * 256))
            blks.append(("uc", i, self.c_uc + i * 256))
        self.n_pre_blocks = len(blks)
        hb = self.HG // 2
        for g in range(self.NGM):
            for kind, c0 in (("qm", self.c_qm), ("km", self.c_km), ("om", self.c_om)):
                for i in range(g * hb, (g + 1) * hb):
                    blks.append((kind, i, c0 + i * 256))
        self.in_blocks = blks
        self.NBI = len(blks)
        KC, CC = self.KC, self.CC
        o = 0
        self.o_gmix = o; o += KC
        self.o_gffn = o; o += KC
        self.fm_kinds = [("qm", HM, self.c_qm), ("km", HM, self.c_km), ("om", HM, self.c_om), ("qa", HA, self.c_qa),
                         ("ka", HA, self.c_ka), ("gc", CC, self.c_gc), ("uc", CC, self.c_uc)]
        self.o_bfm = {}
        for kind, n, c0 in self.fm_kinds:
            self.o_bfm[kind] = o
            o += n
        self.o_mcw = o; o += 4 * 2 * HM
        self.o_mcb = o; o += 2 * HM
        self.o_cdw = o; o += 31 * CC
        self.o_cdb = o; o += CC
        self.o_clg = o; o += CC
        self.o_clb = o; o += CC
        self.o_ghm = o; o += HM
        self.o_gha = o; o += HA
        self.LPP = o
        self.o_gfin = self.NL * self.LPP
        self.NPP = self.o_gfin + KC
        self.NFB = self.NG + DM + DF


def pack_params(cfg, inp):
    c = cfg
    pp = np.zeros((128, c.NPP), np.float32)

    def fm(vec):
        return np.ascontiguousarray(vec.reshape(-1, 128).T)

    for l in range(c.NL):
        b = l * c.LPP
        pp[:, b + c.o_gmix: b + c.o_gmix + c.KC] = fm(inp["norm_mix"][l])
        pp[:, b + c.o_gffn: b + c.o_gffn + c.KC] = fm(inp["norm_ffn"][l])
        for kind, n, c0 in c.fm_kinds:
            pp[:, b + c.o_bfm[kind]: b + c.o_bfm[kind] + n] = fm(inp["b_in"][l, c0:c0 + n * 128])
        for tap in range(4):
            pp[:, b + c.o_mcw + tap * 2 * c.HM: b + c.o_mcw + (tap + 1) * 2 * c.HM] = fm(inp["mlstm_conv_w"][l, tap])
        pp[:, b + c.o_mcb: b + c.o_mcb + 2 * c.HM] = fm(inp["mlstm_conv_b"][l])
        for tap in range(31):
            pp[:, b + c.o_cdw + tap * c.CC: b + c.o_cdw + (tap + 1) * c.CC] = fm(inp["conv_dw_w"][l, tap])
        pp[:, b + c.o_cdb: b + c.o_cdb + c.CC] = fm(inp["conv_dw_b"][l])
        pp[:, b + c.o_clg: b + c.o_clg + c.CC] = fm(inp["conv_ln_g"][l])
        pp[:, b + c.o_clb: b + c.o_clb + c.CC] = fm(inp["conv_ln_b"][l])
        pp[:, b + c.o_ghm: b + c.o_ghm + c.HM] = fm(inp["mlstm_head_norm"][l].reshape(-1))
        pp[:, b + c.o_gha: b + c.o_gha + c.HA] = fm(inp["fox_head_norm"][l].reshape(-1))
    pp[:, c.o_gfin: c.o_gfin + c.KC] = fm(inp["final_norm"])
    fb = np.zeros((c.NL, 128, c.NFB), np.float32)
    for l in range(c.NL):
        row = np.concatenate([inp["b_in"][l, c.c_im:c.c_im + 2 * c.HM], inp["b_in"][l, c.c_fa:c.c_fa + c.HA],
                              inp["b_in"][l, c.c_vm:c.c_vm + c.DM], inp["b_in"][l, c.c_va:c.c_va + c.DF]])
        fb[l] = np.broadcast_to(row[None, :], (128, c.NFB))
    return pp, fb


class _Stop(Exception):
    pass


INTERLEAVE = True
CH_W = (1, 1, 3, 1)


def build(cfg, dbg_names=(), stop_at=None):
    c = cfg
    D, S, NL, NSEQ, KC, DM, HM, DF, HA, DC, CC, DFF = c.D, c.S, c.NL, c.NSEQ, c.KC, c.DM, c.HM, c.DF, c.HA, c.DC, c.CC, c.DFF
    HG, NGM, NT, NB, NG, NF, NBO, NBI = c.HG, c.NGM, c.NT, c.NB, c.NG, c.NF, c.NBO, c.NBI
    TT = c.TT
    nc = bass.Bass("TRN2", target_bir_lowering=False)
    x_d = nc.dram_tensor("x", [NSEQ, S, D], F32, kind="ExternalInput").ap()
    w_in_d = nc.dram_tensor("w_in", [NL, D, c.DIN], F32, kind="ExternalInput").ap()
    w_out_d = nc.dram_tensor("w_out", [NL, D, D], F32, kind="ExternalInput").ap()
    w_up_d = nc.dram_tensor("w_up", [NL, D, DFF], F32, kind="ExternalInput").ap()
    w_dn_d = nc.dram_tensor("w_down", [NL, DFF, D], F32, kind="ExternalInput").ap()
    pp_d = nc.dram_tensor("pp", [128, c.NPP], F32, kind="ExternalInput").ap()
    fb_d = nc.dram_tensor("fb", [NL, 128, c.NFB], F32, kind="ExternalInput").ap()
    y_d = nc.dram_tensor("y", [NSEQ, S, D], F32, kind="ExternalOutput").ap()
    xs_d = nc.dram_tensor("xs_scr", [NSEQ * NT, 128, KC, TT], F32, kind="Internal").ap()
    wsc_in = [nc.dram_tensor("wsc_in%d" % l, [NBI, 128, KC, 256], BF16, kind="Internal").ap() for l in range(NL)]
    wsc_out = [nc.dram_tensor("wsc_out%d" % l, [NBO, 128, KC, 256], BF16, kind="Internal").ap() for l in range(NL)]
    wsc_up = [nc.dram_tensor("wsc_up%d" % l, [4 * NBO, 128, KC, 256], BF16, kind="Internal").ap() for l in range(NL)]
    wsc_dn = [nc.dram_tensor("wsc_dn%d" % l, [4 * NBO, 128, KC, 256], BF16, kind="Internal").ap() for l in range(NL)]
    dbg_out = {}

    st = ExitStack()
    p = Prog(nc)

    def sb(name, shape, dt):
        return st.enter_context(nc.sbuf_tensor("s_" + name, shape, dt))

    x_t = sb("x_t", [128, KC, TT], F32)
    h_t = sb("h_t", [128, KC, TT], BF16)
    m_t = sb("m_t", [128, KC, TT], BF16)
    NWB = 3
    wb = [sb("wb%d" % i, [128, KC, 256], BF16) for i in range(NWB)]
    wg = sb("wg", [128, NL, KC, NG], BF16)
    stg_io = [sb("stg_io%d" % i, [128, 512], F32) for i in range(2)]
    rs = sb("rs", [128, TT], F32)
    sqb = [sb("sqb%d" % i, [128, TT], BF16) for i in range(2)]
    pp = sb("pp", [128, c.NPP], F32)
    fbl = sb("fbl", [128, c.NFB], F32)
    identb = sb("identb", [128, 128], BF16)
    identf = sb("identf", [128, 128], F32)
    ones_b = sb("ones_b", [128, 128], BF16)
    ones_f = sb("ones_f", [128, 128], F32)
    Utri = sb("Utri", [128, 128], F32)
    sel2 = [sb("sel2_%d" % h, [2 * HA, 128], BF16) for h in range(HA)]
    qT = sb("qT", [128, HG, TT], BF16)
    kT = sb("kT", [128, HG, TT], BF16)
    sgo = sb("sgo", [128, HG, TT], BF16)
    vx = sb("vx", [128, NB, HM, 129], BF16)
    Cf = sb("Cf", [128, HM, 129], F32)
    Cb = sb("Cb", [128, HM, 129], BF16)
    cstg = sb("cstg", [128, TT + 3], F32)
    cacc = sb("cacc", [128, TT], F32)
    chist = sb("chist", [128, 2 * HM, 3], F32)
    Gt = sb("Gt", [128, NB, NG], F32)
    Et = sb("Et", [128, NF], F32)
    Lt = sb("Lt", [128, NB, NF], F32)
    gtmp = sb("gtmp", [128, HM], F32)
    gcs = sb("gcs", [128, 128], F32)
    aa = sb("aa", [128, NB, HM], F32)
    einv = sb("einv", [128, NB, HM], F32)
    ebl = sb("ebl", [128, NB, HM], F32)
    carry = sb("carry", [128, HA], F32)
    nF2 = sb("nF2", [128, 2 * HA], BF16)
    qTa = sb("qTa", [128, HA, TT], BF16)
    KT = sb("KT", [128, HA, S], BF16)
    Vext = sb("Vext", [128, c.NBLK, HA, 129], BF16)
    negF2 = sb("negF2", [2 * HA, S], BF16)
    y0 = sb("y0", [128, CC, TT + 30], BF16)
    sg = sb("sg", [128, 2, TT], F32)
    rl = [sb("rl%d" % i, [128, TT], F32) for i in range(2)]
    dn = sb("dn", [128, HG], F32)
    rr = sb("rr", [128, HG], F32)
    dr = sb("dr", [128, HG], F32)
    ss = sb("ss", [128, HG], F32)

    assert CC <= 4 and HG * 128 <= TT
    ktok = sb("ktok", [128, HG * 128], BF16)
    STm = sb("STm", [128, HG * 128], BF16)
    ytok = sb("ytok", [128, HG * 128], BF16)
    sm = {"ktok": ktok[:], "STm": STm[:], "ytok": ytok[:], "hf": cstg[:, 0:HG * 128], "sqf": cacc[:, 0:HG * 128]}
    PTc = sb("PTc", [128, 1024], BF16)
    cstgb = sb("cstgb", [128, TT + 3], BF16)
    dgM = [sb("dgM%d" % i, [128, 128], BF16) for i in range(2)]
    dgC = [sb("dgC%d" % i, [128, 128], BF16) for i in range(4)]
    yas = [sb("ya%d" % i, [128, 128], BF16) for i in range(2)]
    of1 = sb("of1", [128, 128], F32)
    rr1s = [sb("rr1_%d" % i, [128, 1], F32) for i in range(2)]
    ss1s = [sb("ss1_%d" % i, [128, 1], F32) for i in range(2)]
    nFtok = sb("nFtok", [128, c.NBLK, HA], F32)
    maskT = sb("maskT", [128, 128], BF16)
    ofs = [gcs[:, 0:128], of1[:]]

    ps = [st.enter_context(nc.psum_tensor("ps%d" % i, [128, 512], F32)) for i in range(8)]
    psb = [t[:].bitcast(BF16) for t in ps]

    R_x = [Res("x%d" % k) for k in range(KC)]
    R_h = [Res("h%d" % k) for k in range(KC)]
    R_m = [Res("m%d" % k) for k in range(KC)]
    R_wb = [Res("wb%d" % i) for i in range(NWB)]
    R_ps = [Res("ps%d" % i, excl=True) for i in range(8)]
    R_io = [Res("io0"), Res("io1")]
    R_sqb = [Res("sqb0"), Res("sqb1")]
    R_rl = [Res("rl0"), Res("rl1")]
    R = {k: Res(k) for k in ["wg", "rs", "pp", "fbl", "const", "qT", "kT", "sgo", "vx", "Cf", "Cb", "cstg", "cacc", "chist",
                             "Gt", "Et", "Lt", "gtmp", "gcs", "aa", "einv", "ebl", "nFt", "carry", "nF2", "qTa", "KT", "Vext", "negF2",
                             "y0", "sg", "dn", "rr", "dr", "ss", "mx", "negm", "rr1", "ss1",
                             "ktok", "STm", "hf", "sqf", "ytok", "P", "PT", "of", "sq1", "ya", "cv", "cmean", "cm2", "csq"]}
    R["hf"] = R["cstg"]
    R["sqf"] = R["cacc"]
    R["of"] = R["gcs"]
    for i_ in range(2):
        for nm in ("PTc%d", "rr1_%d", "ss1_%d", "ya_%d"):
            R[nm % i_] = Res(nm % i_)
    R_ofs = [R["gcs"], Res("of1")]
    R["cstgb"] = Res("cstgb")
    R_dgM = [Res("dgM%d" % i) for i in range(2)]
    R_dgC = [Res("dgC%d" % i) for i in range(4)]
    dg_ctr = {"M": 0, "C": 0}
    R_cv = [R_rl[0], R_rl[1], R_io[0], R_io[1]]
    cvt = [rl[0], rl[1], stg_io[0], stg_io[1]]

    R_xs = [[Res("xs%d_%d" % (i, k)) for k in range(KC)] for i in range(NSEQ * NT)]
    R_win = [Res("win%d" % l) for l in range(NL)]
    R_wout = [Res("wout%d" % l) for l in range(NL)]
    R_wup = [Res("wup%d" % l) for l in range(NL)]
    R_wdn = [Res("wdn%d" % l) for l in range(NL)]

    V = lambda fn, r=(), w=(): p.op("vector", fn, r, w)
    A = lambda fn, r=(), w=(): p.op("scalar", fn, r, w)
    T = lambda fn, r=(), w=(): p.op("tensor", fn, r, w)
    G = lambda fn, r=(), w=(): p.op("gpsimd", fn, r, w)

    POOLS = {"all": list(range(8)), "F0": [0, 0, 1], "F1": [2, 2, 3], "M": [4, 5], "C": [6, 7], "G": [0, 1], "P": [2, 3, 4, 5, 6, 7]}
    bank_ctr = {"all": 0, "M": 0, "C": 0, "G": 0, "P": 0}
    cur_pool = ["all"]

    def set_pool(name):
        cur_pool[0] = name

    def nb():
        pn = cur_pool[0]
        pool = POOLS[pn]
        b = pool[bank_ctr[pn] % len(pool)]
        bank_ctr[pn] += 1
        return b

    def dbg(name, ap, shape, res):
        if name not in dbg_names:
            return
        key = name
        i = 0
        while key in dbg_out:
            i += 1
            key = "%s_%d" % (name, i)
        d = nc.dram_tensor("dbg_" + key, shape, ap.dtype, kind="ExternalOutput").ap()
        dbg_out[key] = d
        p.dma("sync", lambda e, s: e.dma_start(out=d, in_=ap).then_inc(s, 16), reads=res, is_output=True)

    Rc = [R["const"]]
    G(lambda e: e.memset(identb[:], 0.0), w=Rc)
    G(lambda e: e.affine_select(out=identb[:], in_=identb[:], pattern=[[-1, 128]], compare_op=ALU.not_equal, fill=1.0, base=0, channel_multiplier=1), r=Rc, w=Rc)
    G(lambda e: e.memset(identf[:], 0.0), w=Rc)
    G(lambda e: e.affine_select(out=identf[:], in_=identf[:], pattern=[[-1, 128]], compare_op=ALU.not_equal, fill=1.0, base=0, channel_multiplier=1), r=Rc, w=Rc)
    G(lambda e: e.memset(ones_b[:], 1.0), w=Rc)
    G(lambda e: e.memset(ones_f[:], 1.0), w=Rc)
    G(lambda e: e.memset(Utri[:], 1.0), w=Rc)
    G(lambda e: e.affine_select(out=Utri[:], in_=Utri[:], pattern=[[1, 128]], compare_op=ALU.is_ge, fill=0.0, base=0, channel_multiplier=-1), r=Rc, w=Rc)
    G(lambda e: e.memset(maskT[:], 0.0), w=Rc)
    G(lambda e: e.affine_select(out=maskT[:], in_=maskT[:], pattern=[[1, 128]], compare_op=ALU.is_ge, fill=-30000.0, base=0, channel_multiplier=-1), r=Rc, w=Rc)
    for h in range(HA):
        G(lambda e, h=h: e.memset(sel2[h][:], 0.0), w=Rc)
        G(lambda e, h=h: e.affine_select(out=sel2[h][:], in_=sel2[h][:], pattern=[[0, 128]], compare_op=ALU.not_equal, fill=-1.0, base=-h, channel_multiplier=1), r=Rc, w=Rc)
        G(lambda e, h=h: e.affine_select(out=sel2[h][:], in_=sel2[h][:], pattern=[[0, 128]], compare_op=ALU.not_equal, fill=-1.0, base=-(HA + h), channel_multiplier=1), r=Rc, w=Rc)
    G(lambda e: e.memset(Vext[:, :, :, 128:129], 1.0), w=[R["Vext"]])
    p.dma("sync", lambda e, s: e.dma_start(out=pp[:], in_=pp_d[:, :]).then_inc(s, 16), writes=[R["pp"]])

    def load_wg(e, s):
        for l in range(NL):
            src1 = w_in_d[l, :, c.c_im:c.c_im + 2 * HM].rearrange("(k p) c -> p k c", p=128)
            e.dma_start(out=wg[:, l, :, 0:2 * HM], in_=src1).then_inc(s, 16)
            src2 = w_in_d[l, :, c.c_fa:c.c_fa + HA].rearrange("(k p) c -> p k c", p=128)
            e.dma_start(out=wg[:, l, :, 2 * HM:NG], in_=src2).then_inc(s, 16)
    p.dma("gpsimd", load_wg, writes=[R["wg"]], ndma=2 * NL)

    in_runs = []
    i = 0
    while i < NBI:
        j = i
        while j + 1 < NBI and c.in_blocks[j + 1][2] == c.in_blocks[j][2] + 256:
            j += 1
        in_runs.append((c.in_blocks[i][2], j - i + 1, i))
        i = j + 1

    def mk_cast_in(l):
        def f(e, s):
            for kc in range(KC):
                for (c0, n, b0) in in_runs:
                    src = w_in_d[l, kc * 128:(kc + 1) * 128, c0:c0 + n * 256].rearrange("p (b c) -> p b c", c=256)
                    dst = wsc_in[l][b0:b0 + n, :, kc, :].rearrange("b p c -> p b c")
                    e.dma_start(out=dst, in_=src).then_inc(s, 16)
        return f, KC * len(in_runs)

    def mk_cast_sq(l, srcd, dstd, ncols):
        def f(e, s):
            for kc in range(KC):
                src = srcd[l, kc * 128:(kc + 1) * 128, 0:ncols].rearrange("p (b c) -> p b c", c=256)
                dst = dstd[l][0:ncols // 256, :, kc, :].rearrange("b p c -> p b c")
                e.dma_start(out=dst, in_=src).then_inc(s, 16)
        return f, KC

    def mk_cast_up(l):
        return mk_cast_sq(l, w_up_d, wsc_up, DFF)

    def mk_cast_dn(l):
        def f(e, s):
            for rg in range(DFF // 128):
                qd, kcl = rg // KC, rg % KC
                src = w_dn_d[l, rg * 128:(rg + 1) * 128, :].rearrange("p (b c) -> p b c", c=256)
                dst = wsc_dn[l][qd * NBO:(qd + 1) * NBO, :, kcl, :].rearrange("b p c -> p b c")
                e.dma_start(out=dst, in_=src).then_inc(s, 16)
        return f, DFF // 128

    def cast_ops(l):
        return [(mk_cast_in(l), R_win[l]), (mk_cast_sq(l, w_out_d, wsc_out, D), R_wout[l]), (mk_cast_up(l), R_wup[l]), (mk_cast_dn(l), R_wdn[l])]

    for (f, n), res in cast_ops(0):
        p.dma("gpsimd", f, writes=[res], ndma=n)

    wb_ctr = [0]

    def load_w(src_ap, res):
        slot = wb_ctr[0] % NWB
        wb_ctr[0] += 1
        p.dma("sync", lambda e, s: e.dma_start(out=wb[slot][:], in_=src_ap).then_inc(s, 16), reads=[res], writes=[R_wb[slot]])
        return slot

    def rmsnorm_h(l, goff, inplace_f32=False):
        b = nb()
        last = None
        for kc in range(KC):
            sq = sqb[kc % 2]
            A(lambda e, kc=kc, sq=sq: e.activation(out=sq[:], in_=x_t[:, kc, :], func=AF.Square), r=[R_x[kc]], w=[R_sqb[kc % 2]])
            T(lambda e, kc=kc, sq=sq: e.matmul(ps[b][:, :], lhsT=ones_b[:], rhs=sq[:], start=(kc == 0), stop=(kc == KC - 1)),
              r=[R_sqb[kc % 2], R["const"]], w=[R_ps[b]])
        V(lambda e: e.tensor_scalar(out=rs[:], in0=ps[b][:, :], scalar1=1.0 / D, scalar2=EPS, op0=ALU.mult, op1=ALU.add), r=[R_ps[b]], w=[R["rs"]])
        A(lambda e: e.activation(out=rs[:], in_=rs[:], func=AF.Sqrt), r=[R["rs"]], w=[R["rs"]])
        V(lambda e: e.reciprocal(out=rs[:], in_=rs[:]), r=[R["rs"]], w=[R["rs"]])
        for kc in range(KC):
            gc_ = pp[:, goff + kc: goff + kc + 1]
            if inplace_f32:
                V(lambda e, kc=kc, gc_=gc_: e.scalar_tensor_tensor(out=x_t[:, kc, :], in0=x_t[:, kc, :], scalar=gc_, in1=rs[:], op0=ALU.mult, op1=ALU.mult),
                  r=[R_x[kc], R["rs"], R["pp"]], w=[R_x[kc]])
            else:
                last = V(lambda e, kc=kc, gc_=gc_: e.scalar_tensor_tensor(out=h_t[:, kc, :], in0=x_t[:, kc, :], scalar=gc_, in1=rs[:], op0=ALU.mult, op1=ALU.mult),
                         r=[R_x[kc], R["rs"], R["pp"]], w=[R_h[kc]])
        return last

    def fm_group(slot, cb, rhs_t, R_rhs, b, split=1):
        per = (KC + split - 1) // split
        for k0 in range(0, KC, per):
            def f(e, k0=k0):
                for kc in range(k0, min(KC, k0 + per)):
                    ins = e.matmul(ps[b][:, :], lhsT=wb[slot][:, kc, cb * 128:(cb + 1) * 128], rhs=rhs_t[:, kc, :], start=(kc == 0), stop=(kc == KC - 1))
                return ins
            T(f, r=[R_wb[slot]] + R_rhs[k0:k0 + per], w=[R_ps[b]])

    def ckpt(name):
        if stop_at == name:
            raise _Stop()

    try:
      ckpt('setup')
      for s in range(NSEQ):
          for l in range(NL):
              lp = l * c.LPP
              p.dma("sync", lambda e, sm_, l=l: e.dma_start(out=fbl[:], in_=fb_d[l]).then_inc(sm_, 16), writes=[R["fbl"]])
              V(lambda e: e.memset(Cf[:], 0.0), w=[R["Cf"]])
              V(lambda e: e.memset(Cb[:], 0.0), w=[R["Cb"]])
              V(lambda e: e.memset(carry[:], 0.0), w=[R["carry"]])
              V(lambda e: e.memset(chist[:], 0.0), w=[R["chist"]])
              V(lambda e: e.memset(y0[:, :, 0:30], 0.0), w=[R["y0"]])
              for t in range(NT):
                  xi = s * NT + t
                  tok0 = t * TT
                  if l == 0:
                      k = 0
                      for bi in range(NB):
                          for dq in range(D // 512):
                              io = k % 2
                              k += 1
                              src = x_d[s, tok0 + bi * 128: tok0 + (bi + 1) * 128, dq * 512:(dq + 1) * 512]
                              p.dma("sync", lambda e, sm_, io=io, src=src: e.dma_start(out=stg_io[io][:], in_=src).then_inc(sm_, 16), writes=[R_io[io]])
                              b = nb()

                              def ftr(e, io=io, b=b):
                                  for i4 in range(4):
                                      ins = e.transpose(out=ps[b][:, i4 * 128:(i4 + 1) * 128], in_=stg_io[io][:, i4 * 128:(i4 + 1) * 128], identity=identf[:])
                                  return ins
                              T(ftr, r=[R_io[io], R["const"]], w=[R_ps[b]])
                              V(lambda e, b=b, dq=dq, bi=bi: e.tensor_copy(out=x_t[:, dq * 4:(dq + 1) * 4, bi * 128:(bi + 1) * 128],
                                                                       in_=ps[b][:, :].rearrange("p (k t) -> p k t", k=4)),
                                r=[R_ps[b]], w=R_x[dq * 4:(dq + 1) * 4])
                  else:
                      for kc in range(KC):
                          p.dma("sync", lambda e, sm_, xi=xi, kc=kc: e.dma_start(out=x_t[:, kc, :], in_=xs_d[xi][:, kc, :]).then_inc(sm_, 16), reads=[R_xs[xi][kc]], writes=[R_x[kc]])

                  ckpt('xload')
                  rmsnorm_h(l, lp + c.o_gmix)

                  ckpt('normA')
                  def gates_chain():
                      set_pool("G")
                      for bi in range(NB):
                          gblk = t * NB + bi
                          b = nb()

                          def fg(e, bi=bi, b=b):
                              for kc in range(KC):
                                  ins = e.matmul(ps[b][:, 0:NG], lhsT=h_t[:, kc, bi * 128:(bi + 1) * 128], rhs=wg[:, l, kc, :], start=(kc == 0), stop=(kc == KC - 1))
                              return ins
                          T(fg, r=R_h + [R["wg"]], w=[R_ps[b]])
                          V(lambda e, bi=bi, b=b: e.tensor_tensor(out=Gt[:, bi, :], in0=ps[b][:, 0:NG], in1=fbl[:, 0:NG], op=ALU.add), r=[R_ps[b], R["fbl"]], w=[R["Gt"]])
                          A(lambda e, bi=bi: e.activation(out=Et[:], in_=Gt[:, bi, HM:NG], func=AF.Exp, scale=-1.0), r=[R["Gt"]], w=[R["Et"]])
                          A(lambda e, bi=bi: e.activation(out=Lt[:, bi, :], in_=Et[:], func=AF.Ln, bias=1.0), r=[R["Et"]], w=[R["Lt"]])
                          b2 = nb()

                          def fcs(e, bi=bi, b2=b2):
                              e.matmul(ps[b2][:, 0:NF], lhsT=Utri[:], rhs=Lt[:, bi, :], start=True, stop=True)
                              return e.matmul(ps[b2][:, 64:64 + NF], lhsT=ones_f[:], rhs=Lt[:, bi, :], start=True, stop=True)
                          T(fcs, r=[R["Lt"], R["const"]], w=[R_ps[b2]])
                          V(lambda e, b2=b2: e.tensor_copy(out=gcs[:], in_=ps[b2][:, 0:128]), r=[R_ps[b2]], w=[R["gcs"]])
                          lnc = math.log(128.0 ** -0.5)
                          V(lambda e, bi=bi, b2=b2: e.scalar_tensor_tensor(out=gtmp[:], in0=gcs[:, 0:HM], scalar=lnc, in1=Gt[:, bi, 0:HM], op0=ALU.add, op1=ALU.add),
                            r=[R["gcs"], R["Gt"]], w=[R["gtmp"]])
                          A(lambda e, bi=bi: e.activation(out=aa[:, bi, :], in_=gtmp[:], func=AF.Exp), r=[R["gtmp"]], w=[R["aa"]])
                          A(lambda e, bi=bi, b2=b2: e.activation(out=einv[:, bi, :], in_=gcs[:, 0:HM], func=AF.Exp), r=[R["gcs"]], w=[R["einv"]])
                          A(lambda e, bi=bi, b2=b2: e.activation(out=ebl[:, bi, :], in_=gcs[:, 64:64 + HM], func=AF.Exp, scale=-1.0), r=[R["gcs"]], w=[R["ebl"]])
                          V(lambda e, b2=b2, gblk=gblk: e.tensor_tensor(out=nFtok[:, gblk, :], in0=gcs[:, HM:NF], in1=carry[:], op=ALU.add), r=[R["gcs"], R["carry"]], w=[R["nFt"]])
                          V(lambda e, b2=b2: e.tensor_tensor(out=carry[:], in0=gcs[:, 64 + HM:64 + NF], in1=carry[:], op=ALU.add), r=[R["gcs"], R["carry"]], w=[R["carry"]])
                          V(lambda e, gblk=gblk: e.tensor_copy(out=nF2[:, 0:HA], in_=nFtok[:, gblk, :]), r=[R["nFt"]], w=[R["nF2"]])
                          V(lambda e, gblk=gblk: e.tensor_tensor(out=nF2[:, HA:2 * HA], in0=nFtok[:, gblk, :], in1=nF2[:, 0:HA], op=ALU.subtract), r=[R["nFt"], R["nF2"]], w=[R["nF2"]])
                          b3 = nb()
                          T(lambda e, b3=b3: e.transpose(out=psb[b3][0:2 * HA, 0:128], in_=nF2[:, :], identity=identb[:]), r=[R["nF2"], R["const"]], w=[R_ps[b3]])
                          A(lambda e, b3=b3, gblk=gblk: e.activation(out=negF2[:, gblk * 128:(gblk + 1) * 128], in_=psb[b3][0:2 * HA, 0:128], func=AF.Copy),
                            r=[R_ps[b3]], w=[R["negF2"]])
                          V(lambda e, bi=bi: e.tensor_copy(out=vx[:, bi, :, 128:129], in_=aa[:, bi, :].unsqueeze(2)), r=[R["aa"]], w=[R["vx"]])

                  ckpt('gates')
                  def do_tm(kind, idx, slot):
                      for bi in range(NB):
                          b = nb()

                          def f(e, bi=bi, b=b):
                              for kc in range(KC):
                                  ins = e.matmul(ps[b][:, 0:256], lhsT=h_t[:, kc, bi * 128:(bi + 1) * 128], rhs=wb[slot][:, kc, :], start=(kc == 0), stop=(kc == KC - 1))
                              return ins
                          T(f, r=R_h + [R_wb[slot]], w=[R_ps[b]])
                          h0 = 2 * idx
                          pv = ps[b][:, 0:256].rearrange("p (h e) -> p h e", h=2)
                          if kind == "vm":
                              V(lambda e, bi=bi, pv=pv, h0=h0: e.tensor_tensor(out=vx[:, bi, h0:h0 + 2, 0:128], in0=pv,
                                                                            in1=aa[:, bi, h0:h0 + 2].unsqueeze(2).broadcast_to([128, 2, 128]), op=ALU.mult),
                                r=[R_ps[b], R["aa"]], w=[R["vx"]])
                          else:
                              gblk = t * NB + bi
                              A(lambda e, gblk=gblk, pv=pv, h0=h0: e.activation(out=Vext[:, gblk, h0:h0 + 2, 0:128], in_=pv, func=AF.Copy),
                                r=[R_ps[b]], w=[R["Vext"]])

                  def conv4_evac(b, cj, dest_ap, boff, R_dest):
                      A(lambda e: e.activation(out=cstgb[:, 3:3 + TT], in_=ps[b][:, :], func=AF.Identity, bias=pp[:, boff:boff + 1]),
                        r=[R_ps[b], R["pp"]], w=[R["cstgb"]])
                      V(lambda e: e.tensor_copy(out=cstgb[:, 0:3], in_=chist[:, cj, :]), r=[R["chist"]], w=[R["cstgb"]])
                      V(lambda e: e.tensor_copy(out=chist[:, cj, :], in_=cstgb[:, TT:TT + 3]), r=[R["cstgb"]], w=[R["chist"]])
                      wo = lp + c.o_mcw
                      bo = lp + c.o_mcb + cj
                      b2 = nb()
                      for tap in range(4):
                          k = dg_ctr["M"] % 2
                          dg_ctr["M"] += 1
                          wc = wo + tap * 2 * HM + cj
                          V(lambda e, k=k, wc=wc: e.tensor_scalar(out=dgM[k][:], in0=identb[:], scalar1=pp[:, wc:wc + 1], scalar2=None, op0=ALU.mult),
                            r=[R["const"], R["pp"]], w=[R_dgM[k]])
                          T(lambda e, k=k, tap=tap: e.matmul(ps[b2][:, :], lhsT=dgM[k][:], rhs=cstgb[:, tap:tap + TT], start=(tap == 0), stop=(tap == 3)),
                            r=[R_dgM[k], R["cstgb"]], w=[R_ps[b2]])
                      A(lambda e: e.activation(out=dest_ap, in_=ps[b2][:, :], func=AF.Silu, bias=pp[:, bo:bo + 1]), r=[R_ps[b2], R["pp"]], w=[R_dest])

                  def do_fm(kind, idx, slot, g, split=1):
                      for cb in range(2):
                          j = idx * 2 + cb
                          b = nb()
                          fm_group(slot, cb, h_t, R_h, b, split)
                          boff = lp + c.o_bfm[kind] + j
                          bias = pp[:, boff:boff + 1]
                          if kind in ("qm", "km"):
                              hl = j - g * HG
                              dest = (qT if kind == "qm" else kT)
                              conv4_evac(b, (0 if kind == "qm" else HM) + j, dest[:, hl, :], boff, R["qT"] if kind == "qm" else R["kT"])
                          elif kind == "om":
                              hl = j - g * HG
                              A(lambda e, hl=hl, b=b, bias=bias: e.activation(out=sgo[:, hl, :], in_=ps[b][:, :], func=AF.Sigmoid, bias=bias),
                                r=[R_ps[b], R["pp"]], w=[R["sgo"]])
                          elif kind == "qa":
                              V(lambda e, j=j, b=b, bias=bias: e.tensor_scalar(out=qTa[:, j, :], in0=ps[b][:, :], scalar1=bias, scalar2=128.0 ** -0.5, op0=ALU.add, op1=ALU.mult),
                                r=[R_ps[b], R["pp"]], w=[R["qTa"]])
                          elif kind == "ka":
                              A(lambda e, j=j, b=b, bias=bias: e.activation(out=KT[:, j, tok0:tok0 + TT], in_=ps[b][:, :], func=AF.Identity, bias=bias),
                                r=[R_ps[b], R["pp"]], w=[R["KT"]])
                          elif kind == "gc":
                              A(lambda e, cb=cb, b=b, bias=bias: e.activation(out=sg[:, cb, :], in_=ps[b][:, :], func=AF.Sigmoid, bias=bias),
                                r=[R_ps[b], R["pp"]], w=[R["sg"]])
                          elif kind == "uc":
                              V(lambda e, j=j, cb=cb, b=b, bias=bias: e.scalar_tensor_tensor(out=y0[:, j, 30:30 + TT], in0=ps[b][:, :], scalar=bias, in1=sg[:, cb, :],
                                                                                           op0=ALU.add, op1=ALU.mult), r=[R_ps[b], R["pp"], R["sg"]], w=[R["y0"]])

                  def nd_slot(hl):
                      return hl // 3, (hl % 3) * 129

                  def mlstm_group(g):
                      for bi in range(NB):
                          c0 = bi * 128
                          hs = g * HG
                          bS = nb()

                          def fS(e, bS=bS, c0=c0):
                              for hl in range(HG):
                                  ins = e.matmul(ps[bS][:, hl * 128:(hl + 1) * 128], lhsT=kT[:, hl, c0:c0 + 128], rhs=qT[:, hl, c0:c0 + 128], start=True, stop=True)
                              return ins
                          T(fS, r=[R["qT"], R["kT"]], w=[R_ps[bS]])
                          V(lambda e, bS=bS: e.tensor_tensor(out=sm["STm"].rearrange("p (h l) -> p h l", h=HG), in0=ps[bS][:, 0:HG * 128].rearrange("p (h l) -> p h l", h=HG),
                                                           in1=Utri[:].unsqueeze(1).broadcast_to([128, HG, 128]), op=ALU.mult), r=[R_ps[bS], R["const"]], w=[R["STm"]])
                          bK = nb()

                          def fK(e, bK=bK, c0=c0):
                              for hl in range(HG):
                                  ins = e.transpose(out=psb[bK][:, hl * 128:(hl + 1) * 128], in_=kT[:, hl, c0:c0 + 128], identity=identb[:])
                              return ins
                          T(fK, r=[R["kT"], R["const"]], w=[R_ps[bK]])
                          A(lambda e, bK=bK: e.activation(out=sm["ktok"], in_=psb[bK][:, 0:HG * 128], func=AF.Copy), r=[R_ps[bK]], w=[R["ktok"]])
                          nbk = (HG + 2) // 3
                          bN = [nb() for _ in range(nbk)]

                          def fN(e, bN=bN, c0=c0, bi=bi):
                              for hl in range(HG):
                                  bk, off = nd_slot(hl)
                                  o = ps[bN[bk]][:, off:off + 129]
                                  e.matmul(o, lhsT=qT[:, hl, c0:c0 + 128], rhs=Cb[:, hs + hl, :], start=True, stop=False)
                                  ins = e.matmul(o, lhsT=sm["STm"][:, hl * 128:(hl + 1) * 128], rhs=vx[:, bi, hs + hl, :], start=False, stop=True)
                              return ins
                          T(fN, r=[R["qT"], R["Cb"], R["STm"], R["vx"]], w=[R_ps[x] for x in bN])
                          for bk in range(nbk):
                              h0 = bk * 3
                              n = min(3, HG - h0)
                              pv = ps[bN[bk]][:, 0:n * 129].rearrange("p (h e) -> p h e", h=n)
                              V(lambda e, pv=pv, h0=h0, n=n: e.tensor_reduce(out=dn[:, h0:h0 + n], in_=pv[:, :, 128:129], axis=AX.X, op=ALU.max, apply_absolute_value=True),
                                r=[R_ps[bN[bk]]], w=[R["dn"]])
                          V(lambda e, bi=bi: e.tensor_tensor(out=dn[:], in0=dn[:], in1=einv[:, bi, hs:hs + HG], op=ALU.max), r=[R["dn"], R["einv"]], w=[R["dn"]])
                          V(lambda e: e.reciprocal(out=rr[:], in_=dn[:]), r=[R["dn"]], w=[R["rr"]])
                          for bk in range(nbk):
                              h0 = bk * 3
                              n = min(3, HG - h0)
                              pv = ps[bN[bk]][:, 0:n * 129].rearrange("p (h e) -> p h e", h=n)
                              V(lambda e, pv=pv, h0=h0, n=n: e.tensor_tensor(out=dr[:, h0:h0 + n], in0=pv[:, :, 128], in1=rr[:, h0:h0 + n], op=ALU.mult),
                                r=[R_ps[bN[bk]], R["rr"]], w=[R["dr"]])
                              V(lambda e, pv=pv, h0=h0, n=n: e.tensor_tensor(out=sm["hf"][:, h0 * 128:(h0 + n) * 128].rearrange("p (h e) -> p h e", h=n), in0=pv[:, :, 0:128],
                                                                           in1=rr[:, h0:h0 + n].unsqueeze(2).broadcast_to([128, n, 128]), op=ALU.mult),
                                r=[R_ps[bN[bk]], R["rr"]], w=[R["hf"]])
                          for hl in range(HG):
                              hh = hs + hl
                              V(lambda e, hl=hl, hh=hh: e.scalar_tensor_tensor(out=sm["hf"][:, hl * 128:(hl + 1) * 128], in0=fbl[:, NG + hh * 128: NG + (hh + 1) * 128],
                                                                             scalar=dr[:, hl:hl + 1], in1=sm["hf"][:, hl * 128:(hl + 1) * 128], op0=ALU.mult, op1=ALU.add),
                                r=[R["fbl"], R["dr"], R["hf"]], w=[R["hf"]])
                          V(lambda e: e.tensor_tensor(out=sm["sqf"], in0=sm["hf"], in1=sm["hf"], op=ALU.mult), r=[R["hf"]], w=[R["sqf"]])
                          V(lambda e: e.tensor_reduce(out=ss[:], in_=sm["sqf"].rearrange("p (h e) -> p h e", h=HG), axis=AX.X, op=ALU.add), r=[R["sqf"]], w=[R["ss"]])
                          V(lambda e: e.tensor_scalar(out=ss[:], in0=ss[:], scalar1=1.0 / 128, scalar2=EPS, op0=ALU.mult, op1=ALU.add), r=[R["ss"]], w=[R["ss"]])
                          A(lambda e: e.activation(out=ss[:], in_=ss[:], func=AF.Sqrt), r=[R["ss"]], w=[R["ss"]])
                          V(lambda e: e.reciprocal(out=ss[:], in_=ss[:]), r=[R["ss"]], w=[R["ss"]])
                          V(lambda e: e.tensor_tensor(out=sm["ytok"].rearrange("p (h e) -> p h e", h=HG), in0=sm["hf"].rearrange("p (h e) -> p h e", h=HG),
                                                      in1=ss[:].unsqueeze(2).broadcast_to([128, HG, 128]), op=ALU.mult), r=[R["hf"], R["ss"]], w=[R["ytok"]])
                          bY = nb()

                          def fY(e, bY=bY):
                              for hl in range(HG):
                                  ins = e.transpose(out=psb[bY][:, hl * 128:(hl + 1) * 128], in_=sm["ytok"][:, hl * 128:(hl + 1) * 128], identity=identb[:])
                              return ins
                          T(fY, r=[R["ytok"], R["const"]], w=[R_ps[bY]])
                          for hl in range(HG):
                              hh = hs + hl
                              go = lp + c.o_ghm + hh
                              V(lambda e, hl=hl, hh=hh, go=go, bY=bY, c0=c0: e.scalar_tensor_tensor(out=m_t[:, hh, c0:c0 + 128], in0=psb[bY][:, hl * 128:(hl + 1) * 128],
                                                                                                  scalar=pp[:, go:go + 1], in1=sgo[:, hl, c0:c0 + 128], op0=ALU.mult, op1=ALU.mult),
                                r=[R_ps[bY], R["pp"], R["sgo"]], w=[R_m[hh]])
                          bD = [nb() for _ in range(nbk)]

                          def fD(e, bD=bD, bi=bi):
                              for hl in range(HG):
                                  bk, off = nd_slot(hl)
                                  ins = e.matmul(ps[bD[bk]][:, off:off + 129], lhsT=sm["ktok"][:, hl * 128:(hl + 1) * 128], rhs=vx[:, bi, hs + hl, :], start=True, stop=True)
                              return ins
                          T(fD, r=[R["ktok"], R["vx"]], w=[R_ps[x] for x in bD])
                          for bk in range(nbk):
                              h0 = bk * 3
                              n = min(3, HG - h0)
                              pv = ps[bD[bk]][:, 0:n * 129].rearrange("p (h e) -> p h e", h=n)
                              V(lambda e, pv=pv, h0=h0, n=n: e.tensor_tensor(out=Cf[:, hs + h0:hs + h0 + n, :], in0=pv, in1=Cf[:, hs + h0:hs + h0 + n, :], op=ALU.add),
                                r=[R_ps[bD[bk]], R["Cf"]], w=[R["Cf"]])
                          V(lambda e, bi=bi: e.tensor_tensor(out=Cf[:, hs:hs + HG, :], in0=Cf[:, hs:hs + HG, :],
                                                           in1=ebl[:, bi, hs:hs + HG].unsqueeze(2).broadcast_to([128, HG, 129]), op=ALU.mult), r=[R["Cf"], R["ebl"]], w=[R["Cf"]])
                          A(lambda e: e.activation(out=Cb[:, hs:hs + HG, :], in_=Cf[:, hs:hs + HG, :], func=AF.Copy), r=[R["Cf"]], w=[R["Cb"]])

                  def fox_stream(st_):
                      SB = POOLS["F%d" % st_][0:2]
                      FO = POOLS["F%d" % st_][2]
                      halves = [(sqb[st_], R_sqb[st_]), (PTc[:, st_ * 512:(st_ + 1) * 512], R["PTc%d" % st_])]
                      rr1_, ss1_, of_, ya_ = rr1s[st_], ss1s[st_], ofs[st_], yas[st_]
                      R_rr1, R_ss1, R_of, R_ya = R["rr1_%d" % st_], R["ss1_%d" % st_], R_ofs[st_], R["ya_%d" % st_]
                      it = -1
                      for bi in range(NB):
                          gi = t * NB + bi
                          nk = gi + 1
                          ng = (nk + 3) // 4
                          for ha in range(HA):
                              it += 1
                              if it % 2 != st_:
                                  continue

                              def slot(kb):
                                  hv = halves[(kb // 4) % 2][0]
                                  return hv[:, (kb % 4) * 128:(kb % 4 + 1) * 128]

                              def scores(g, bi=bi, ha=ha, gi=gi, nk=nk):
                                  bank = SB[g % 2]

                                  def f(e):
                                      for kb in range(g * 4, min(nk, g * 4 + 4)):
                                          o = ps[bank][:, (kb % 4) * 128:(kb % 4 + 1) * 128]
                                          e.matmul(o, lhsT=KT[:, ha, kb * 128:(kb + 1) * 128], rhs=qTa[:, ha, bi * 128:(bi + 1) * 128], start=True, stop=False)
                                          last = kb == gi
                                          ins = e.matmul(o, lhsT=sel2[ha][:], rhs=negF2[:, gi * 128:(gi + 1) * 128], start=False, stop=not last)
                                          if last:
                                              ins = e.matmul(o, lhsT=identb[:], rhs=maskT[:], start=False, stop=True)
                                      return ins
                                  T(f, r=[R["qTa"], R["KT"], R["negF2"], R["const"]], w=[R_ps[bank]])

                              def exps(g, ha=ha, nk=nk):
                                  bank = SB[g % 2]
                                  hres = halves[g % 2][1]
                                  for kb in range(g * 4, min(nk, g * 4 + 4)):
                                      A(lambda e, kb=kb: e.activation(out=slot(kb), in_=ps[bank][:, (kb % 4) * 128:(kb % 4 + 1) * 128], func=AF.Exp, bias=nFtok[:, kb, ha:ha + 1]),
                                        r=[R_ps[bank], R["nFt"]], w=[hres])

                              def pv(g, ha=ha, nk=nk):
                                  hres = halves[g % 2][1]

                                  def f(e):
                                      for kb in range(g * 4, min(nk, g * 4 + 4)):
                                          ins = e.matmul(ps[FO][:, 0:129], lhsT=slot(kb), rhs=Vext[:, kb, ha, :], start=(kb == 0), stop=(kb == nk - 1))
                                      return ins
                                  T(f, r=[hres, R["Vext"]], w=[R_ps[FO]])

                              scores(0)
                              exps(0)
                              for g in range(1, ng):
                                  scores(g)
                                  exps(g)
                                  pv(g - 1)
                              pv(ng - 1)
                              V(lambda e: e.reciprocal(out=rr1_[:], in_=ps[FO][:, 128:129]), r=[R_ps[FO]], w=[R_rr1])
                              bvo = NG + DM + ha * 128
                              V(lambda e, bvo=bvo: e.scalar_tensor_tensor(out=of_, in0=ps[FO][:, 0:128], scalar=rr1_[:, 0:1], in1=fbl[:, bvo:bvo + 128], op0=ALU.mult, op1=ALU.add),
                                r=[R_ps[FO], R_rr1, R["fbl"]], w=[R_of])
                              V(lambda e: e.tensor_tensor(out=ya_[:], in0=of_, in1=of_, op=ALU.mult), r=[R_of], w=[R_ya])
                              V(lambda e: e.tensor_reduce(out=ss1_[:], in_=ya_[:], axis=AX.X, op=ALU.add), r=[R_ya], w=[R_ss1])
                              V(lambda e: e.tensor_scalar(out=ss1_[:], in0=ss1_[:], scalar1=1.0 / 128, scalar2=EPS, op0=ALU.mult, op1=ALU.add), r=[R_ss1], w=[R_ss1])
                              A(lambda e: e.activation(out=ss1_[:], in_=ss1_[:], func=AF.Sqrt), r=[R_ss1], w=[R_ss1])
                              V(lambda e: e.reciprocal(out=ss1_[:], in_=ss1_[:]), r=[R_ss1], w=[R_ss1])
                              V(lambda e: e.tensor_scalar(out=ya_[:], in0=of_, scalar1=ss1_[:, 0:1], scalar2=None, op0=ALU.mult), r=[R_of, R_ss1], w=[R_ya])
                              T(lambda e: e.transpose(out=psb[FO][:, 0:128], in_=ya_[:], identity=identb[:]), r=[R_ya, R["const"]], w=[R_ps[FO]])
                              go = lp + c.o_gha + ha
                              A(lambda e, go=go, ha=ha, bi=bi: e.activation(out=m_t[:, HM + ha, bi * 128:(bi + 1) * 128], in_=psb[FO][:, 0:128], func=AF.Identity, scale=pp[:, go:go + 1]),
                                r=[R_ps[FO], R["pp"]], w=[R_m[HM + ha]])

                  def conv_all():
                      wo = lp + c.o_cdw
                      cmean = rs[:]
                      cm2 = sg[:, 0, :]
                      csq = sg[:, 1, :]
                      for cc in range(CC):
                          bC = nb()
                          for tap in range(31):
                              k = dg_ctr["C"] % 4
                              dg_ctr["C"] += 1
                              wc = wo + tap * CC + cc
                              V(lambda e, k=k, wc=wc: e.tensor_scalar(out=dgC[k][:], in0=identb[:], scalar1=pp[:, wc:wc + 1], scalar2=None, op0=ALU.mult),
                                r=[R["const"], R["pp"]], w=[R_dgC[k]])
                              T(lambda e, k=k, tap=tap, cc=cc, bC=bC: e.matmul(ps[bC][:, :], lhsT=dgC[k][:], rhs=y0[:, cc, tap:tap + TT], start=(tap == 0), stop=(tap == 30)),
                                r=[R_dgC[k], R["y0"]], w=[R_ps[bC]])
                          bo_ = lp + c.o_cdb + cc
                          A(lambda e, cc=cc, bC=bC, bo_=bo_: e.activation(out=cvt[cc][:], in_=ps[bC][:, :], func=AF.Identity, bias=pp[:, bo_:bo_ + 1]),
                            r=[R_ps[bC], R["pp"]], w=[R_cv[cc]])
                          A(lambda e, cc=cc: e.activation(out=y0[:, cc, 0:30], in_=y0[:, cc, TT:TT + 30], func=AF.Copy), r=[R["y0"]], w=[R["y0"]])
                      bM = nb()
                      for cc in range(CC):
                          T(lambda e, cc=cc: e.matmul(ps[bM][:, :], lhsT=ones_f[:], rhs=cvt[cc][:], start=(cc == 0), stop=(cc == CC - 1)), r=[R_cv[cc], R["const"]], w=[R_ps[bM]])
                      V(lambda e: e.tensor_scalar(out=cmean, in0=ps[bM][:, :], scalar1=1.0 / DC, scalar2=None, op0=ALU.mult), r=[R_ps[bM]], w=[R["rs"]])
                      bQ = nb()
                      for cc in range(CC):
                          A(lambda e, cc=cc: e.activation(out=csq, in_=cvt[cc][:], func=AF.Square), r=[R_cv[cc]], w=[R["sg"]])
                          T(lambda e, cc=cc: e.matmul(ps[bQ][:, :], lhsT=ones_f[:], rhs=csq, start=(cc == 0), stop=(cc == CC - 1)), r=[R["sg"], R["const"]], w=[R_ps[bQ]])
                      V(lambda e: e.tensor_tensor(out=cm2, in0=cmean, in1=cmean, op=ALU.mult), r=[R["rs"]], w=[R["sg"]])
                      V(lambda e: e.scalar_tensor_tensor(out=cm2, in0=ps[bQ][:, :], scalar=1.0 / DC, in1=cm2, op0=ALU.mult, op1=ALU.subtract), r=[R_ps[bQ], R["sg"]], w=[R["sg"]])
                      V(lambda e: e.tensor_scalar(out=cm2, in0=cm2, scalar1=EPS, scalar2=None, op0=ALU.add), r=[R["sg"]], w=[R["sg"]])
                      A(lambda e: e.activation(out=cm2, in_=cm2, func=AF.Sqrt), r=[R["sg"]], w=[R["sg"]])
                      V(lambda e: e.reciprocal(out=cm2, in_=cm2), r=[R["sg"]], w=[R["sg"]])
                      for cc in range(CC):
                          V(lambda e, cc=cc: e.tensor_tensor(out=cvt[cc][:], in0=cvt[cc][:], in1=cmean, op=ALU.subtract), r=[R_cv[cc], R["rs"]], w=[R_cv[cc]])
                          V(lambda e, cc=cc: e.tensor_tensor(out=cvt[cc][:], in0=cvt[cc][:], in1=cm2, op=ALU.mult), r=[R_cv[cc], R["sg"]], w=[R_cv[cc]])
                          go = lp + c.o_clg + cc
                          bo = lp + c.o_clb + cc
                          A(lambda e, cc=cc, go=go, bo=bo: e.activation(out=m_t[:, HM + HA + cc, :], in_=cvt[cc][:], func=AF.Silu, scale=pp[:, go:go + 1], bias=pp[:, bo:bo + 1]),
                            r=[R_cv[cc], R["pp"]], w=[R_m[HM + HA + cc]])

                  hb = HG // 2
                  pre = list(enumerate(c.in_blocks[:c.n_pre_blocks]))

                  def pre_chain():
                      set_pool("P")
                      for bidx_, (kind, idx, c0) in pre:
                          if kind == "vm":
                              continue
                          slot = load_w(wsc_in[l][bidx_], R_win[l])
                          if kind == "va":
                              do_tm(kind, idx, slot)
                          else:
                              do_fm(kind, idx, slot, 0, split=4)

                  if INTERLEAVE:
                      p.run_chains([gates_chain, pre_chain], (1, 1))
                  else:
                      gates_chain()
                      pre_chain()
                  set_pool("all")
                  for bidx_, (kind, idx, c0) in pre:
                      if kind == "vm":
                          slot = load_w(wsc_in[l][bidx_], R_win[l])
                          do_tm(kind, idx, slot)
                  ckpt('proj')

                  def m_chain():
                      set_pool("M")
                      bi_ = c.n_pre_blocks
                      for (kind, idx, c0) in c.in_blocks[c.n_pre_blocks:]:
                          slot = load_w(wsc_in[l][bi_], R_win[l])
                          bi_ += 1
                          g = idx // hb
                          do_fm(kind, idx, slot, g, split=4)
                          if kind == "om" and (idx % hb) == hb - 1:
                              mlstm_group(g)

                  def c_chain():
                      set_pool("C")
                      conv_all()

                  if INTERLEAVE:
                      p.run_chains([lambda: fox_stream(0), lambda: fox_stream(1), m_chain, c_chain], CH_W)
                  else:
                      m_chain()
                      c_chain()
                      fox_stream(0)
                      fox_stream(1)
                  set_pool("all")
                  ckpt('conv')
                  for j in range(NBO):
                      slot = load_w(wsc_out[l][j], R_wout[l])
                      for cb in range(2):
                          b = nb()
                          fm_group(slot, cb, m_t, R_m, b)
                          dblk = 2 * j + cb
                          V(lambda e, b=b, dblk=dblk: e.tensor_tensor(out=x_t[:, dblk, :], in0=x_t[:, dblk, :], in1=ps[b][:, :], op=ALU.add), r=[R_ps[b], R_x[dblk]], w=[R_x[dblk]])

                  ckpt('stageC')
                  nid = rmsnorm_h(l, lp + c.o_gffn)
                  if s == 0 and l + 1 < NL and t < 4:
                      co = cast_ops(l + 1)
                      sel = co[t:t + 1] if NT >= 4 else (co if t == 0 else [])
                      for (f, n), res in sel:
                          p.dma("gpsimd", f, writes=[res], ndma=n, after=[nid])
                  kk = 0
                  for qd in range(4):
                      for j in range(NBO):
                          slot = load_w(wsc_up[l][qd * NBO + j], R_wup[l])
                          for cb in range(2):
                              fcl = 2 * j + cb
                              b = nb()
                              fm_group(slot, cb, h_t, R_h, b)
                              ri = kk % 2
                              kk += 1
                              A(lambda e, b=b, ri=ri: e.activation(out=rl[ri][:], in_=ps[b][:, :], func=AF.Relu), r=[R_ps[b]], w=[R_rl[ri]])
                              V(lambda e, ri=ri, fcl=fcl: e.tensor_tensor(out=m_t[:, fcl, :], in0=rl[ri][:], in1=rl[ri][:], op=ALU.mult), r=[R_rl[ri]], w=[R_m[fcl]])
                      for j in range(NBO):
                          slot = load_w(wsc_dn[l][qd * NBO + j], R_wdn[l])
                          for cb in range(2):
                              dblk = 2 * j + cb
                              b = nb()
                              fm_group(slot, cb, m_t, R_m, b)
                              V(lambda e, b=b, dblk=dblk: e.tensor_tensor(out=x_t[:, dblk, :], in0=x_t[:, dblk, :], in1=ps[b][:, :], op=ALU.add), r=[R_ps[b], R_x[dblk]], w=[R_x[dblk]])

                  ckpt('ffn')
                  if l < NL - 1:
                      for kc in range(KC):
                          p.dma("sync", lambda e, sm_, xi=xi, kc=kc: e.dma_start(out=xs_d[xi][:, kc, :], in_=x_t[:, kc, :]).then_inc(sm_, 16), reads=[R_x[kc]], writes=[R_xs[xi][kc]])
                  else:
                      rmsnorm_h(l, c.o_gfin, inplace_f32=True)
                      k = 0
                      for bi in range(NB):
                          for dq in range(D // 512):
                              io = k % 2
                              k += 1
                              b = nb()

                              def fto(e, b=b, dq=dq, bi=bi):
                                  for i4 in range(4):
                                      ins = e.transpose(out=ps[b][:, i4 * 128:(i4 + 1) * 128], in_=x_t[:, dq * 4 + i4, bi * 128:(bi + 1) * 128], identity=identf[:])
                                  return ins
                              T(fto, r=R_x[dq * 4:(dq + 1) * 4] + [R["const"]], w=[R_ps[b]])
                              if k % 2 == 0:
                                  V(lambda e, b=b, io=io: e.tensor_copy(out=stg_io[io][:], in_=ps[b][:, :]), r=[R_ps[b]], w=[R_io[io]])
                              else:
                                  A(lambda e, b=b, io=io: e.activation(out=stg_io[io][:], in_=ps[b][:, :], func=AF.Copy), r=[R_ps[b]], w=[R_io[io]])
                              dst = y_d[s, tok0 + bi * 128: tok0 + (bi + 1) * 128, dq * 512:(dq + 1) * 512]
                              p.dma("sync", lambda e, sm_, io=io, dst=dst: e.dma_start(out=dst, in_=stg_io[io][:]).then_inc(sm_, 16), reads=[R_io[io]], is_output=True)

    except _Stop:
        pass
    p.emit()
    st.close()
    return nc, p, dbg_out


_CACHE = {}


def kernel(**inputs):
    cfg = Cfg()
    ncores = 8
    pp, fb = pack_params(cfg, inputs)
    nc, prog, _ = build(cfg)
    x = np.ascontiguousarray(inputs["x"], dtype=np.float32)
    in_maps = []
    for ci in range(ncores):
        in_maps.append({
            "x": np.ascontiguousarray(x[ci * cfg.NSEQ:(ci + 1) * cfg.NSEQ]),
            "w_in": inputs["w_in"], "w_out": inputs["w_out"], "w_up": inputs["w_up"], "w_down": inputs["w_down"],
            "pp": pp, "fb": fb,
        })
    res = run_bass_kernel_spmd(nc, in_maps, core_ids=list(range(ncores)))
    out = np.concatenate([np.asarray(r["y"]) for r in res.results], axis=0)
    return out.astype(np.float32, copy=False)
```

```python
import math
from contextlib import ExitStack
import numpy as np
import concourse.bass as bass
import concourse.mybir as mybir
from concourse.bass_utils import run_bass_kernel_spmd

F32 = mybir.dt.float32
BF16 = mybir.dt.bfloat16
AF = mybir.ActivationFunctionType
ALU = mybir.AluOpType
AX = mybir.AxisListType
EPS = 1e-6
ENGS = ("sync", "scalar", "vector", "gpsimd", "tensor")
NSLOT = 16


class Res:
    __slots__ = ("name", "w", "r", "excl")

    def __init__(self, name, excl=False):
        self.name = name
        self.w = None
        self.r = {}
        self.excl = excl


class Op:
    __slots__ = ("eng", "fn", "deps", "sig", "rank", "is_dma", "slot", "val", "ndma", "calls")


class _Tok:
    def __init__(self, rec, idx):
        self.rec, self.idx = rec, idx

    def then_inc(self, sem, n):
        self.rec.calls[self.idx][3] = n
        return self


class _RecEng:
    def __init__(self):
        self.calls = []

    def __getattr__(self, name):
        def f(*a, **k):
            self.calls.append([name, a, k, None])
            return _Tok(self, len(self.calls) - 1)
        return f


class Prog:
    def __init__(self, nc):
        self.nc = nc
        self.ops = []
        self.q = {e: [] for e in ENGS}
        self.dma_cnt = {"sync": 0, "gpsimd": 0, "scalar": 0}
        self.slot_total = {}
        self.slot_last = {}
        self.out_ops = []
        self.cur_chain = None

    def _key(self, oid):
        op = self.ops[oid]
        return ("dma", oid) if op.is_dma else op.eng

    def _rec(self, eng, fn, reads, writes, is_dma=False, ndma=1, is_output=False, after=()):
        rec = _RecEng()
        if fn is not None:
            if is_dma:
                fn(rec, None)
            else:
                fn(rec)
        ent = (eng, rec.calls, list(reads), list(writes), is_dma, ndma, is_output, tuple(after))
        if self.cur_chain is not None:
            self.cur_chain.append(ent)
            return None
        return self.commit(ent)

    def commit(self, ent):
        eng, calls, reads, writes, is_dma, ndma, is_output, after = ent
        if any(r.excl for r in reads):
            writes = list(writes) + [r for r in reads if r.excl]
            reads = [r for r in reads if not r.excl]
        deps = set(after)
        for r in reads:
            if r.w is not None:
                deps.add(r.w)
        for r in writes:
            if r.w is not None:
                deps.add(r.w)
            deps.update(r.r.values())
        op = Op()
        op.eng, op.fn, op.sig, op.rank, op.is_dma, op.ndma = eng, True, False, 0, is_dma, ndma
        op.slot = op.val = None
        oid = len(self.ops)
        if is_dma:
            j = self.dma_cnt[eng]
            self.dma_cnt[eng] = j + 1
            slot = j % NSLOT
            op.slot = slot
            tot = self.slot_total.get((eng, slot), 0) + 16 * ndma
            self.slot_total[(eng, slot)] = tot
            op.val = tot
            prev = self.slot_last.get((eng, slot))
            if prev is not None:
                deps.add(prev)
            self.slot_last[(eng, slot)] = oid
        if eng == "tensor":
            deps = {d for d in deps if not (self.ops[d].eng == "tensor" and not self.ops[d].is_dma)}
        op.deps = deps
        op.calls = calls
        self.ops.append(op)
        self.q[eng].append(oid)
        key = ("dma", oid) if is_dma else eng
        for r in reads:
            r.r[key] = oid
        for r in writes:
            r.w = oid
            r.r = {}
        if is_output:
            self.out_ops.append(oid)
        return oid

    def op(self, eng, fn, reads=(), writes=()):
        return self._rec(eng, fn, reads, writes)

    def dma(self, queue, fn, reads=(), writes=(), ndma=1, is_output=False, after=()):
        return self._rec(queue, fn, reads, writes, is_dma=True, ndma=ndma, is_output=is_output, after=after)

    def run_chains(self, chains, weights):
        lists = []
        for fn in chains:
            self.cur_chain = []
            fn()
            lists.append(self.cur_chain)
            self.cur_chain = None
        idx = [0] * len(lists)
        while any(idx[i] < len(lists[i]) for i in range(len(lists))):
            for i, L in enumerate(lists):
                for _ in range(weights[i]):
                    if idx[i] < len(L):
                        self.commit(L[idx[i]])
                        idx[i] += 1

    def fence(self, frm, to):
        evs = {}
        for fr in frm:
            ids = list(fr.r.values())
            if fr.w is not None:
                ids.append(fr.w)
            for oid in ids:
                k = self._key(oid)
                if evs.get(k, -1) < oid:
                    evs[k] = oid
        for t in to:
            for k, v in evs.items():
                if t.r.get(k, -1) < v:
                    t.r[k] = v

    def emit(self):
        nc = self.nc
        ops = self.ops
        fin = Op()
        fin.eng, fin.fn, fin.sig, fin.rank, fin.is_dma, fin.ndma = "sync", None, False, 0, False, 0
        fin.slot = fin.val = None
        fin.deps = set(self.out_ops)
        fin.calls = []
        ops.append(fin)
        self.q["sync"].append(len(ops) - 1)
        for op in ops:
            for d in op.deps:
                if not ops[d].is_dma:
                    ops[d].sig = True
        for e in ENGS:
            r = 0
            for oid in self.q[e]:
                op = ops[oid]
                if (not op.is_dma) and op.sig:
                    r += 1
                    op.rank = r
        self.nwaits = 0
        with ExitStack() as st:
            esem = {e: st.enter_context(nc.semaphore("es_" + e)) for e in ENGS}
            dsem = {}
            for qn in ("sync", "gpsimd", "scalar"):
                for i in range(NSLOT):
                    dsem[(qn, i)] = st.enter_context(nc.semaphore("ds_%s_%d" % (qn, i)))
            block = st.enter_context(nc.Block())

            def run(ename, e):
                known = {}
                for oid in self.q[ename]:
                    op = ops[oid]
                    waits = {}
                    for d in op.deps:
                        x = ops[d]
                        if x.is_dma:
                            key, val = ("d", x.eng, x.slot), x.val
                        else:
                            key, val = ("e", x.eng), x.rank
                        if waits.get(key, 0) < val:
                            waits[key] = val
                    for key, val in waits.items():
                        if known.get(key, 0) >= val:
                            continue
                        sem = dsem[(key[1], key[2])] if key[0] == "d" else esem[key[1]]
                        e.wait_ge(sem, val)
                        self.nwaits += 1
                        known[key] = val
                    if op.fn is None:
                        continue
                    if op.is_dma:
                        for (name, a, k, n) in op.calls:
                            getattr(e, name)(*a, **k).then_inc(dsem[(ename, op.slot)], 16)
                    else:
                        ins = None
                        for (name, a, k, n) in op.calls:
                            ins = getattr(e, name)(*a, **k)
                        if op.sig:
                            ins.then_inc(esem[ename], 1)

            @block.sync
            def _(e):
                run("sync", e)

            @block.scalar
            def _(e):
                run("scalar", e)

            @block.vector
            def _(e):
                run("vector", e)

            @block.gpsimd
            def _(e):
                run("gpsimd", e)

            @block.tensor
            def _(e):
                run("tensor", e)


class Cfg:
    def __init__(self, D=2048, S=2048, NL=4, NSEQ=2, HG=4):
        self.D, self.S, self.NL, self.NSEQ = D, S, NL, NSEQ
        self.KC = D // 128
        self.DM = D // 2
        self.HM = self.DM // 128
        self.DF = D // 4
        self.HA = self.DF // 128
        self.DC = D - self.DM - self.DF
        self.CC = self.DC // 128
        self.DFF = 4 * D
        self.HG = min(HG, self.HM)
        self.NGM = self.HM // self.HG
        self.TT = 512
        self.NT = S // self.TT
        self.NB = 4
        self.NBLK = S // 128
        DM, HM, DF, HA, DC = self.DM, self.HM, self.DF, self.HA, self.DC
        self.DIN = 4 * DM + 2 * HM + 3 * DF + HA + 2 * DC
        self.c_qm, self.c_km, self.c_vm, self.c_om = 0, DM, 2 * DM, 3 * DM
        self.c_im, self.c_fm = 4 * DM, 4 * DM + HM
        self.c_qa = 4 * DM + 2 * HM
        self.c_ka = self.c_qa + DF
        self.c_va = self.c_qa + 2 * DF
        self.c_fa = self.c_qa + 3 * DF
        self.c_uc = self.c_fa + HA
        self.c_gc = self.c_uc + DC
        self.NG = 2 * HM + HA
        self.NF = HM + HA
        self.NBO = D // 256
        blks = []
        for i in range(DM // 256):
            blks.append(("vm", i, self.c_vm + i * 256))
        for i in range(DF // 256):
            blks.append(("va", i, self.c_va + i * 256))
        for i in range(DF // 256):
            blks.append(("qa", i, self.c_qa + i * 256))
        for i in range(DF // 256):
            blks.append(("ka", i, self.c_ka + i * 256))
        for i in range(DC // 256):
            blks.append(("gc", i, self.c_gc + i * 256))
            blks.append(("uc", i, self.c_uc + i * 256))
        self.n_pre_blocks = len(blks)
        hb = self.HG // 2
        for g in range(self.NGM):
            for kind, c0 in (("qm", self.c_qm), ("km", self.c_km), ("om", self.c_om)):
                for i in range(g * hb, (g + 1) * hb):
                    blks.append((kind, i, c0 + i * 256))
        self.in_blocks = blks
        self.NBI = len(blks)
        KC, CC = self.KC, self.CC
        o = 0
        self.o_gmix = o; o += KC
        self.o_gffn = o; o += KC
        self.fm_kinds = [("qm", HM, self.c_qm), ("km", HM, self.c_km), ("om", HM, self.c_om), ("qa", HA, self.c_qa),
                         ("ka", HA, self.c_ka), ("gc", CC, self.c_gc), ("uc", CC, self.c_uc)]
        self.o_bfm = {}
        for kind, n, c0 in self.fm_kinds:
            self.o_bfm[kind] = o
            o += n
        self.o_mcw = o; o += 4 * 2 * HM
        self.o_mcb = o; o += 2 * HM
        self.o_cdw = o; o += 31 * CC
        self.o_cdb = o; o += CC
        self.o_clg = o; o += CC
        self.o_clb = o; o += CC
        self.o_ghm = o; o += HM
        self.o_gha = o; o += HA
        self.LPP = o
        self.o_gfin = self.NL * self.LPP
        self.NPP = self.o_gfin + KC
        self.NFB = self.NG + DM + DF


def pack_params(cfg, inp):
    c = cfg
    pp = np.zeros((128, c.NPP), np.float32)

    def fm(vec):
        return np.ascontiguousarray(vec.reshape(-1, 128).T)

    for l in range(c.NL):
        b = l * c.LPP
        pp[:, b + c.o_gmix: b + c.o_gmix + c.KC] = fm(inp["norm_mix"][l])
        pp[:, b + c.o_gffn: b + c.o_gffn + c.KC] = fm(inp["norm_ffn"][l])
        for kind, n, c0 in c.fm_kinds:
            pp[:, b + c.o_bfm[kind]: b + c.o_bfm[kind] + n] = fm(inp["b_in"][l, c0:c0 + n * 128])
        for tap in range(4):
            pp[:, b + c.o_mcw + tap * 2 * c.HM: b + c.o_mcw + (tap + 1) * 2 * c.HM] = fm(inp["mlstm_conv_w"][l, tap])
        pp[:, b + c.o_mcb: b + c.o_mcb + 2 * c.HM] = fm(inp["mlstm_conv_b"][l])
        for tap in range(31):
            pp[:, b + c.o_cdw + tap * c.CC: b + c.o_cdw + (tap + 1) * c.CC] = fm(inp["conv_dw_w"][l, tap])
        pp[:, b + c.o_cdb: b + c.o_cdb + c.CC] = fm(inp["conv_dw_b"][l])
        pp[:, b + c.o_clg: b + c.o_clg + c.CC] = fm(inp["conv_ln_g"][l])
        pp[:, b + c.o_clb: b + c.o_clb + c.CC] = fm(inp["conv_ln_b"][l])
        pp[:, b + c.o_ghm: b + c.o_ghm + c.HM] = fm(inp["mlstm_head_norm"][l].reshape(-1))
        pp[:, b + c.o_gha: b + c.o_gha + c.HA] = fm(inp["fox_head_norm"][l].reshape(-1))
    pp[:, c.o_gfin: c.o_gfin + c.KC] = fm(inp["final_norm"])
    fb = np.zeros((c.NL, 128, c.NFB), np.float32)
    for l in range(c.NL):
        row = np.concatenate([inp["b_in"][l, c.c_im:c.c_im + 2 * c.HM], inp["b_in"][l, c.c_fa:c.c_fa + c.HA],
                              inp["b_in"][l, c.c_vm:c.c_vm + c.DM], inp["b_in"][l, c.c_va:c.c_va + c.DF]])
        fb[l] = np.broadcast_to(row[None, :], (128, c.NFB))
    return pp, fb


class _Stop(Exception):
    pass


INTERLEAVE = True
XQ = "scalar"
import os as _os
CH_W = tuple(int(v) for v in _os.environ.get("CHW", "1,1,2,1").split(","))


def build(cfg, dbg_names=(), stop_at=None):
    c = cfg
    D, S, NL, NSEQ, KC, DM, HM, DF, HA, DC, CC, DFF = c.D, c.S, c.NL, c.NSEQ, c.KC, c.DM, c.HM, c.DF, c.HA, c.DC, c.CC, c.DFF
    HG, NGM, NT, NB, NG, NF, NBO, NBI = c.HG, c.NGM, c.NT, c.NB, c.NG, c.NF, c.NBO, c.NBI
    TT = c.TT
    nc = bass.Bass("TRN2", target_bir_lowering=False)
    x_d = nc.dram_tensor("x", [NSEQ, S, D], F32, kind="ExternalInput").ap()
    w_in_d = nc.dram_tensor("w_in", [NL, D, c.DIN], F32, kind="ExternalInput").ap()
    w_out_d = nc.dram_tensor("w_out", [NL, D, D], F32, kind="ExternalInput").ap()
    w_up_d = nc.dram_tensor("w_up", [NL, D, DFF], F32, kind="ExternalInput").ap()
    w_dn_d = nc.dram_tensor("w_down", [NL, DFF, D], F32, kind="ExternalInput").ap()
    pp_d = nc.dram_tensor("pp", [128, c.NPP], F32, kind="ExternalInput").ap()
    fb_d = nc.dram_tensor("fb", [NL, 128, c.NFB], F32, kind="ExternalInput").ap()
    y_d = nc.dram_tensor("y", [NSEQ, S, D], F32, kind="ExternalOutput").ap()
    xs_d = nc.dram_tensor("xs_scr", [NSEQ * NT, 128, KC, TT], F32, kind="Internal").ap()
    wsc_in = [nc.dram_tensor("wsc_in%d" % l, [NBI, 128, KC, 256], BF16, kind="Internal").ap() for l in range(NL)]
    wsc_out = [nc.dram_tensor("wsc_out%d" % l, [NBO, 128, KC, 256], BF16, kind="Internal").ap() for l in range(NL)]
    wsc_up = [nc.dram_tensor("wsc_up%d" % l, [4 * NBO, 128, KC, 256], BF16, kind="Internal").ap() for l in range(NL)]
    wsc_dn = [nc.dram_tensor("wsc_dn%d" % l, [4 * NBO, 128, KC, 256], BF16, kind="Internal").ap() for l in range(NL)]
    dbg_out = {}

    st = ExitStack()
    p = Prog(nc)

    def sb(name, shape, dt):
        return st.enter_context(nc.sbuf_tensor("s_" + name, shape, dt))

    x_t = sb("x_t", [128, KC, TT], F32)
    h_t = sb("h_t", [128, KC, TT], BF16)
    m_t = sb("m_t", [128, KC, TT], BF16)
    NWB = 3
    wb = [sb("wb%d" % i, [128, KC, 256], BF16) for i in range(NWB)]
    wg = sb("wg", [128, NL, KC, NG], BF16)
    stg_io = [sb("stg_io%d" % i, [128, 512], F32) for i in range(2)]
    rs = sb("rs", [128, TT], F32)
    sqb = [sb("sqb%d" % i, [128, TT], BF16) for i in range(2)]
    pp = sb("pp", [128, c.NPP], F32)
    fbl = sb("fbl", [128, c.NFB], F32)
    identb = sb("identb", [128, 128], BF16)
    identf = sb("identf", [128, 128], F32)
    ones_b = sb("ones_b", [128, 128], BF16)
    ones_f = sb("ones_f", [128, 128], F32)
    Utri = sb("Utri", [128, 128], F32)
    sel2 = [sb("sel2_%d" % h, [2 * HA, 128], BF16) for h in range(HA)]
    qT = sb("qT", [128, HG, TT], BF16)
    kT = sb("kT", [128, HG, TT], BF16)
    sgo = sb("sgo", [128, HG, TT], BF16)
    vx = sb("vx", [128, NB, HM, 129], BF16)
    Cf = sb("Cf", [128, HM, 129], F32)
    Cb = sb("Cb", [128, HM, 129], BF16)
    cstg = sb("cstg", [128, TT + 3], F32)
    cacc = sb("cacc", [128, TT], F32)
    chist = sb("chist", [128, 2 * HM, 3], F32)
    Gt = sb("Gt", [128, NB, NG], F32)
    Et = sb("Et", [128, NF], F32)
    Lt = sb("Lt", [128, NB, NF], F32)
    gtmp = sb("gtmp", [128, HM], F32)
    gcs = sb("gcs", [128, 128], F32)
    aa = sb("aa", [128, NB, HM], F32)
    einv = sb("einv", [128, NB, HM], F32)
    ebl = sb("ebl", [128, NB, HM], F32)
    carry = sb("carry", [128, HA], F32)
    nF2 = sb("nF2", [128, 2 * HA], BF16)
    qTa = sb("qTa", [128, HA, TT], BF16)
    KT = sb("KT", [128, HA, S], BF16)
    Vext = sb("Vext", [128, c.NBLK, HA, 129], BF16)
    negF2 = sb("negF2", [2 * HA, S], BF16)
    y0 = sb("y0", [128, CC, TT + 30], BF16)
    sg = sb("sg", [128, 2, TT], F32)
    rl = [sb("rl%d" % i, [128, TT], F32) for i in range(2)]
    dn = sb("dn", [128, HG], F32)
    rr = sb("rr", [128, HG], F32)
    dr = sb("dr", [128, HG], F32)
    ss = sb("ss", [128, HG], F32)

    assert CC <= 4 and HG * 128 <= TT
    ktok = sb("ktok", [128, HG * 128], BF16)
    STm = sb("STm", [128, HG * 128], BF16)
    ytok = sb("ytok", [128, HG * 128], BF16)
    sm = {"ktok": ktok[:], "STm": STm[:], "ytok": ytok[:], "hf": cstg[:, 0:HG * 128], "sqf": cacc[:, 0:HG * 128]}
    PTc = sb("PTc", [128, 1024], BF16)
    cstgb = sb("cstgb", [128, TT + 3], BF16)
    dgM = [sb("dgM%d" % i, [128, 128], BF16) for i in range(2)]
    dgC = [sb("dgC%d" % i, [128, 128], BF16) for i in range(4)]
    yas = [sb("ya%d" % i, [128, 128], BF16) for i in range(2)]
    of1 = sb("of1", [128, 128], F32)
    rr1s = [sb("rr1_%d" % i, [128, 1], F32) for i in range(2)]
    ss1s = [sb("ss1_%d" % i, [128, 1], F32) for i in range(2)]
    nFtok = sb("nFtok", [128, c.NBLK, HA], F32)
    maskT = sb("maskT", [128, 128], BF16)
    ofs = [gcs[:, 0:128], of1[:]]

    ps = [st.enter_context(nc.psum_tensor("ps%d" % i, [128, 512], F32)) for i in range(8)]
    psb = [t[:].bitcast(BF16) for t in ps]

    R_x = [Res("x%d" % k) for k in range(KC)]
    R_h = [Res("h%d" % k) for k in range(KC)]
    R_m = [Res("m%d" % k) for k in range(KC)]
    R_wb = [Res("wb%d" % i) for i in range(NWB)]
    R_ps = [Res("ps%d" % i, excl=True) for i in range(8)]
    R_io = [Res("io0"), Res("io1")]
    R_sqb = [Res("sqb0"), Res("sqb1")]
    R_rl = [Res("rl0"), Res("rl1")]
    R = {k: Res(k) for k in ["wg", "rs", "pp", "fbl", "const", "qT", "kT", "sgo", "vx", "Cf", "Cb", "cstg", "cacc", "chist",
                             "Gt", "Et", "Lt", "gtmp", "gcs", "aa", "einv", "ebl", "nFt", "carry", "nF2", "qTa", "KT", "Vext", "negF2",
                             "y0", "sg", "dn", "rr", "dr", "ss", "mx", "negm", "rr1", "ss1",
                             "ktok", "STm", "hf", "sqf", "ytok", "P", "PT", "of", "sq1", "ya", "cv", "cmean", "cm2", "csq"]}
    R["hf"] = R["cstg"]
    R["sqf"] = R["cacc"]
    R["of"] = R["gcs"]
    for i_ in range(2):
        for nm in ("PTc%d", "rr1_%d", "ss1_%d", "ya_%d"):
            R[nm % i_] = Res(nm % i_)
    R_ofs = [R["gcs"], Res("of1")]
    R["cstgb"] = Res("cstgb")
    R_dgM = [Res("dgM%d" % i) for i in range(2)]
    R_dgC = [Res("dgC%d" % i) for i in range(4)]
    dg_ctr = {"M": 0, "C": 0}
    R_cv = [R_rl[0], R_rl[1], R_io[0], R_io[1]]
    cvt = [rl[0], rl[1], stg_io[0], stg_io[1]]
    stgs = [(stg_io[0][:], R_io[0]), (stg_io[1][:], R_io[1]), (rl[0][:], R_rl[0]), (rl[1][:], R_rl[1]), (cstg[:, 0:512], R["cstg"]), (cacc[:, 0:512], R["cacc"])]
    NSTG = len(stgs)

    R_xs = [[Res("xs%d_%d" % (i, k)) for k in range(KC)] for i in range(NSEQ * NT)]
    R_win = [Res("win%d" % l) for l in range(NL)]
    R_wout = [Res("wout%d" % l) for l in range(NL)]
    R_wup = [Res("wup%d" % l) for l in range(NL)]
    R_wdn = [Res("wdn%d" % l) for l in range(NL)]

    V = lambda fn, r=(), w=(): p.op("vector", fn, r, w)
    A = lambda fn, r=(), w=(): p.op("scalar", fn, r, w)
    T = lambda fn, r=(), w=(): p.op("tensor", fn, r, w)
    G = lambda fn, r=(), w=(): p.op("gpsimd", fn, r, w)

    POOLS = {"all": list(range(8)), "F0": [0, 0, 1], "F1": [2, 2, 3], "M": [4, 5], "C": [6, 7], "G": [0, 1], "P": [2, 3, 4, 5, 6, 7], "S": [0, 1, 2, 3, 4, 5, 6]}
    bank_ctr = {"all": 0, "M": 0, "C": 0, "G": 0, "P": 0, "S": 0}
    cur_pool = ["all"]

    def set_pool(name):
        cur_pool[0] = name

    def nb():
        pn = cur_pool[0]
        pool = POOLS[pn]
        b = pool[bank_ctr[pn] % len(pool)]
        bank_ctr[pn] += 1
        return b

    def dbg(name, ap, shape, res):
        if name not in dbg_names:
            return
        key = name
        i = 0
        while key in dbg_out:
            i += 1
            key = "%s_%d" % (name, i)
        d = nc.dram_tensor("dbg_" + key, shape, ap.dtype, kind="ExternalOutput").ap()
        dbg_out[key] = d
        p.dma("sync", lambda e, s: e.dma_start(out=d, in_=ap).then_inc(s, 16), reads=res, is_output=True)

    Rc = [R["const"]]
    G(lambda e: e.memset(identb[:], 0.0), w=Rc)
    G(lambda e: e.affine_select(out=identb[:], in_=identb[:], pattern=[[-1, 128]], compare_op=ALU.not_equal, fill=1.0, base=0, channel_multiplier=1), r=Rc, w=Rc)
    G(lambda e: e.memset(identf[:], 0.0), w=Rc)
    G(lambda e: e.affine_select(out=identf[:], in_=identf[:], pattern=[[-1, 128]], compare_op=ALU.not_equal, fill=1.0, base=0, channel_multiplier=1), r=Rc, w=Rc)
    G(lambda e: e.memset(ones_b[:], 1.0), w=Rc)
    G(lambda e: e.memset(ones_f[:], 1.0), w=Rc)
    G(lambda e: e.memset(Utri[:], 1.0), w=Rc)
    G(lambda e: e.affine_select(out=Utri[:], in_=Utri[:], pattern=[[1, 128]], compare_op=ALU.is_ge, fill=0.0, base=0, channel_multiplier=-1), r=Rc, w=Rc)
    G(lambda e: e.memset(maskT[:], 0.0), w=Rc)
    G(lambda e: e.affine_select(out=maskT[:], in_=maskT[:], pattern=[[1, 128]], compare_op=ALU.is_ge, fill=-30000.0, base=0, channel_multiplier=-1), r=Rc, w=Rc)
    for h in range(HA):
        G(lambda e, h=h: e.memset(sel2[h][:], 0.0), w=Rc)
        G(lambda e, h=h: e.affine_select(out=sel2[h][:], in_=sel2[h][:], pattern=[[0, 128]], compare_op=ALU.not_equal, fill=-1.0, base=-h, channel_multiplier=1), r=Rc, w=Rc)
        G(lambda e, h=h: e.affine_select(out=sel2[h][:], in_=sel2[h][:], pattern=[[0, 128]], compare_op=ALU.not_equal, fill=-1.0, base=-(HA + h), channel_multiplier=1), r=Rc, w=Rc)
    G(lambda e: e.memset(Vext[:, :, :, 128:129], 1.0), w=[R["Vext"]])
    p.dma("sync", lambda e, s: e.dma_start(out=pp[:], in_=pp_d[:, :]).then_inc(s, 16), writes=[R["pp"]])

    def load_wg(e, s):
        for l in range(NL):
            src1 = w_in_d[l, :, c.c_im:c.c_im + 2 * HM].rearrange("(k p) c -> p k c", p=128)
            e.dma_start(out=wg[:, l, :, 0:2 * HM], in_=src1).then_inc(s, 16)
            src2 = w_in_d[l, :, c.c_fa:c.c_fa + HA].rearrange("(k p) c -> p k c", p=128)
            e.dma_start(out=wg[:, l, :, 2 * HM:NG], in_=src2).then_inc(s, 16)
    p.dma("gpsimd", load_wg, writes=[R["wg"]], ndma=2 * NL)

    in_runs = []
    i = 0
    while i < NBI:
        j = i
        while j + 1 < NBI and c.in_blocks[j + 1][2] == c.in_blocks[j][2] + 256:
            j += 1
        in_runs.append((c.in_blocks[i][2], j - i + 1, i))
        i = j + 1

    def mk_cast_in(l):
        def f(e, s):
            for kc in range(KC):
                for (c0, n, b0) in in_runs:
                    src = w_in_d[l, kc * 128:(kc + 1) * 128, c0:c0 + n * 256].rearrange("p (b c) -> p b c", c=256)
                    dst = wsc_in[l][b0:b0 + n, :, kc, :].rearrange("b p c -> p b c")
                    e.dma_start(out=dst, in_=src).then_inc(s, 16)
        return f, KC * len(in_runs)

    def mk_cast_sq(l, srcd, dstd, ncols):
        def f(e, s):
            for kc in range(KC):
                src = srcd[l, kc * 128:(kc + 1) * 128, 0:ncols].rearrange("p (b c) -> p b c", c=256)
                dst = dstd[l][0:ncols // 256, :, kc, :].rearrange("b p c -> p b c")
                e.dma_start(out=dst, in_=src).then_inc(s, 16)
        return f, KC

    def mk_cast_up(l):
        return mk_cast_sq(l, w_up_d, wsc_up, DFF)

    def mk_cast_dn(l):
        def f(e, s):
            for rg in range(DFF // 128):
                qd, kcl = rg // KC, rg % KC
                src = w_dn_d[l, rg * 128:(rg + 1) * 128, :].rearrange("p (b c) -> p b c", c=256)
                dst = wsc_dn[l][qd * NBO:(qd + 1) * NBO, :, kcl, :].rearrange("b p c -> p b c")
                e.dma_start(out=dst, in_=src).then_inc(s, 16)
        return f, DFF // 128

    def cast_ops(l):
        return [(mk_cast_in(l), R_win[l]), (mk_cast_sq(l, w_out_d, wsc_out, D), R_wout[l]), (mk_cast_up(l), R_wup[l]), (mk_cast_dn(l), R_wdn[l])]

    for (f, n), res in cast_ops(0):
        p.dma("gpsimd", f, writes=[res], ndma=n)

    wb_ctr = [0]

    def load_w(src_ap, res):
        slot = wb_ctr[0] % NWB
        wb_ctr[0] += 1
        p.dma("sync", lambda e, s: e.dma_start(out=wb[slot][:], in_=src_ap).then_inc(s, 16), reads=[res], writes=[R_wb[slot]])
        return slot

    def rmsnorm_h(l, goff, inplace_f32=False):
        b = nb()
        last = None
        for kc in range(KC):
            sq = sqb[kc % 2]
            A(lambda e, kc=kc, sq=sq: e.activation(out=sq[:], in_=x_t[:, kc, :], func=AF.Square), r=[R_x[kc]], w=[R_sqb[kc % 2]])
            T(lambda e, kc=kc, sq=sq: e.matmul(ps[b][:, :], lhsT=ones_b[:], rhs=sq[:], start=(kc == 0), stop=(kc == KC - 1)),
              r=[R_sqb[kc % 2], R["const"]], w=[R_ps[b]])
        V(lambda e: e.tensor_scalar(out=rs[:], in0=ps[b][:, :], scalar1=1.0 / D, scalar2=EPS, op0=ALU.mult, op1=ALU.add), r=[R_ps[b]], w=[R["rs"]])
        A(lambda e: e.activation(out=rs[:], in_=rs[:], func=AF.Sqrt), r=[R["rs"]], w=[R["rs"]])
        V(lambda e: e.reciprocal(out=rs[:], in_=rs[:]), r=[R["rs"]], w=[R["rs"]])
        for kc in range(KC):
            gc_ = pp[:, goff + kc: goff + kc + 1]
            if inplace_f32:
                V(lambda e, kc=kc, gc_=gc_: e.scalar_tensor_tensor(out=x_t[:, kc, :], in0=x_t[:, kc, :], scalar=gc_, in1=rs[:], op0=ALU.mult, op1=ALU.mult),
                  r=[R_x[kc], R["rs"], R["pp"]], w=[R_x[kc]])
            else:
                last = V(lambda e, kc=kc, gc_=gc_: e.scalar_tensor_tensor(out=h_t[:, kc, :], in0=x_t[:, kc, :], scalar=gc_, in1=rs[:], op0=ALU.mult, op1=ALU.mult),
                         r=[R_x[kc], R["rs"], R["pp"]], w=[R_h[kc]])
        return last

    def fm_group(slot, cb, rhs_t, R_rhs, b, split=1):
        per = (KC + split - 1) // split
        for k0 in range(0, KC, per):
            def f(e, k0=k0):
                for kc in range(k0, min(KC, k0 + per)):
                    ins = e.matmul(ps[b][:, :], lhsT=wb[slot][:, kc, cb * 128:(cb + 1) * 128], rhs=rhs_t[:, kc, :], start=(kc == 0), stop=(kc == KC - 1))
                return ins
            T(f, r=[R_wb[slot]] + R_rhs[k0:k0 + per], w=[R_ps[b]])

    def ckpt(name):
        if stop_at == name:
            raise _Stop()

    try:
      ckpt('setup')
      for s in range(NSEQ):
          for l in range(NL):
              lp = l * c.LPP
              p.dma("sync", lambda e, sm_, l=l: e.dma_start(out=fbl[:], in_=fb_d[l]).then_inc(sm_, 16), writes=[R["fbl"]])
              V(lambda e: e.memset(Cf[:], 0.0), w=[R["Cf"]])
              V(lambda e: e.memset(Cb[:], 0.0), w=[R["Cb"]])
              V(lambda e: e.memset(carry[:], 0.0), w=[R["carry"]])
              V(lambda e: e.memset(chist[:], 0.0), w=[R["chist"]])
              V(lambda e: e.memset(y0[:, :, 0:30], 0.0), w=[R["y0"]])
              for t in range(NT):
                  xi = s * NT + t
                  tok0 = t * TT
                  if l == 0:
                      k = 0
                      for bi in range(NB):
                          for dq in range(D // 512):
                              sa, sr = stgs[k % NSTG]
                              k += 1
                              src = x_d[s, tok0 + bi * 128: tok0 + (bi + 1) * 128, dq * 512:(dq + 1) * 512]
                              p.dma("sync", lambda e, sm_, sa=sa, src=src: e.dma_start(out=sa, in_=src).then_inc(sm_, 16), writes=[sr])
                              b = nb()

                              def ftr(e, sa=sa, b=b):
                                  for i4 in range(4):
                                      ins = e.transpose(out=ps[b][:, i4 * 128:(i4 + 1) * 128], in_=sa[:, i4 * 128:(i4 + 1) * 128], identity=identf[:])
                                  return ins
                              T(ftr, r=[sr, R["const"]], w=[R_ps[b]])
                              V(lambda e, b=b, dq=dq, bi=bi: e.tensor_copy(out=x_t[:, dq * 4:(dq + 1) * 4, bi * 128:(bi + 1) * 128],
                                                                       in_=ps[b][:, :].rearrange("p (k t) -> p k t", k=4)),
                                r=[R_ps[b]], w=R_x[dq * 4:(dq + 1) * 4])
                  else:
                      for kc in range(KC):
                          p.dma(XQ, lambda e, sm_, xi=xi, kc=kc: e.dma_start(out=x_t[:, kc, :], in_=xs_d[xi][:, kc, :]).then_inc(sm_, 16), reads=[R_xs[xi][kc]], writes=[R_x[kc]])

                  ckpt('xload')
                  rmsnorm_h(l, lp + c.o_gmix)

                  ckpt('normA')
                  def gates_chain():
                      set_pool("G")
                      for bi in range(NB):
                          gblk = t * NB + bi
                          b = nb()

                          def fg(e, bi=bi, b=b):
                              for kc in range(KC):
                                  ins = e.matmul(ps[b][:, 0:NG], lhsT=h_t[:, kc, bi * 128:(bi + 1) * 128], rhs=wg[:, l, kc, :], start=(kc == 0), stop=(kc == KC - 1))
                              return ins
                          T(fg, r=R_h + [R["wg"]], w=[R_ps[b]])
                          V(lambda e, bi=bi, b=b: e.tensor_tensor(out=Gt[:, bi, :], in0=ps[b][:, 0:NG], in1=fbl[:, 0:NG], op=ALU.add), r=[R_ps[b], R["fbl"]], w=[R["Gt"]])
                          A(lambda e, bi=bi: e.activation(out=Et[:], in_=Gt[:, bi, HM:NG], func=AF.Exp, scale=-1.0), r=[R["Gt"]], w=[R["Et"]])
                          A(lambda e, bi=bi: e.activation(out=Lt[:, bi, :], in_=Et[:], func=AF.Ln, bias=1.0), r=[R["Et"]], w=[R["Lt"]])
                          b2 = nb()

                          def fcs(e, bi=bi, b2=b2):
                              e.matmul(ps[b2][:, 0:NF], lhsT=Utri[:], rhs=Lt[:, bi, :], start=True, stop=True)
                              return e.matmul(ps[b2][:, 64:64 + NF], lhsT=ones_f[:], rhs=Lt[:, bi, :], start=True, stop=True)
                          T(fcs, r=[R["Lt"], R["const"]], w=[R_ps[b2]])
                          V(lambda e, b2=b2: e.tensor_copy(out=gcs[:], in_=ps[b2][:, 0:128]), r=[R_ps[b2]], w=[R["gcs"]])
                          lnc = math.log(128.0 ** -0.5)
                          V(lambda e, bi=bi, b2=b2: e.scalar_tensor_tensor(out=gtmp[:], in0=gcs[:, 0:HM], scalar=lnc, in1=Gt[:, bi, 0:HM], op0=ALU.add, op1=ALU.add),
                            r=[R["gcs"], R["Gt"]], w=[R["gtmp"]])
                          A(lambda e, bi=bi: e.activation(out=aa[:, bi, :], in_=gtmp[:], func=AF.Exp), r=[R["gtmp"]], w=[R["aa"]])
                          A(lambda e, bi=bi, b2=b2: e.activation(out=einv[:, bi, :], in_=gcs[:, 0:HM], func=AF.Exp), r=[R["gcs"]], w=[R["einv"]])
                          A(lambda e, bi=bi, b2=b2: e.activation(out=ebl[:, bi, :], in_=gcs[:, 64:64 + HM], func=AF.Exp, scale=-1.0), r=[R["gcs"]], w=[R["ebl"]])
                          V(lambda e, b2=b2, gblk=gblk: e.tensor_tensor(out=nFtok[:, gblk, :], in0=gcs[:, HM:NF], in1=carry[:], op=ALU.add), r=[R["gcs"], R["carry"]], w=[R["nFt"]])
                          V(lambda e, b2=b2: e.tensor_tensor(out=carry[:], in0=gcs[:, 64 + HM:64 + NF], in1=carry[:], op=ALU.add), r=[R["gcs"], R["carry"]], w=[R["carry"]])
                          V(lambda e, gblk=gblk: e.tensor_copy(out=nF2[:, 0:HA], in_=nFtok[:, gblk, :]), r=[R["nFt"]], w=[R["nF2"]])
                          V(lambda e, gblk=gblk: e.tensor_tensor(out=nF2[:, HA:2 * HA], in0=nFtok[:, gblk, :], in1=nF2[:, 0:HA], op=ALU.subtract), r=[R["nFt"], R["nF2"]], w=[R["nF2"]])
                          b3 = nb()
                          T(lambda e, b3=b3: e.transpose(out=psb[b3][0:2 * HA, 0:128], in_=nF2[:, :], identity=identb[:]), r=[R["nF2"], R["const"]], w=[R_ps[b3]])
                          A(lambda e, b3=b3, gblk=gblk: e.activation(out=negF2[:, gblk * 128:(gblk + 1) * 128], in_=psb[b3][0:2 * HA, 0:128], func=AF.Copy),
                            r=[R_ps[b3]], w=[R["negF2"]])
                          V(lambda e, bi=bi: e.tensor_copy(out=vx[:, bi, :, 128:129], in_=aa[:, bi, :].unsqueeze(2)), r=[R["aa"]], w=[R["vx"]])

                  ckpt('gates')
                  def do_tm(kind, idx, slot):
                      for bi in range(NB):
                          b = nb()

                          def f(e, bi=bi, b=b):
                              for kc in range(KC):
                                  ins = e.matmul(ps[b][:, 0:256], lhsT=h_t[:, kc, bi * 128:(bi + 1) * 128], rhs=wb[slot][:, kc, :], start=(kc == 0), stop=(kc == KC - 1))
                              return ins
                          T(f, r=R_h + [R_wb[slot]], w=[R_ps[b]])
                          h0 = 2 * idx
                          pv = ps[b][:, 0:256].rearrange("p (h e) -> p h e", h=2)
                          if kind == "vm":
                              V(lambda e, bi=bi, pv=pv, h0=h0: e.tensor_tensor(out=vx[:, bi, h0:h0 + 2, 0:128], in0=pv,
                                                                            in1=aa[:, bi, h0:h0 + 2].unsqueeze(2).broadcast_to([128, 2, 128]), op=ALU.mult),
                                r=[R_ps[b], R["aa"]], w=[R["vx"]])
                          else:
                              gblk = t * NB + bi
                              A(lambda e, gblk=gblk, pv=pv, h0=h0: e.activation(out=Vext[:, gblk, h0:h0 + 2, 0:128], in_=pv, func=AF.Copy),
                                r=[R_ps[b]], w=[R["Vext"]])

                  def conv4_evac(b, cj, dest_ap, boff, R_dest):
                      A(lambda e: e.activation(out=cstgb[:, 3:3 + TT], in_=ps[b][:, :], func=AF.Identity, bias=pp[:, boff:boff + 1]),
                        r=[R_ps[b], R["pp"]], w=[R["cstgb"]])
                      V(lambda e: e.tensor_copy(out=cstgb[:, 0:3], in_=chist[:, cj, :]), r=[R["chist"]], w=[R["cstgb"]])
                      V(lambda e: e.tensor_copy(out=chist[:, cj, :], in_=cstgb[:, TT:TT + 3]), r=[R["cstgb"]], w=[R["chist"]])
                      wo = lp + c.o_mcw
                      bo = lp + c.o_mcb + cj
                      b2 = nb()
                      for tap in range(4):
                          k = dg_ctr["M"] % 2
                          dg_ctr["M"] += 1
                          wc = wo + tap * 2 * HM + cj
                          V(lambda e, k=k, wc=wc: e.tensor_scalar(out=dgM[k][:], in0=identb[:], scalar1=pp[:, wc:wc + 1], scalar2=None, op0=ALU.mult),
                            r=[R["const"], R["pp"]], w=[R_dgM[k]])
                          T(lambda e, k=k, tap=tap: e.matmul(ps[b2][:, :], lhsT=dgM[k][:], rhs=cstgb[:, tap:tap + TT], start=(tap == 0), stop=(tap == 3)),
                            r=[R_dgM[k], R["cstgb"]], w=[R_ps[b2]])
                      A(lambda e: e.activation(out=dest_ap, in_=ps[b2][:, :], func=AF.Silu, bias=pp[:, bo:bo + 1]), r=[R_ps[b2], R["pp"]], w=[R_dest])

                  def do_fm(kind, idx, slot, g, split=1):
                      for cb in range(2):
                          j = idx * 2 + cb
                          b = nb()
                          fm_group(slot, cb, h_t, R_h, b, split)
                          boff = lp + c.o_bfm[kind] + j
                          bias = pp[:, boff:boff + 1]
                          if kind in ("qm", "km"):
                              hl = j - g * HG
                              dest = (qT if kind == "qm" else kT)
                              conv4_evac(b, (0 if kind == "qm" else HM) + j, dest[:, hl, :], boff, R["qT"] if kind == "qm" else R["kT"])
                          elif kind == "om":
                              hl = j - g * HG
                              A(lambda e, hl=hl, b=b, bias=bias: e.activation(out=sgo[:, hl, :], in_=ps[b][:, :], func=AF.Sigmoid, bias=bias),
                                r=[R_ps[b], R["pp"]], w=[R["sgo"]])
                          elif kind == "qa":
                              V(lambda e, j=j, b=b, bias=bias: e.tensor_scalar(out=qTa[:, j, :], in0=ps[b][:, :], scalar1=bias, scalar2=128.0 ** -0.5, op0=ALU.add, op1=ALU.mult),
                                r=[R_ps[b], R["pp"]], w=[R["qTa"]])
                          elif kind == "ka":
                              A(lambda e, j=j, b=b, bias=bias: e.activation(out=KT[:, j, tok0:tok0 + TT], in_=ps[b][:, :], func=AF.Identity, bias=bias),
                                r=[R_ps[b], R["pp"]], w=[R["KT"]])
                          elif kind == "gc":
                              A(lambda e, cb=cb, b=b, bias=bias: e.activation(out=sg[:, cb, :], in_=ps[b][:, :], func=AF.Sigmoid, bias=bias),
                                r=[R_ps[b], R["pp"]], w=[R["sg"]])
                          elif kind == "uc":
                              V(lambda e, j=j, cb=cb, b=b, bias=bias: e.scalar_tensor_tensor(out=y0[:, j, 30:30 + TT], in0=ps[b][:, :], scalar=bias, in1=sg[:, cb, :],
                                                                                           op0=ALU.add, op1=ALU.mult), r=[R_ps[b], R["pp"], R["sg"]], w=[R["y0"]])

                  def nd_slot(hl):
                      return hl // 3, (hl % 3) * 129

                  def mlstm_group(g):
                      for bi in range(NB):
                          c0 = bi * 128
                          hs = g * HG
                          bS = nb()

                          def fS(e, bS=bS, c0=c0):
                              for hl in range(HG):
                                  ins = e.matmul(ps[bS][:, hl * 128:(hl + 1) * 128], lhsT=kT[:, hl, c0:c0 + 128], rhs=qT[:, hl, c0:c0 + 128], start=True, stop=True)
                              return ins
                          T(fS, r=[R["qT"], R["kT"]], w=[R_ps[bS]])
                          V(lambda e, bS=bS: e.tensor_tensor(out=sm["STm"].rearrange("p (h l) -> p h l", h=HG), in0=ps[bS][:, 0:HG * 128].rearrange("p (h l) -> p h l", h=HG),
                                                           in1=Utri[:].unsqueeze(1).broadcast_to([128, HG, 128]), op=ALU.mult), r=[R_ps[bS], R["const"]], w=[R["STm"]])
                          bK = nb()

                          def fK(e, bK=bK, c0=c0):
                              for hl in range(HG):
                                  ins = e.transpose(out=psb[bK][:, hl * 128:(hl + 1) * 128], in_=kT[:, hl, c0:c0 + 128], identity=identb[:])
                              return ins
                          T(fK, r=[R["kT"], R["const"]], w=[R_ps[bK]])
                          A(lambda e, bK=bK: e.activation(out=sm["ktok"], in_=psb[bK][:, 0:HG * 128], func=AF.Copy), r=[R_ps[bK]], w=[R["ktok"]])
                          nbk = (HG + 2) // 3
                          bN = [nb() for _ in range(nbk)]

                          def fN(e, bN=bN, c0=c0, bi=bi):
                              for hl in range(HG):
                                  bk, off = nd_slot(hl)
                                  o = ps[bN[bk]][:, off:off + 129]
                                  e.matmul(o, lhsT=qT[:, hl, c0:c0 + 128], rhs=Cb[:, hs + hl, :], start=True, stop=False)
                                  ins = e.matmul(o, lhsT=sm["STm"][:, hl * 128:(hl + 1) * 128], rhs=vx[:, bi, hs + hl, :], start=False, stop=True)
                              return ins
                          T(fN, r=[R["qT"], R["Cb"], R["STm"], R["vx"]], w=[R_ps[x] for x in bN])
                          for bk in range(nbk):
                              h0 = bk * 3
                              n = min(3, HG - h0)
                              pv = ps[bN[bk]][:, 0:n * 129].rearrange("p (h e) -> p h e", h=n)
                              V(lambda e, pv=pv, h0=h0, n=n: e.tensor_reduce(out=dn[:, h0:h0 + n], in_=pv[:, :, 128:129], axis=AX.X, op=ALU.max, apply_absolute_value=True),
                                r=[R_ps[bN[bk]]], w=[R["dn"]])
                          V(lambda e, bi=bi: e.tensor_tensor(out=dn[:], in0=dn[:], in1=einv[:, bi, hs:hs + HG], op=ALU.max), r=[R["dn"], R["einv"]], w=[R["dn"]])
                          V(lambda e: e.reciprocal(out=rr[:], in_=dn[:]), r=[R["dn"]], w=[R["rr"]])
                          for bk in range(nbk):
                              h0 = bk * 3
                              n = min(3, HG - h0)
                              pv = ps[bN[bk]][:, 0:n * 129].rearrange("p (h e) -> p h e", h=n)
                              V(lambda e, pv=pv, h0=h0, n=n: e.tensor_tensor(out=dr[:, h0:h0 + n], in0=pv[:, :, 128], in1=rr[:, h0:h0 + n], op=ALU.mult),
                                r=[R_ps[bN[bk]], R["rr"]], w=[R["dr"]])
                              V(lambda e, pv=pv, h0=h0, n=n: e.tensor_tensor(out=sm["hf"][:, h0 * 128:(h0 + n) * 128].rearrange("p (h e) -> p h e", h=n), in0=pv[:, :, 0:128],
                                                                           in1=rr[:, h0:h0 + n].unsqueeze(2).broadcast_to([128, n, 128]), op=ALU.mult),
                                r=[R_ps[bN[bk]], R["rr"]], w=[R["hf"]])
                          for hl in range(HG):
                              hh = hs + hl
                              V(lambda e, hl=hl, hh=hh: e.scalar_tensor_tensor(out=sm["hf"][:, hl * 128:(hl + 1) * 128], in0=fbl[:, NG + hh * 128: NG + (hh + 1) * 128],
                                                                             scalar=dr[:, hl:hl + 1], in1=sm["hf"][:, hl * 128:(hl + 1) * 128], op0=ALU.mult, op1=ALU.add),
                                r=[R["fbl"], R["dr"], R["hf"]], w=[R["hf"]])
                          V(lambda e: e.tensor_tensor(out=sm["sqf"], in0=sm["hf"], in1=sm["hf"], op=ALU.mult), r=[R["hf"]], w=[R["sqf"]])
                          V(lambda e: e.tensor_reduce(out=ss[:], in_=sm["sqf"].rearrange("p (h e) -> p h e", h=HG), axis=AX.X, op=ALU.add), r=[R["sqf"]], w=[R["ss"]])
                          V(lambda e: e.tensor_scalar(out=ss[:], in0=ss[:], scalar1=1.0 / 128, scalar2=EPS, op0=ALU.mult, op1=ALU.add), r=[R["ss"]], w=[R["ss"]])
                          A(lambda e: e.activation(out=ss[:], in_=ss[:], func=AF.Sqrt), r=[R["ss"]], w=[R["ss"]])
                          V(lambda e: e.reciprocal(out=ss[:], in_=ss[:]), r=[R["ss"]], w=[R["ss"]])
                          V(lambda e: e.tensor_tensor(out=sm["ytok"].rearrange("p (h e) -> p h e", h=HG), in0=sm["hf"].rearrange("p (h e) -> p h e", h=HG),
                                                      in1=ss[:].unsqueeze(2).broadcast_to([128, HG, 128]), op=ALU.mult), r=[R["hf"], R["ss"]], w=[R["ytok"]])
                          bY = nb()

                          def fY(e, bY=bY):
                              for hl in range(HG):
                                  ins = e.transpose(out=psb[bY][:, hl * 128:(hl + 1) * 128], in_=sm["ytok"][:, hl * 128:(hl + 1) * 128], identity=identb[:])
                              return ins
                          T(fY, r=[R["ytok"], R["const"]], w=[R_ps[bY]])
                          for hl in range(HG):
                              hh = hs + hl
                              go = lp + c.o_ghm + hh
                              V(lambda e, hl=hl, hh=hh, go=go, bY=bY, c0=c0: e.scalar_tensor_tensor(out=m_t[:, hh, c0:c0 + 128], in0=psb[bY][:, hl * 128:(hl + 1) * 128],
                                                                                                  scalar=pp[:, go:go + 1], in1=sgo[:, hl, c0:c0 + 128], op0=ALU.mult, op1=ALU.mult),
                                r=[R_ps[bY], R["pp"], R["sgo"]], w=[R_m[hh]])
                          bD = [nb() for _ in range(nbk)]

                          def fD(e, bD=bD, bi=bi):
                              for hl in range(HG):
                                  bk, off = nd_slot(hl)
                                  ins = e.matmul(ps[bD[bk]][:, off:off + 129], lhsT=sm["ktok"][:, hl * 128:(hl + 1) * 128], rhs=vx[:, bi, hs + hl, :], start=True, stop=True)
                              return ins
                          T(fD, r=[R["ktok"], R["vx"]], w=[R_ps[x] for x in bD])
                          for bk in range(nbk):
                              h0 = bk * 3
                              n = min(3, HG - h0)
                              pv = ps[bD[bk]][:, 0:n * 129].rearrange("p (h e) -> p h e", h=n)
                              V(lambda e, pv=pv, h0=h0, n=n: e.tensor_tensor(out=Cf[:, hs + h0:hs + h0 + n, :], in0=pv, in1=Cf[:, hs + h0:hs + h0 + n, :], op=ALU.add),
                                r=[R_ps[bD[bk]], R["Cf"]], w=[R["Cf"]])
                          V(lambda e, bi=bi: e.tensor_tensor(out=Cf[:, hs:hs + HG, :], in0=Cf[:, hs:hs + HG, :],
                                                           in1=ebl[:, bi, hs:hs + HG].unsqueeze(2).broadcast_to([128, HG, 129]), op=ALU.mult), r=[R["Cf"], R["ebl"]], w=[R["Cf"]])
                          A(lambda e: e.activation(out=Cb[:, hs:hs + HG, :], in_=Cf[:, hs:hs + HG, :], func=AF.Copy), r=[R["Cf"]], w=[R["Cb"]])

                  def fox_stream(st_):
                      SB = POOLS["F%d" % st_][0:2]
                      FO = POOLS["F%d" % st_][2]
                      halves = [(sqb[st_], R_sqb[st_]), (PTc[:, st_ * 512:(st_ + 1) * 512], R["PTc%d" % st_])]
                      rr1_, ss1_, of_, ya_ = rr1s[st_], ss1s[st_], ofs[st_], yas[st_]
                      R_rr1, R_ss1, R_of, R_ya = R["rr1_%d" % st_], R["ss1_%d" % st_], R_ofs[st_], R["ya_%d" % st_]
                      it = -1
                      for bi in range(NB):
                          gi = t * NB + bi
                          nk = gi + 1
                          ng = (nk + 3) // 4
                          for ha in range(HA):
                              it += 1
                              if it % 2 != st_:
                                  continue

                              def slot(kb):
                                  hv = halves[(kb // 4) % 2][0]
                                  return hv[:, (kb % 4) * 128:(kb % 4 + 1) * 128]

                              def scores(g, bi=bi, ha=ha, gi=gi, nk=nk):
                                  bank = SB[g % 2]

                                  def f(e):
                                      for kb in range(g * 4, min(nk, g * 4 + 4)):
                                          o = ps[bank][:, (kb % 4) * 128:(kb % 4 + 1) * 128]
                                          e.matmul(o, lhsT=KT[:, ha, kb * 128:(kb + 1) * 128], rhs=qTa[:, ha, bi * 128:(bi + 1) * 128], start=True, stop=False)
                                          last = kb == gi
                                          ins = e.matmul(o, lhsT=sel2[ha][:], rhs=negF2[:, gi * 128:(gi + 1) * 128], start=False, stop=not last)
                                          if last:
                                              ins = e.matmul(o, lhsT=identb[:], rhs=maskT[:], start=False, stop=True)
                                      return ins
                                  T(f, r=[R["qTa"], R["KT"], R["negF2"], R["const"]], w=[R_ps[bank]])

                              def exps(g, ha=ha, nk=nk):
                                  bank = SB[g % 2]
                                  hres = halves[g % 2][1]
                                  for kb in range(g * 4, min(nk, g * 4 + 4)):
                                      A(lambda e, kb=kb: e.activation(out=slot(kb), in_=ps[bank][:, (kb % 4) * 128:(kb % 4 + 1) * 128], func=AF.Exp, bias=nFtok[:, kb, ha:ha + 1]),
                                        r=[R_ps[bank], R["nFt"]], w=[hres])

                              def pv(g, ha=ha, nk=nk):
                                  hres = halves[g % 2][1]

                                  def f(e):
                                      for kb in range(g * 4, min(nk, g * 4 + 4)):
                                          ins = e.matmul(ps[FO][:, 0:129], lhsT=slot(kb), rhs=Vext[:, kb, ha, :], start=(kb == 0), stop=(kb == nk - 1))
                                      return ins
                                  T(f, r=[hres, R["Vext"]], w=[R_ps[FO]])

                              scores(0)
                              exps(0)
                              for g in range(1, ng):
                                  scores(g)
                                  exps(g)
                                  pv(g - 1)
                              pv(ng - 1)
                              V(lambda e: e.reciprocal(out=rr1_[:], in_=ps[FO][:, 128:129]), r=[R_ps[FO]], w=[R_rr1])
                              bvo = NG + DM + ha * 128
                              V(lambda e, bvo=bvo: e.scalar_tensor_tensor(out=of_, in0=ps[FO][:, 0:128], scalar=rr1_[:, 0:1], in1=fbl[:, bvo:bvo + 128], op0=ALU.mult, op1=ALU.add),
                                r=[R_ps[FO], R_rr1, R["fbl"]], w=[R_of])
                              V(lambda e: e.tensor_tensor(out=ya_[:], in0=of_, in1=of_, op=ALU.mult), r=[R_of], w=[R_ya])
                              V(lambda e: e.tensor_reduce(out=ss1_[:], in_=ya_[:], axis=AX.X, op=ALU.add), r=[R_ya], w=[R_ss1])
                              V(lambda e: e.tensor_scalar(out=ss1_[:], in0=ss1_[:], scalar1=1.0 / 128, scalar2=EPS, op0=ALU.mult, op1=ALU.add), r=[R_ss1], w=[R_ss1])
                              A(lambda e: e.activation(out=ss1_[:], in_=ss1_[:], func=AF.Sqrt), r=[R_ss1], w=[R_ss1])
                              V(lambda e: e.reciprocal(out=ss1_[:], in_=ss1_[:]), r=[R_ss1], w=[R_ss1])
                              V(lambda e: e.tensor_scalar(out=ya_[:], in0=of_, scalar1=ss1_[:, 0:1], scalar2=None, op0=ALU.mult), r=[R_of, R_ss1], w=[R_ya])
                              T(lambda e: e.transpose(out=psb[FO][:, 0:128], in_=ya_[:], identity=identb[:]), r=[R_ya, R["const"]], w=[R_ps[FO]])
                              go = lp + c.o_gha + ha
                              A(lambda e, go=go, ha=ha, bi=bi: e.activation(out=m_t[:, HM + ha, bi * 128:(bi + 1) * 128], in_=psb[FO][:, 0:128], func=AF.Identity, scale=pp[:, go:go + 1]),
                                r=[R_ps[FO], R["pp"]], w=[R_m[HM + ha]])

                  def conv_all():
                      wo = lp + c.o_cdw
                      cmean = rs[:]
                      cm2 = sg[:, 0, :]
                      csq = sg[:, 1, :]
                      for cc in range(CC):
                          bC = nb()
                          for tap in range(31):
                              k = dg_ctr["C"] % 4
                              dg_ctr["C"] += 1
                              wc = wo + tap * CC + cc
                              V(lambda e, k=k, wc=wc: e.tensor_scalar(out=dgC[k][:], in0=identb[:], scalar1=pp[:, wc:wc + 1], scalar2=None, op0=ALU.mult),
                                r=[R["const"], R["pp"]], w=[R_dgC[k]])
                              T(lambda e, k=k, tap=tap, cc=cc, bC=bC: e.matmul(ps[bC][:, :], lhsT=dgC[k][:], rhs=y0[:, cc, tap:tap + TT], start=(tap == 0), stop=(tap == 30)),
                                r=[R_dgC[k], R["y0"]], w=[R_ps[bC]])
                          bo_ = lp + c.o_cdb + cc
                          A(lambda e, cc=cc, bC=bC, bo_=bo_: e.activation(out=cvt[cc][:], in_=ps[bC][:, :], func=AF.Identity, bias=pp[:, bo_:bo_ + 1]),
                            r=[R_ps[bC], R["pp"]], w=[R_cv[cc]])
                          A(lambda e, cc=cc: e.activation(out=y0[:, cc, 0:30], in_=y0[:, cc, TT:TT + 30], func=AF.Copy), r=[R["y0"]], w=[R["y0"]])
                      bM = nb()
                      for cc in range(CC):
                          T(lambda e, cc=cc: e.matmul(ps[bM][:, :], lhsT=ones_f[:], rhs=cvt[cc][:], start=(cc == 0), stop=(cc == CC - 1)), r=[R_cv[cc], R["const"]], w=[R_ps[bM]])
                      V(lambda e: e.tensor_scalar(out=cmean, in0=ps[bM][:, :], scalar1=1.0 / DC, scalar2=None, op0=ALU.mult), r=[R_ps[bM]], w=[R["rs"]])
                      bQ = nb()
                      for cc in range(CC):
                          A(lambda e, cc=cc: e.activation(out=csq, in_=cvt[cc][:], func=AF.Square), r=[R_cv[cc]], w=[R["sg"]])
                          T(lambda e, cc=cc: e.matmul(ps[bQ][:, :], lhsT=ones_f[:], rhs=csq, start=(cc == 0), stop=(cc == CC - 1)), r=[R["sg"], R["const"]], w=[R_ps[bQ]])
                      V(lambda e: e.tensor_tensor(out=cm2, in0=cmean, in1=cmean, op=ALU.mult), r=[R["rs"]], w=[R["sg"]])
                      V(lambda e: e.scalar_tensor_tensor(out=cm2, in0=ps[bQ][:, :], scalar=1.0 / DC, in1=cm2, op0=ALU.mult, op1=ALU.subtract), r=[R_ps[bQ], R["sg"]], w=[R["sg"]])
                      V(lambda e: e.tensor_scalar(out=cm2, in0=cm2, scalar1=EPS, scalar2=None, op0=ALU.add), r=[R["sg"]], w=[R["sg"]])
                      A(lambda e: e.activation(out=cm2, in_=cm2, func=AF.Sqrt), r=[R["sg"]], w=[R["sg"]])
                      V(lambda e: e.reciprocal(out=cm2, in_=cm2), r=[R["sg"]], w=[R["sg"]])
                      for cc in range(CC):
                          V(lambda e, cc=cc: e.tensor_tensor(out=cvt[cc][:], in0=cvt[cc][:], in1=cmean, op=ALU.subtract), r=[R_cv[cc], R["rs"]], w=[R_cv[cc]])
                          V(lambda e, cc=cc: e.tensor_tensor(out=cvt[cc][:], in0=cvt[cc][:], in1=cm2, op=ALU.mult), r=[R_cv[cc], R["sg"]], w=[R_cv[cc]])
                          go = lp + c.o_clg + cc
                          bo = lp + c.o_clb + cc
                          A(lambda e, cc=cc, go=go, bo=bo: e.activation(out=m_t[:, HM + HA + cc, :], in_=cvt[cc][:], func=AF.Silu, scale=pp[:, go:go + 1], bias=pp[:, bo:bo + 1]),
                            r=[R_cv[cc], R["pp"]], w=[R_m[HM + HA + cc]])

                  hb = HG // 2
                  pre = list(enumerate(c.in_blocks[:c.n_pre_blocks]))

                  def pre_chain():
                      set_pool("P")
                      for bidx_, (kind, idx, c0) in pre:
                          if kind == "vm":
                              continue
                          slot = load_w(wsc_in[l][bidx_], R_win[l])
                          if kind == "va":
                              do_tm(kind, idx, slot)
                          else:
                              do_fm(kind, idx, slot, 0, split=4)

                  if INTERLEAVE:
                      p.run_chains([gates_chain, pre_chain], (1, 1))
                  else:
                      gates_chain()
                      pre_chain()
                  set_pool("all")
                  for bidx_, (kind, idx, c0) in pre:
                      if kind == "vm":
                          slot = load_w(wsc_in[l][bidx_], R_win[l])
                          do_tm(kind, idx, slot)
                  ckpt('proj')

                  def m_chain():
                      set_pool("M")
                      bi_ = c.n_pre_blocks
                      for (kind, idx, c0) in c.in_blocks[c.n_pre_blocks:]:
                          slot = load_w(wsc_in[l][bi_], R_win[l])
                          bi_ += 1
                          g = idx // hb
                          do_fm(kind, idx, slot, g, split=4)
                          if kind == "om" and (idx % hb) == hb - 1:
                              mlstm_group(g)

                  def c_chain():
                      set_pool("C")
                      conv_all()

                  if INTERLEAVE:
                      p.run_chains([lambda: fox_stream(0), lambda: fox_stream(1), m_chain, c_chain], CH_W)
                  else:
                      m_chain()
                      c_chain()
                      fox_stream(0)
                      fox_stream(1)
                  set_pool("all")
                  ckpt('conv')
                  set_pool("S")
                  BN = 7
                  for j in range(NBO):
                      slot = load_w(wsc_out[l][j], R_wout[l])
                      for cb in range(2):
                          b = nb()
                          fm_group(slot, cb, m_t, R_m, b)
                          dblk = 2 * j + cb
                          V(lambda e, b=b, dblk=dblk: e.tensor_tensor(out=x_t[:, dblk, :], in0=x_t[:, dblk, :], in1=ps[b][:, :], op=ALU.add), r=[R_ps[b], R_x[dblk]], w=[R_x[dblk]])
                          sq = sqb[dblk % 2]
                          A(lambda e, dblk=dblk, sq=sq: e.activation(out=sq[:], in_=x_t[:, dblk, :], func=AF.Square), r=[R_x[dblk]], w=[R_sqb[dblk % 2]])
                          go_ = lp + c.o_gffn + dblk
                          V(lambda e, dblk=dblk, go_=go_: e.tensor_scalar(out=h_t[:, dblk, :], in0=x_t[:, dblk, :], scalar1=pp[:, go_:go_ + 1], scalar2=None, op0=ALU.mult),
                            r=[R_x[dblk], R["pp"]], w=[R_h[dblk]])
                          if dblk >= 1:
                              d1 = dblk - 1
                              T(lambda e, d1=d1: e.matmul(ps[BN][:, :], lhsT=ones_b[:], rhs=sqb[d1 % 2][:], start=(d1 == 0), stop=False),
                                r=[R_sqb[d1 % 2], R["const"]], w=[R_ps[BN]])
                  T(lambda e: e.matmul(ps[BN][:, :], lhsT=ones_b[:], rhs=sqb[(KC - 1) % 2][:], start=(KC == 1), stop=True),
                    r=[R_sqb[(KC - 1) % 2], R["const"]], w=[R_ps[BN]])
                  set_pool("all")
                  ckpt('stageC')
                  V(lambda e: e.tensor_scalar(out=rs[:], in0=ps[BN][:, :], scalar1=1.0 / D, scalar2=EPS, op0=ALU.mult, op1=ALU.add), r=[R_ps[BN]], w=[R["rs"]])
                  A(lambda e: e.activation(out=rs[:], in_=rs[:], func=AF.Sqrt), r=[R["rs"]], w=[R["rs"]])
                  V(lambda e: e.reciprocal(out=rs[:], in_=rs[:]), r=[R["rs"]], w=[R["rs"]])
                  nid = V(lambda e: e.tensor_tensor(out=rs[:], in0=rs[:], in1=rs[:], op=ALU.mult), r=[R["rs"]], w=[R["rs"]])
                  if s == 0 and l + 1 < NL and t < 4:
                      co = cast_ops(l + 1)
                      sel = co[t:t + 1] if NT >= 4 else (co if t == 0 else [])
                      for (f, n), res in sel:
                          p.dma("gpsimd", f, writes=[res], ndma=n, after=[nid])
                  kk = 0
                  for qd in range(4):
                      for j in range(NBO):
                          slot = load_w(wsc_up[l][qd * NBO + j], R_wup[l])
                          for cb in range(2):
                              fcl = 2 * j + cb
                              b = nb()
                              fm_group(slot, cb, h_t, R_h, b)
                              ri = kk % 2
                              kk += 1
                              A(lambda e, b=b, ri=ri: e.activation(out=rl[ri][:], in_=ps[b][:, :], func=AF.Relu), r=[R_ps[b]], w=[R_rl[ri]])
                              V(lambda e, ri=ri: e.tensor_tensor(out=rl[ri][:], in0=rl[ri][:], in1=rl[ri][:], op=ALU.mult), r=[R_rl[ri]], w=[R_rl[ri]])
                              V(lambda e, ri=ri, fcl=fcl: e.tensor_tensor(out=m_t[:, fcl, :], in0=rl[ri][:], in1=rs[:], op=ALU.mult), r=[R_rl[ri], R["rs"]], w=[R_m[fcl]])
                      for j in range(NBO):
                          slot = load_w(wsc_dn[l][qd * NBO + j], R_wdn[l])
                          for cb in range(2):
                              dblk = 2 * j + cb
                              b = nb()
                              fm_group(slot, cb, m_t, R_m, b)
                              V(lambda e, b=b, dblk=dblk: e.tensor_tensor(out=x_t[:, dblk, :], in0=x_t[:, dblk, :], in1=ps[b][:, :], op=ALU.add), r=[R_ps[b], R_x[dblk]], w=[R_x[dblk]])

                  ckpt('ffn')
                  if l < NL - 1:
                      for kc in range(KC):
                          p.dma(XQ, lambda e, sm_, xi=xi, kc=kc: e.dma_start(out=xs_d[xi][:, kc, :], in_=x_t[:, kc, :]).then_inc(sm_, 16), reads=[R_x[kc]], writes=[R_xs[xi][kc]])
                  else:
                      rmsnorm_h(l, c.o_gfin, inplace_f32=True)
                      k = 0
                      for bi in range(NB):
                          for dq in range(D // 512):
                              sa, sr = stgs[k % NSTG]
                              k += 1
                              b = nb()

                              def fto(e, b=b, dq=dq, bi=bi):
                                  for i4 in range(4):
                                      ins = e.transpose(out=ps[b][:, i4 * 128:(i4 + 1) * 128], in_=x_t[:, dq * 4 + i4, bi * 128:(bi + 1) * 128], identity=identf[:])
                                  return ins
                              T(fto, r=R_x[dq * 4:(dq + 1) * 4] + [R["const"]], w=[R_ps[b]])
                              if k % 2 == 0:
                                  V(lambda e, b=b, sa=sa: e.tensor_copy(out=sa, in_=ps[b][:, :]), r=[R_ps[b]], w=[sr])
                              else:
                                  A(lambda e, b=b, sa=sa: e.activation(out=sa, in_=ps[b][:, :], func=AF.Copy), r=[R_ps[b]], w=[sr])
                              dst = y_d[s, tok0 + bi * 128: tok0 + (bi + 1) * 128, dq * 512:(dq + 1) * 512]
                              p.dma("sync", lambda e, sm_, sa=sa, dst=dst: e.dma_start(out=dst, in_=sa).then_inc(sm_, 16), reads=[sr], is_output=True)

    except _Stop:
        pass
    p.emit()
    st.close()
    return nc, p, dbg_out


_CACHE = {}


def kernel(**inputs):
    cfg = Cfg()
    ncores = 8
    pp, fb = pack_params(cfg, inputs)
    nc, prog, _ = build(cfg)
    x = np.ascontiguousarray(inputs["x"], dtype=np.float32)
    in_maps = []
    for ci in range(ncores):
        in_maps.append({
            "x": np.ascontiguousarray(x[ci * cfg.NSEQ:(ci + 1) * cfg.NSEQ]),
            "w_in": inputs["w_in"], "w_out": inputs["w_out"], "w_up": inputs["w_up"], "w_down": inputs["w_down"],
            "pp": pp, "fb": fb,
        })
    res = run_bass_kernel_spmd(nc, in_maps, core_ids=list(range(ncores)))
    out = np.concatenate([np.asarray(r["y"]) for r in res.results], axis=0)
    return out.astype(np.float32, copy=False)
```

```python
import math
from contextlib import ExitStack
import numpy as np
import concourse.bass as bass
import concourse.mybir as mybir
from concourse.bass_utils import run_bass_kernel_spmd

F32 = mybir.dt.float32
BF16 = mybir.dt.bfloat16
AF = mybir.ActivationFunctionType
ALU = mybir.AluOpType
AX = mybir.AxisListType
EPS = 1e-6
ENGS = ("sync", "scalar", "vector", "gpsimd", "tensor")
NSLOT = 16


class Res:
    __slots__ = ("name", "w", "r", "excl")

    def __init__(self, name, excl=False):
        self.name = name
        self.w = None
        self.r = {}
        self.excl = excl


class Op:
    __slots__ = ("eng", "fn", "deps", "sig", "rank", "is_dma", "slot", "val", "ndma", "calls")


class _Tok:
    def __init__(self, rec, idx):
        self.rec, self.idx = rec, idx

    def then_inc(self, sem, n):
        self.rec.calls[self.idx][3] = n
        return self


class _RecEng:
    def __init__(self):
        self.calls = []

    def __getattr__(self, name):
        def f(*a, **k):
            self.calls.append([name, a, k, None])
            return _Tok(self, len(self.calls) - 1)
        return f


class Prog:
    def __init__(self, nc):
        self.nc = nc
        self.ops = []
        self.q = {e: [] for e in ENGS}
        self.dma_cnt = {"sync": 0, "gpsimd": 0, "scalar": 0}
        self.slot_total = {}
        self.slot_last = {}
        self.out_ops = []
        self.cur_chain = None

    def _key(self, oid):
        op = self.ops[oid]
        return ("dma", oid) if op.is_dma else op.eng

    def _rec(self, eng, fn, reads, writes, is_dma=False, ndma=1, is_output=False, after=()):
        rec = _RecEng()
        if fn is not None:
            if is_dma:
                fn(rec, None)
            else:
                fn(rec)
        ent = (eng, rec.calls, list(reads), list(writes), is_dma, ndma, is_output, tuple(after))
        if self.cur_chain is not None:
            self.cur_chain.append(ent)
            return None
        return self.commit(ent)

    def commit(self, ent):
        eng, calls, reads, writes, is_dma, ndma, is_output, after = ent
        if any(r.excl for r in reads):
            writes = list(writes) + [r for r in reads if r.excl]
            reads = [r for r in reads if not r.excl]
        deps = set(after)
        for r in reads:
            if r.w is not None:
                deps.add(r.w)
        for r in writes:
            if r.w is not None:
                deps.add(r.w)
            deps.update(r.r.values())
        op = Op()
        op.eng, op.fn, op.sig, op.rank, op.is_dma, op.ndma = eng, True, False, 0, is_dma, ndma
        op.slot = op.val = None
        oid = len(self.ops)
        if is_dma:
            j = self.dma_cnt[eng]
            self.dma_cnt[eng] = j + 1
            slot = j % NSLOT
            op.slot = slot
            tot = self.slot_total.get((eng, slot), 0) + 16 * ndma
            self.slot_total[(eng, slot)] = tot
            op.val = tot
            prev = self.slot_last.get((eng, slot))
            if prev is not None:
                deps.add(prev)
            self.slot_last[(eng, slot)] = oid
        if eng == "tensor":
            deps = {d for d in deps if not (self.ops[d].eng == "tensor" and not self.ops[d].is_dma)}
        op.deps = deps
        op.calls = calls
        self.ops.append(op)
        self.q[eng].append(oid)
        key = ("dma", oid) if is_dma else eng
        for r in reads:
            r.r[key] = oid
        for r in writes:
            r.w = oid
            r.r = {}
        if is_output:
            self.out_ops.append(oid)
        return oid

    def op(self, eng, fn, reads=(), writes=()):
        return self._rec(eng, fn, reads, writes)

    def dma(self, queue, fn, reads=(), writes=(), ndma=1, is_output=False, after=()):
        return self._rec(queue, fn, reads, writes, is_dma=True, ndma=ndma, is_output=is_output, after=after)

    def run_chains(self, chains, weights):
        lists = []
        for fn in chains:
            self.cur_chain = []
            fn()
            lists.append(self.cur_chain)
            self.cur_chain = None
        idx = [0] * len(lists)
        while any(idx[i] < len(lists[i]) for i in range(len(lists))):
            for i, L in enumerate(lists):
                for _ in range(weights[i]):
                    if idx[i] < len(L):
                        self.commit(L[idx[i]])
                        idx[i] += 1

    def fence(self, frm, to):
        evs = {}
        for fr in frm:
            ids = list(fr.r.values())
            if fr.w is not None:
                ids.append(fr.w)
            for oid in ids:
                k = self._key(oid)
                if evs.get(k, -1) < oid:
                    evs[k] = oid
        for t in to:
            for k, v in evs.items():
                if t.r.get(k, -1) < v:
                    t.r[k] = v

    def emit(self):
        nc = self.nc
        ops = self.ops
        fin = Op()
        fin.eng, fin.fn, fin.sig, fin.rank, fin.is_dma, fin.ndma = "sync", None, False, 0, False, 0
        fin.slot = fin.val = None
        fin.deps = set(self.out_ops)
        fin.calls = []
        ops.append(fin)
        self.q["sync"].append(len(ops) - 1)
        for op in ops:
            for d in op.deps:
                if not ops[d].is_dma:
                    ops[d].sig = True
        for e in ENGS:
            r = 0
            for oid in self.q[e]:
                op = ops[oid]
                if (not op.is_dma) and op.sig:
                    r += 1
                    op.rank = r
        self.nwaits = 0
        with ExitStack() as st:
            esem = {e: st.enter_context(nc.semaphore("es_" + e)) for e in ENGS}
            dsem = {}
            for qn in ("sync", "gpsimd", "scalar"):
                for i in range(NSLOT):
                    dsem[(qn, i)] = st.enter_context(nc.semaphore("ds_%s_%d" % (qn, i)))
            block = st.enter_context(nc.Block())

            def run(ename, e):
                known = {}
                for oid in self.q[ename]:
                    op = ops[oid]
                    waits = {}
                    for d in op.deps:
                        x = ops[d]
                        if x.is_dma:
                            key, val = ("d", x.eng, x.slot), x.val
                        else:
                            key, val = ("e", x.eng), x.rank
                        if waits.get(key, 0) < val:
                            waits[key] = val
                    for key, val in waits.items():
                        if known.get(key, 0) >= val:
                            continue
                        sem = dsem[(key[1], key[2])] if key[0] == "d" else esem[key[1]]
                        e.wait_ge(sem, val)
                        self.nwaits += 1
                        known[key] = val
                    if op.fn is None:
                        continue
                    if op.is_dma:
                        for (name, a, k, n) in op.calls:
                            getattr(e, name)(*a, **k).then_inc(dsem[(ename, op.slot)], 16)
                    else:
                        ins = None
                        for (name, a, k, n) in op.calls:
                            ins = getattr(e, name)(*a, **k)
                        if op.sig:
                            ins.then_inc(esem[ename], 1)

            @block.sync
            def _(e):
                run("sync", e)

            @block.scalar
            def _(e):
                run("scalar", e)

            @block.vector
            def _(e):
                run("vector", e)

            @block.gpsimd
            def _(e):
                run("gpsimd", e)

            @block.tensor
            def _(e):
                run("tensor", e)


class Cfg:
    def __init__(self, D=2048, S=2048, NL=4, NSEQ=2, HG=4):
        self.D, self.S, self.NL, self.NSEQ = D, S, NL, NSEQ
        self.KC = D // 128
        self.DM = D // 2
        self.HM = self.DM // 128
        self.DF = D // 4
        self.HA = self.DF // 128
        self.DC = D - self.DM - self.DF
        self.CC = self.DC // 128
        self.DFF = 4 * D
        self.HG = min(HG, self.HM)
        self.NGM = self.HM // self.HG
        self.TT = 512
        self.NT = S // self.TT
        self.NB = 4
        self.NBLK = S // 128
        DM, HM, DF, HA, DC = self.DM, self.HM, self.DF, self.HA, self.DC
        self.DIN = 4 * DM + 2 * HM + 3 * DF + HA + 2 * DC
        self.c_qm, self.c_km, self.c_vm, self.c_om = 0, DM, 2 * DM, 3 * DM
        self.c_im, self.c_fm = 4 * DM, 4 * DM + HM
        self.c_qa = 4 * DM + 2 * HM
        self.c_ka = self.c_qa + DF
        self.c_va = self.c_qa + 2 * DF
        self.c_fa = self.c_qa + 3 * DF
        self.c_uc = self.c_fa + HA
        self.c_gc = self.c_uc + DC
        self.NG = 2 * HM + HA
        self.NF = HM + HA
        self.NBO = D // 256
        blks = []
        for i in range(DM // 256):
            blks.append(("vm", i, self.c_vm + i * 256))
        for i in range(DF // 256):
            blks.append(("va", i, self.c_va + i * 256))
        for i in range(DF // 256):
            blks.append(("qa", i, self.c_qa + i * 256))
        for i in range(DF // 256):
            blks.append(("ka", i, self.c_ka + i * 256))
        for i in range(DC // 256):
            blks.append(("gc", i, self.c_gc + i * 256))
            blks.append(("uc", i, self.c_uc + i * 256))
        self.n_pre_blocks = len(blks)
        hb = self.HG // 2
        for g in range(self.NGM):
            for kind, c0 in (("qm", self.c_qm), ("km", self.c_km), ("om", self.c_om)):
                for i in range(g * hb, (g + 1) * hb):
                    blks.append((kind, i, c0 + i * 256))
        self.in_blocks = blks
        self.NBI = len(blks)
        KC, CC = self.KC, self.CC
        o = 0
        self.o_gmix = o; o += KC
        self.o_gffn = o; o += KC
        self.fm_kinds = [("qm", HM, self.c_qm), ("km", HM, self.c_km), ("om", HM, self.c_om), ("qa", HA, self.c_qa),
                         ("ka", HA, self.c_ka), ("gc", CC, self.c_gc), ("uc", CC, self.c_uc)]
        self.o_bfm = {}
        for kind, n, c0 in self.fm_kinds:
            self.o_bfm[kind] = o
            o += n
        self.o_mcw = o; o += 4 * 2 * HM
        self.o_mcb = o; o += 2 * HM
        self.o_cdw = o; o += 31 * CC
        self.o_cdb = o; o += CC
        self.o_clg = o; o += CC
        self.o_clb = o; o += CC
        self.o_ghm = o; o += HM
        self.o_gha = o; o += HA
        self.LPP = o
        self.o_gfin = self.NL * self.LPP
        self.NPP = self.o_gfin + KC
        self.NFB = self.NG + DM + DF


def pack_params(cfg, inp):
    c = cfg
    pp = np.zeros((128, c.NPP), np.float32)

    def fm(vec):
        return np.ascontiguousarray(vec.reshape(-1, 128).T)

    for l in range(c.NL):
        b = l * c.LPP
        pp[:, b + c.o_gmix: b + c.o_gmix + c.KC] = fm(inp["norm_mix"][l])
        pp[:, b + c.o_gffn: b + c.o_gffn + c.KC] = fm(inp["norm_ffn"][l])
        for kind, n, c0 in c.fm_kinds:
            pp[:, b + c.o_bfm[kind]: b + c.o_bfm[kind] + n] = fm(inp["b_in"][l, c0:c0 + n * 128])
        for tap in range(4):
            pp[:, b + c.o_mcw + tap * 2 * c.HM: b + c.o_mcw + (tap + 1) * 2 * c.HM] = fm(inp["mlstm_conv_w"][l, tap])
        pp[:, b + c.o_mcb: b + c.o_mcb + 2 * c.HM] = fm(inp["mlstm_conv_b"][l])
        for tap in range(31):
            pp[:, b + c.o_cdw + tap * c.CC: b + c.o_cdw + (tap + 1) * c.CC] = fm(inp["conv_dw_w"][l, tap])
        pp[:, b + c.o_cdb: b + c.o_cdb + c.CC] = fm(inp["conv_dw_b"][l])
        pp[:, b + c.o_clg: b + c.o_clg + c.CC] = fm(inp["conv_ln_g"][l])
        pp[:, b + c.o_clb: b + c.o_clb + c.CC] = fm(inp["conv_ln_b"][l])
        pp[:, b + c.o_ghm: b + c.o_ghm + c.HM] = fm(inp["mlstm_head_norm"][l].reshape(-1))
        pp[:, b + c.o_gha: b + c.o_gha + c.HA] = fm(inp["fox_head_norm"][l].reshape(-1))
    pp[:, c.o_gfin: c.o_gfin + c.KC] = fm(inp["final_norm"])
    fb = np.zeros((c.NL, 128, c.NFB), np.float32)
    for l in range(c.NL):
        row = np.concatenate([inp["b_in"][l, c.c_im:c.c_im + 2 * c.HM], inp["b_in"][l, c.c_fa:c.c_fa + c.HA],
                              inp["b_in"][l, c.c_vm:c.c_vm + c.DM], inp["b_in"][l, c.c_va:c.c_va + c.DF]])
        fb[l] = np.broadcast_to(row[None, :], (128, c.NFB))
    return pp, fb


class _Stop(Exception):
    pass


import os as _os
INTERLEAVE = True
CONV4_PE = bool(int(_os.environ.get("CONV4_PE", "1")))
XQ = "scalar"
CH_W = tuple(int(v) for v in _os.environ.get("CHW", "1,1,2,1").split(","))


def build(cfg, dbg_names=(), stop_at=None):
    c = cfg
    D, S, NL, NSEQ, KC, DM, HM, DF, HA, DC, CC, DFF = c.D, c.S, c.NL, c.NSEQ, c.KC, c.DM, c.HM, c.DF, c.HA, c.DC, c.CC, c.DFF
    HG, NGM, NT, NB, NG, NF, NBO, NBI = c.HG, c.NGM, c.NT, c.NB, c.NG, c.NF, c.NBO, c.NBI
    TT = c.TT
    nc = bass.Bass("TRN2", target_bir_lowering=False)
    x_d = nc.dram_tensor("x", [NSEQ, S, D], F32, kind="ExternalInput").ap()
    w_in_d = nc.dram_tensor("w_in", [NL, D, c.DIN], F32, kind="ExternalInput").ap()
    w_out_d = nc.dram_tensor("w_out", [NL, D, D], F32, kind="ExternalInput").ap()
    w_up_d = nc.dram_tensor("w_up", [NL, D, DFF], F32, kind="ExternalInput").ap()
    w_dn_d = nc.dram_tensor("w_down", [NL, DFF, D], F32, kind="ExternalInput").ap()
    pp_d = nc.dram_tensor("pp", [128, c.NPP], F32, kind="ExternalInput").ap()
    fb_d = nc.dram_tensor("fb", [NL, 128, c.NFB], F32, kind="ExternalInput").ap()
    y_d = nc.dram_tensor("y", [NSEQ, S, D], F32, kind="ExternalOutput").ap()
    xs_d = nc.dram_tensor("xs_scr", [NSEQ * NT, 128, KC, TT], F32, kind="Internal").ap()
    wsc_in = [nc.dram_tensor("wsc_in%d" % l, [NBI, 128, KC, 256], BF16, kind="Internal").ap() for l in range(NL)]
    wsc_out = [nc.dram_tensor("wsc_out%d" % l, [NBO, 128, KC, 256], BF16, kind="Internal").ap() for l in range(NL)]
    wsc_up = [nc.dram_tensor("wsc_up%d" % l, [4 * NBO, 128, KC, 256], BF16, kind="Internal").ap() for l in range(NL)]
    wsc_dn = [nc.dram_tensor("wsc_dn%d" % l, [4 * NBO, 128, KC, 256], BF16, kind="Internal").ap() for l in range(NL)]
    dbg_out = {}

    st = ExitStack()
    p = Prog(nc)

    def sb(name, shape, dt):
        return st.enter_context(nc.sbuf_tensor("s_" + name, shape, dt))

    x_t = sb("x_t", [128, KC, TT], F32)
    h_t = sb("h_t", [128, KC, TT], BF16)
    m_t = sb("m_t", [128, KC, TT], BF16)
    NWB = 3
    wb = [sb("wb%d" % i, [128, KC, 256], BF16) for i in range(NWB)]
    wg = sb("wg", [128, NL, KC, NG], BF16)
    stg_io = [sb("stg_io%d" % i, [128, 512], F32) for i in range(2)]
    rs = sb("rs", [128, TT], F32)
    sqb = [sb("sqb%d" % i, [128, TT], BF16) for i in range(2)]
    pp = sb("pp", [128, c.NPP], F32)
    fbl = sb("fbl", [128, c.NFB], F32)
    identb = sb("identb", [128, 128], BF16)
    identf = sb("identf", [128, 128], F32)
    ones_b = sb("ones_b", [128, 128], BF16)
    ones_f = sb("ones_f", [128, 128], F32)
    Utri = sb("Utri", [128, 128], F32)
    sel2 = [sb("sel2_%d" % h, [2 * HA, 128], BF16) for h in range(HA)]
    qT = sb("qT", [128, HG, TT], BF16)
    kT = sb("kT", [128, HG, TT], BF16)
    sgo = sb("sgo", [128, HG, TT], BF16)
    vx = sb("vx", [128, NB, HM, 129], BF16)
    Cf = sb("Cf", [128, HM, 129], F32)
    Cb = sb("Cb", [128, HM, 129], BF16)
    cstg = sb("cstg", [128, TT + 3], F32)
    cacc = sb("cacc", [128, TT], F32)
    chist = sb("chist", [128, 2 * HM, 3], F32)
    Gt = sb("Gt", [128, NB, NG], F32)
    Et = sb("Et", [128, NF], F32)
    Lt = sb("Lt", [128, NB, NF], F32)
    gtmp = sb("gtmp", [128, HM], F32)
    gcs = sb("gcs", [128, 128], F32)
    aa = sb("aa", [128, NB, HM], F32)
    einv = sb("einv", [128, NB, HM], F32)
    ebl = sb("ebl", [128, NB, HM], F32)
    carry = sb("carry", [128, HA], F32)
    nF2 = sb("nF2", [128, 2 * HA], BF16)
    qTa = sb("qTa", [128, HA, TT], BF16)
    KT = sb("KT", [128, HA, S], BF16)
    Vext = sb("Vext", [128, c.NBLK, HA, 129], BF16)
    negF2 = sb("negF2", [2 * HA, S], BF16)
    y0 = sb("y0", [128, CC, TT + 30], BF16)
    sg = sb("sg", [128, 2, TT], F32)
    rl = [sb("rl%d" % i, [128, TT], F32) for i in range(2)]
    dn = sb("dn", [128, HG], F32)
    rr = sb("rr", [128, HG], F32)
    dr = sb("dr", [128, HG], F32)
    ss = sb("ss", [128, HG], F32)

    assert CC <= 4 and HG * 128 <= TT
    ktok = sb("ktok", [128, HG * 128], BF16)
    STm = sb("STm", [128, HG * 128], BF16)
    ytok = sb("ytok", [128, HG * 128], BF16)
    sm = {"ktok": ktok[:], "STm": STm[:], "ytok": ytok[:], "hf": cstg[:, 0:HG * 128], "sqf": cacc[:, 0:HG * 128]}
    PTc = sb("PTc", [128, 1024], BF16)
    cstgb = sb("cstgb", [128, TT + 3], BF16)
    dgM = [sb("dgM%d" % i, [128, 128], BF16) for i in range(2)]
    dgC = [sb("dgC%d" % i, [128, 128], BF16) for i in range(4)]
    yas = [sb("ya%d" % i, [128, 128], BF16) for i in range(2)]
    of1 = sb("of1", [128, 128], F32)
    rr1s = [sb("rr1_%d" % i, [128, 1], F32) for i in range(2)]
    ss1s = [sb("ss1_%d" % i, [128, 1], F32) for i in range(2)]
    nFtok = sb("nFtok", [128, c.NBLK, HA], F32)
    maskT = sb("maskT", [128, 128], BF16)
    ofs = [gcs[:, 0:128], of1[:]]

    ps = [st.enter_context(nc.psum_tensor("ps%d" % i, [128, 512], F32)) for i in range(8)]
    psb = [t[:].bitcast(BF16) for t in ps]

    R_x = [Res("x%d" % k) for k in range(KC)]
    R_h = [Res("h%d" % k) for k in range(KC)]
    R_m = [Res("m%d" % k) for k in range(KC)]
    R_wb = [Res("wb%d" % i) for i in range(NWB)]
    R_ps = [Res("ps%d" % i, excl=True) for i in range(8)]
    R_io = [Res("io0"), Res("io1")]
    R_sqb = [Res("sqb0"), Res("sqb1")]
    R_rl = [Res("rl0"), Res("rl1")]
    R = {k: Res(k) for k in ["wg", "rs", "pp", "fbl", "const", "qT", "kT", "sgo", "vx", "Cf", "Cb", "cstg", "cacc", "chist",
                             "Gt", "Et", "Lt", "gtmp", "gcs", "aa", "einv", "ebl", "nFt", "carry", "nF2", "qTa", "KT", "Vext", "negF2",
                             "y0", "sg", "dn", "rr", "dr", "ss", "mx", "negm", "rr1", "ss1",
                             "ktok", "STm", "hf", "sqf", "ytok", "P", "PT", "of", "sq1", "ya", "cv", "cmean", "cm2", "csq"]}
    R["hf"] = R["cstg"]
    R["sqf"] = R["cacc"]
    R["of"] = R["gcs"]
    for i_ in range(2):
        for nm in ("PTc%d", "rr1_%d", "ss1_%d", "ya_%d"):
            R[nm % i_] = Res(nm % i_)
    R_ofs = [R["gcs"], Res("of1")]
    R["cstgb"] = Res("cstgb")
    R_dgM = [Res("dgM%d" % i) for i in range(2)]
    R_dgC = [Res("dgC%d" % i) for i in range(4)]
    dg_ctr = {"M": 0, "C": 0}
    R_cv = [R_rl[0], R_rl[1], R_io[0], R_io[1]]
    cvt = [rl[0], rl[1], stg_io[0], stg_io[1]]
    stgs = [(stg_io[0][:], R_io[0]), (stg_io[1][:], R_io[1]), (rl[0][:], R_rl[0]), (rl[1][:], R_rl[1]), (cstg[:, 0:512], R["cstg"]), (cacc[:, 0:512], R["cacc"])]
    NSTG = len(stgs)

    R_xs = [[Res("xs%d_%d" % (i, k)) for k in range(KC)] for i in range(NSEQ * NT)]
    R_win = [Res("win%d" % l) for l in range(NL)]
    R_wout = [Res("wout%d" % l) for l in range(NL)]
    R_wup = [Res("wup%d" % l) for l in range(NL)]
    R_wdn = [Res("wdn%d" % l) for l in range(NL)]

    V = lambda fn, r=(), w=(): p.op("vector", fn, r, w)
    A = lambda fn, r=(), w=(): p.op("scalar", fn, r, w)
    T = lambda fn, r=(), w=(): p.op("tensor", fn, r, w)
    G = lambda fn, r=(), w=(): p.op("gpsimd", fn, r, w)

    POOLS = {"all": list(range(8)), "F0": [0, 0, 1], "F1": [2, 2, 3], "M": [4, 5], "C": [6, 7], "G": [0, 1], "P": [2, 3, 4, 5, 6, 7], "S": [0, 1, 2, 3, 4, 5, 6]}
    bank_ctr = {"all": 0, "M": 0, "C": 0, "G": 0, "P": 0, "S": 0}
    cur_pool = ["all"]

    def set_pool(name):
        cur_pool[0] = name

    def nb():
        pn = cur_pool[0]
        pool = POOLS[pn]
        b = pool[bank_ctr[pn] % len(pool)]
        bank_ctr[pn] += 1
        return b

    def dbg(name, ap, shape, res):
        if name not in dbg_names:
            return
        key = name
        i = 0
        while key in dbg_out:
            i += 1
            key = "%s_%d" % (name, i)
        d = nc.dram_tensor("dbg_" + key, shape, ap.dtype, kind="ExternalOutput").ap()
        dbg_out[key] = d
        p.dma("sync", lambda e, s: e.dma_start(out=d, in_=ap).then_inc(s, 16), reads=res, is_output=True)

    Rc = [R["const"]]
    G(lambda e: e.memset(identb[:], 0.0), w=Rc)
    G(lambda e: e.affine_select(out=identb[:], in_=identb[:], pattern=[[-1, 128]], compare_op=ALU.not_equal, fill=1.0, base=0, channel_multiplier=1), r=Rc, w=Rc)
    G(lambda e: e.memset(identf[:], 0.0), w=Rc)
    G(lambda e: e.affine_select(out=identf[:], in_=identf[:], pattern=[[-1, 128]], compare_op=ALU.not_equal, fill=1.0, base=0, channel_multiplier=1), r=Rc, w=Rc)
    G(lambda e: e.memset(ones_b[:], 1.0), w=Rc)
    G(lambda e: e.memset(ones_f[:], 1.0), w=Rc)
    G(lambda e: e.memset(Utri[:], 1.0), w=Rc)
    G(lambda e: e.affine_select(out=Utri[:], in_=Utri[:], pattern=[[1, 128]], compare_op=ALU.is_ge, fill=0.0, base=0, channel_multiplier=-1), r=Rc, w=Rc)
    G(lambda e: e.memset(maskT[:], 0.0), w=Rc)
    G(lambda e: e.affine_select(out=maskT[:], in_=maskT[:], pattern=[[1, 128]], compare_op=ALU.is_ge, fill=-30000.0, base=0, channel_multiplier=-1), r=Rc, w=Rc)
    for h in range(HA):
        G(lambda e, h=h: e.memset(sel2[h][:], 0.0), w=Rc)
        G(lambda e, h=h: e.affine_select(out=sel2[h][:], in_=sel2[h][:], pattern=[[0, 128]], compare_op=ALU.not_equal, fill=-1.0, base=-h, channel_multiplier=1), r=Rc, w=Rc)
        G(lambda e, h=h: e.affine_select(out=sel2[h][:], in_=sel2[h][:], pattern=[[0, 128]], compare_op=ALU.not_equal, fill=-1.0, base=-(HA + h), channel_multiplier=1), r=Rc, w=Rc)
    G(lambda e: e.memset(Vext[:, :, :, 128:129], 1.0), w=[R["Vext"]])
    p.dma("sync", lambda e, s: e.dma_start(out=pp[:], in_=pp_d[:, :]).then_inc(s, 16), writes=[R["pp"]])

    def load_wg(e, s):
        for l in range(NL):
            src1 = w_in_d[l, :, c.c_im:c.c_im + 2 * HM].rearrange("(k p) c -> p k c", p=128)
            e.dma_start(out=wg[:, l, :, 0:2 * HM], in_=src1).then_inc(s, 16)
            src2 = w_in_d[l, :, c.c_fa:c.c_fa + HA].rearrange("(k p) c -> p k c", p=128)
            e.dma_start(out=wg[:, l, :, 2 * HM:NG], in_=src2).then_inc(s, 16)
    p.dma("gpsimd", load_wg, writes=[R["wg"]], ndma=2 * NL)

    in_runs = []
    i = 0
    while i < NBI:
        j = i
        while j + 1 < NBI and c.in_blocks[j + 1][2] == c.in_blocks[j][2] + 256:
            j += 1
        in_runs.append((c.in_blocks[i][2], j - i + 1, i))
        i = j + 1

    def mk_cast_in(l):
        def f(e, s):
            for kc in range(KC):
                for (c0, n, b0) in in_runs:
                    src = w_in_d[l, kc * 128:(kc + 1) * 128, c0:c0 + n * 256].rearrange("p (b c) -> p b c", c=256)
                    dst = wsc_in[l][b0:b0 + n, :, kc, :].rearrange("b p c -> p b c")
                    e.dma_start(out=dst, in_=src).then_inc(s, 16)
        return f, KC * len(in_runs)

    def mk_cast_sq(l, srcd, dstd, ncols):
        def f(e, s):
            for kc in range(KC):
                src = srcd[l, kc * 128:(kc + 1) * 128, 0:ncols].rearrange("p (b c) -> p b c", c=256)
                dst = dstd[l][0:ncols // 256, :, kc, :].rearrange("b p c -> p b c")
                e.dma_start(out=dst, in_=src).then_inc(s, 16)
        return f, KC

    def mk_cast_up(l):
        return mk_cast_sq(l, w_up_d, wsc_up, DFF)

    def mk_cast_dn(l):
        def f(e, s):
            for rg in range(DFF // 128):
                qd, kcl = rg // KC, rg % KC
                src = w_dn_d[l, rg * 128:(rg + 1) * 128, :].rearrange("p (b c) -> p b c", c=256)
                dst = wsc_dn[l][qd * NBO:(qd + 1) * NBO, :, kcl, :].rearrange("b p c -> p b c")
                e.dma_start(out=dst, in_=src).then_inc(s, 16)
        return f, DFF // 128

    def cast_ops(l):
        return [(mk_cast_in(l), R_win[l]), (mk_cast_sq(l, w_out_d, wsc_out, D), R_wout[l]), (mk_cast_up(l), R_wup[l]), (mk_cast_dn(l), R_wdn[l])]

    for (f, n), res in cast_ops(0):
        p.dma("gpsimd", f, writes=[res], ndma=n)

    wb_ctr = [0]

    def load_w(src_ap, res):
        slot = wb_ctr[0] % NWB
        wb_ctr[0] += 1
        p.dma("sync", lambda e, s: e.dma_start(out=wb[slot][:], in_=src_ap).then_inc(s, 16), reads=[res], writes=[R_wb[slot]])
        return slot

    def rmsnorm_h(l, goff, inplace_f32=False):
        b = nb()
        last = None
        for kc in range(KC):
            sq = sqb[kc % 2]
            A(lambda e, kc=kc, sq=sq: e.activation(out=sq[:], in_=x_t[:, kc, :], func=AF.Square), r=[R_x[kc]], w=[R_sqb[kc % 2]])
            T(lambda e, kc=kc, sq=sq: e.matmul(ps[b][:, :], lhsT=ones_b[:], rhs=sq[:], start=(kc == 0), stop=(kc == KC - 1)),
              r=[R_sqb[kc % 2], R["const"]], w=[R_ps[b]])
        V(lambda e: e.tensor_scalar(out=rs[:], in0=ps[b][:, :], scalar1=1.0 / D, scalar2=EPS, op0=ALU.mult, op1=ALU.add), r=[R_ps[b]], w=[R["rs"]])
        A(lambda e: e.activation(out=rs[:], in_=rs[:], func=AF.Sqrt), r=[R["rs"]], w=[R["rs"]])
        V(lambda e: e.reciprocal(out=rs[:], in_=rs[:]), r=[R["rs"]], w=[R["rs"]])
        for kc in range(KC):
            gc_ = pp[:, goff + kc: goff + kc + 1]
            if inplace_f32:
                V(lambda e, kc=kc, gc_=gc_: e.scalar_tensor_tensor(out=x_t[:, kc, :], in0=x_t[:, kc, :], scalar=gc_, in1=rs[:], op0=ALU.mult, op1=ALU.mult),
                  r=[R_x[kc], R["rs"], R["pp"]], w=[R_x[kc]])
            else:
                last = V(lambda e, kc=kc, gc_=gc_: e.scalar_tensor_tensor(out=h_t[:, kc, :], in0=x_t[:, kc, :], scalar=gc_, in1=rs[:], op0=ALU.mult, op1=ALU.mult),
                         r=[R_x[kc], R["rs"], R["pp"]], w=[R_h[kc]])
        return last

    def fm_group(slot, cb, rhs_t, R_rhs, b, split=1):
        per = (KC + split - 1) // split
        for k0 in range(0, KC, per):
            def f(e, k0=k0):
                for kc in range(k0, min(KC, k0 + per)):
                    ins = e.matmul(ps[b][:, :], lhsT=wb[slot][:, kc, cb * 128:(cb + 1) * 128], rhs=rhs_t[:, kc, :], start=(kc == 0), stop=(kc == KC - 1))
                return ins
            T(f, r=[R_wb[slot]] + R_rhs[k0:k0 + per], w=[R_ps[b]])

    def ckpt(name):
        if stop_at == name:
            raise _Stop()

    try:
      ckpt('setup')
      for s in range(NSEQ):
          for l in range(NL):
              lp = l * c.LPP
              p.dma("sync", lambda e, sm_, l=l: e.dma_start(out=fbl[:], in_=fb_d[l]).then_inc(sm_, 16), writes=[R["fbl"]])
              V(lambda e: e.memset(Cf[:], 0.0), w=[R["Cf"]])
              V(lambda e: e.memset(Cb[:], 0.0), w=[R["Cb"]])
              V(lambda e: e.memset(carry[:], 0.0), w=[R["carry"]])
              V(lambda e: e.memset(chist[:], 0.0), w=[R["chist"]])
              V(lambda e: e.memset(y0[:, :, 0:30], 0.0), w=[R["y0"]])
              for t in range(NT):
                  xi = s * NT + t
                  tok0 = t * TT
                  if l == 0:
                      k = 0
                      for bi in range(NB):
                          for dq in range(D // 512):
                              sa, sr = stgs[k % NSTG]
                              k += 1
                              src = x_d[s, tok0 + bi * 128: tok0 + (bi + 1) * 128, dq * 512:(dq + 1) * 512]
                              p.dma("sync", lambda e, sm_, sa=sa, src=src: e.dma_start(out=sa, in_=src).then_inc(sm_, 16), writes=[sr])
                              b = nb()

                              def ftr(e, sa=sa, b=b):
                                  for i4 in range(4):
                                      ins = e.transpose(out=ps[b][:, i4 * 128:(i4 + 1) * 128], in_=sa[:, i4 * 128:(i4 + 1) * 128], identity=identf[:])
                                  return ins
                              T(ftr, r=[sr, R["const"]], w=[R_ps[b]])
                              V(lambda e, b=b, dq=dq, bi=bi: e.tensor_copy(out=x_t[:, dq * 4:(dq + 1) * 4, bi * 128:(bi + 1) * 128],
                                                                       in_=ps[b][:, :].rearrange("p (k t) -> p k t", k=4)),
                                r=[R_ps[b]], w=R_x[dq * 4:(dq + 1) * 4])
                  else:
                      for kc in range(KC):
                          p.dma(XQ, lambda e, sm_, xi=xi, kc=kc: e.dma_start(out=x_t[:, kc, :], in_=xs_d[xi][:, kc, :]).then_inc(sm_, 16), reads=[R_xs[xi][kc]], writes=[R_x[kc]])

                  ckpt('xload')
                  rmsnorm_h(l, lp + c.o_gmix)

                  ckpt('normA')
                  def gates_chain():
                      set_pool("G")
                      for bi in range(NB):
                          gblk = t * NB + bi
                          b = nb()

                          def fg(e, bi=bi, b=b):
                              for kc in range(KC):
                                  ins = e.matmul(ps[b][:, 0:NG], lhsT=h_t[:, kc, bi * 128:(bi + 1) * 128], rhs=wg[:, l, kc, :], start=(kc == 0), stop=(kc == KC - 1))
                              return ins
                          T(fg, r=R_h + [R["wg"]], w=[R_ps[b]])
                          V(lambda e, bi=bi, b=b: e.tensor_tensor(out=Gt[:, bi, :], in0=ps[b][:, 0:NG], in1=fbl[:, 0:NG], op=ALU.add), r=[R_ps[b], R["fbl"]], w=[R["Gt"]])
                          A(lambda e, bi=bi: e.activation(out=Et[:], in_=Gt[:, bi, HM:NG], func=AF.Exp, scale=-1.0), r=[R["Gt"]], w=[R["Et"]])
                          A(lambda e, bi=bi: e.activation(out=Lt[:, bi, :], in_=Et[:], func=AF.Ln, bias=1.0), r=[R["Et"]], w=[R["Lt"]])
                          b2 = nb()

                          def fcs(e, bi=bi, b2=b2):
                              e.matmul(ps[b2][:, 0:NF], lhsT=Utri[:], rhs=Lt[:, bi, :], start=True, stop=True)
                              return e.matmul(ps[b2][:, 64:64 + NF], lhsT=ones_f[:], rhs=Lt[:, bi, :], start=True, stop=True)
                          T(fcs, r=[R["Lt"], R["const"]], w=[R_ps[b2]])
                          V(lambda e, b2=b2: e.tensor_copy(out=gcs[:], in_=ps[b2][:, 0:128]), r=[R_ps[b2]], w=[R["gcs"]])
                          lnc = math.log(128.0 ** -0.5)
                          V(lambda e, bi=bi, b2=b2: e.scalar_tensor_tensor(out=gtmp[:], in0=gcs[:, 0:HM], scalar=lnc, in1=Gt[:, bi, 0:HM], op0=ALU.add, op1=ALU.add),
                            r=[R["gcs"], R["Gt"]], w=[R["gtmp"]])
                          A(lambda e, bi=bi: e.activation(out=aa[:, bi, :], in_=gtmp[:], func=AF.Exp), r=[R["gtmp"]], w=[R["aa"]])
                          A(lambda e, bi=bi, b2=b2: e.activation(out=einv[:, bi, :], in_=gcs[:, 0:HM], func=AF.Exp), r=[R["gcs"]], w=[R["einv"]])
                          A(lambda e, bi=bi, b2=b2: e.activation(out=ebl[:, bi, :], in_=gcs[:, 64:64 + HM], func=AF.Exp, scale=-1.0), r=[R["gcs"]], w=[R["ebl"]])
                          V(lambda e, b2=b2, gblk=gblk: e.tensor_tensor(out=nFtok[:, gblk, :], in0=gcs[:, HM:NF], in1=carry[:], op=ALU.add), r=[R["gcs"], R["carry"]], w=[R["nFt"]])
                          V(lambda e, b2=b2: e.tensor_tensor(out=carry[:], in0=gcs[:, 64 + HM:64 + NF], in1=carry[:], op=ALU.add), r=[R["gcs"], R["carry"]], w=[R["carry"]])
                          V(lambda e, gblk=gblk: e.tensor_copy(out=nF2[:, 0:HA], in_=nFtok[:, gblk, :]), r=[R["nFt"]], w=[R["nF2"]])
                          V(lambda e, gblk=gblk: e.tensor_tensor(out=nF2[:, HA:2 * HA], in0=nFtok[:, gblk, :], in1=nF2[:, 0:HA], op=ALU.subtract), r=[R["nFt"], R["nF2"]], w=[R["nF2"]])
                          b3 = nb()
                          T(lambda e, b3=b3: e.transpose(out=psb[b3][0:2 * HA, 0:128], in_=nF2[:, :], identity=identb[:]), r=[R["nF2"], R["const"]], w=[R_ps[b3]])
                          A(lambda e, b3=b3, gblk=gblk: e.activation(out=negF2[:, gblk * 128:(gblk + 1) * 128], in_=psb[b3][0:2 * HA, 0:128], func=AF.Copy),
                            r=[R_ps[b3]], w=[R["negF2"]])
                          V(lambda e, bi=bi: e.tensor_copy(out=vx[:, bi, :, 128:129], in_=aa[:, bi, :].unsqueeze(2)), r=[R["aa"]], w=[R["vx"]])

                  ckpt('gates')
                  def do_tm(kind, idx, slot):
                      for bi in range(NB):
                          b = nb()

                          def f(e, bi=bi, b=b):
                              for kc in range(KC):
                                  ins = e.matmul(ps[b][:, 0:256], lhsT=h_t[:, kc, bi * 128:(bi + 1) * 128], rhs=wb[slot][:, kc, :], start=(kc == 0), stop=(kc == KC - 1))
                              return ins
                          T(f, r=R_h + [R_wb[slot]], w=[R_ps[b]])
                          h0 = 2 * idx
                          pv = ps[b][:, 0:256].rearrange("p (h e) -> p h e", h=2)
                          if kind == "vm":
                              V(lambda e, bi=bi, pv=pv, h0=h0: e.tensor_tensor(out=vx[:, bi, h0:h0 + 2, 0:128], in0=pv,
                                                                            in1=aa[:, bi, h0:h0 + 2].unsqueeze(2).broadcast_to([128, 2, 128]), op=ALU.mult),
                                r=[R_ps[b], R["aa"]], w=[R["vx"]])
                          else:
                              gblk = t * NB + bi
                              A(lambda e, gblk=gblk, pv=pv, h0=h0: e.activation(out=Vext[:, gblk, h0:h0 + 2, 0:128], in_=pv, func=AF.Copy),
                                r=[R_ps[b]], w=[R["Vext"]])

                  def conv4_evac_dve(b, cj, dest_ap, boff, R_dest):
                      A(lambda e: e.activation(out=cstg[:, 3:3 + TT], in_=ps[b][:, :], func=AF.Identity, bias=pp[:, boff:boff + 1]),
                        r=[R_ps[b], R["pp"]], w=[R["cstg"]])
                      V(lambda e: e.tensor_copy(out=cstg[:, 0:3], in_=chist[:, cj, :]), r=[R["chist"]], w=[R["cstg"]])
                      wo = lp + c.o_mcw
                      bo = lp + c.o_mcb + cj
                      V(lambda e: e.tensor_scalar(out=cacc[:], in0=cstg[:, 3:3 + TT], scalar1=pp[:, wo + 3 * 2 * HM + cj: wo + 3 * 2 * HM + cj + 1],
                                                  scalar2=pp[:, bo:bo + 1], op0=ALU.mult, op1=ALU.add), r=[R["cstg"], R["pp"]], w=[R["cacc"]])
                      for tap in (2, 1, 0):
                          V(lambda e, tap=tap: e.scalar_tensor_tensor(out=cacc[:], in0=cstg[:, tap:tap + TT], scalar=pp[:, wo + tap * 2 * HM + cj: wo + tap * 2 * HM + cj + 1],
                                                                      in1=cacc[:], op0=ALU.mult, op1=ALU.add), r=[R["cstg"], R["cacc"], R["pp"]], w=[R["cacc"]])
                      V(lambda e: e.tensor_copy(out=chist[:, cj, :], in_=cstg[:, TT:TT + 3]), r=[R["cstg"]], w=[R["chist"]])
                      A(lambda e: e.activation(out=dest_ap, in_=cacc[:], func=AF.Silu), r=[R["cacc"]], w=[R_dest])

                  def conv4_evac(b, cj, dest_ap, boff, R_dest):
                      A(lambda e: e.activation(out=cstgb[:, 3:3 + TT], in_=ps[b][:, :], func=AF.Identity, bias=pp[:, boff:boff + 1]),
                        r=[R_ps[b], R["pp"]], w=[R["cstgb"]])
                      V(lambda e: e.tensor_copy(out=cstgb[:, 0:3], in_=chist[:, cj, :]), r=[R["chist"]], w=[R["cstgb"]])
                      V(lambda e: e.tensor_copy(out=chist[:, cj, :], in_=cstgb[:, TT:TT + 3]), r=[R["cstgb"]], w=[R["chist"]])
                      wo = lp + c.o_mcw
                      bo = lp + c.o_mcb + cj
                      b2 = nb()
                      for tap in range(4):
                          k = dg_ctr["M"] % 2
                          dg_ctr["M"] += 1
                          wc = wo + tap * 2 * HM + cj
                          V(lambda e, k=k, wc=wc: e.tensor_scalar(out=dgM[k][:], in0=identb[:], scalar1=pp[:, wc:wc + 1], scalar2=None, op0=ALU.mult),
                            r=[R["const"], R["pp"]], w=[R_dgM[k]])
                          T(lambda e, k=k, tap=tap: e.matmul(ps[b2][:, :], lhsT=dgM[k][:], rhs=cstgb[:, tap:tap + TT], start=(tap == 0), stop=(tap == 3)),
                            r=[R_dgM[k], R["cstgb"]], w=[R_ps[b2]])
                      A(lambda e: e.activation(out=dest_ap, in_=ps[b2][:, :], func=AF.Silu, bias=pp[:, bo:bo + 1]), r=[R_ps[b2], R["pp"]], w=[R_dest])

                  def do_fm(kind, idx, slot, g, split=1):
                      for cb in range(2):
                          j = idx * 2 + cb
                          b = nb()
                          fm_group(slot, cb, h_t, R_h, b, split)
                          boff = lp + c.o_bfm[kind] + j
                          bias = pp[:, boff:boff + 1]
                          if kind in ("qm", "km"):
                              hl = j - g * HG
                              dest = (qT if kind == "qm" else kT)
                              (conv4_evac if CONV4_PE else conv4_evac_dve)(b, (0 if kind == "qm" else HM) + j, dest[:, hl, :], boff, R["qT"] if kind == "qm" else R["kT"])
                          elif kind == "om":
                              hl = j - g * HG
                              A(lambda e, hl=hl, b=b, bias=bias: e.activation(out=sgo[:, hl, :], in_=ps[b][:, :], func=AF.Sigmoid, bias=bias),
                                r=[R_ps[b], R["pp"]], w=[R["sgo"]])
                          elif kind == "qa":
                              V(lambda e, j=j, b=b, bias=bias: e.tensor_scalar(out=qTa[:, j, :], in0=ps[b][:, :], scalar1=bias, scalar2=128.0 ** -0.5, op0=ALU.add, op1=ALU.mult),
                                r=[R_ps[b], R["pp"]], w=[R["qTa"]])
                          elif kind == "ka":
                              A(lambda e, j=j, b=b, bias=bias: e.activation(out=KT[:, j, tok0:tok0 + TT], in_=ps[b][:, :], func=AF.Identity, bias=bias),
                                r=[R_ps[b], R["pp"]], w=[R["KT"]])
                          elif kind == "gc":
                              A(lambda e, cb=cb, b=b, bias=bias: e.activation(out=sg[:, cb, :], in_=ps[b][:, :], func=AF.Sigmoid, bias=bias),
                                r=[R_ps[b], R["pp"]], w=[R["sg"]])
                          elif kind == "uc":
                              V(lambda e, j=j, cb=cb, b=b, bias=bias: e.scalar_tensor_tensor(out=y0[:, j, 30:30 + TT], in0=ps[b][:, :], scalar=bias, in1=sg[:, cb, :],
                                                                                           op0=ALU.add, op1=ALU.mult), r=[R_ps[b], R["pp"], R["sg"]], w=[R["y0"]])

                  def nd_slot(hl):
                      return hl // 3, (hl % 3) * 129

                  def mlstm_group(g):
                      for bi in range(NB):
                          c0 = bi * 128
                          hs = g * HG
                          bS = nb()

                          def fS(e, bS=bS, c0=c0):
                              for hl in range(HG):
                                  ins = e.matmul(ps[bS][:, hl * 128:(hl + 1) * 128], lhsT=kT[:, hl, c0:c0 + 128], rhs=qT[:, hl, c0:c0 + 128], start=True, stop=True)
                              return ins
                          T(fS, r=[R["qT"], R["kT"]], w=[R_ps[bS]])
                          V(lambda e, bS=bS: e.tensor_tensor(out=sm["STm"].rearrange("p (h l) -> p h l", h=HG), in0=ps[bS][:, 0:HG * 128].rearrange("p (h l) -> p h l", h=HG),
                                                           in1=Utri[:].unsqueeze(1).broadcast_to([128, HG, 128]), op=ALU.mult), r=[R_ps[bS], R["const"]], w=[R["STm"]])
                          bK = nb()

                          def fK(e, bK=bK, c0=c0):
                              for hl in range(HG):
                                  ins = e.transpose(out=psb[bK][:, hl * 128:(hl + 1) * 128], in_=kT[:, hl, c0:c0 + 128], identity=identb[:])
                              return ins
                          T(fK, r=[R["kT"], R["const"]], w=[R_ps[bK]])
                          A(lambda e, bK=bK: e.activation(out=sm["ktok"], in_=psb[bK][:, 0:HG * 128], func=AF.Copy), r=[R_ps[bK]], w=[R["ktok"]])
                          nbk = (HG + 2) // 3
                          bN = [nb() for _ in range(nbk)]

                          def fN(e, bN=bN, c0=c0, bi=bi):
                              for hl in range(HG):
                                  bk, off = nd_slot(hl)
                                  o = ps[bN[bk]][:, off:off + 129]
                                  e.matmul(o, lhsT=qT[:, hl, c0:c0 + 128], rhs=Cb[:, hs + hl, :], start=True, stop=False)
                                  ins = e.matmul(o, lhsT=sm["STm"][:, hl * 128:(hl + 1) * 128], rhs=vx[:, bi, hs + hl, :], start=False, stop=True)
                              return ins
                          T(fN, r=[R["qT"], R["Cb"], R["STm"], R["vx"]], w=[R_ps[x] for x in bN])
                          for bk in range(nbk):
                              h0 = bk * 3
                              n = min(3, HG - h0)
                              pv = ps[bN[bk]][:, 0:n * 129].rearrange("p (h e) -> p h e", h=n)
                              V(lambda e, pv=pv, h0=h0, n=n: e.tensor_reduce(out=dn[:, h0:h0 + n], in_=pv[:, :, 128:129], axis=AX.X, op=ALU.max, apply_absolute_value=True),
                                r=[R_ps[bN[bk]]], w=[R["dn"]])
                          V(lambda e, bi=bi: e.tensor_tensor(out=dn[:], in0=dn[:], in1=einv[:, bi, hs:hs + HG], op=ALU.max), r=[R["dn"], R["einv"]], w=[R["dn"]])
                          V(lambda e: e.reciprocal(out=rr[:], in_=dn[:]), r=[R["dn"]], w=[R["rr"]])
                          for bk in range(nbk):
                              h0 = bk * 3
                              n = min(3, HG - h0)
                              pv = ps[bN[bk]][:, 0:n * 129].rearrange("p (h e) -> p h e", h=n)
                              V(lambda e, pv=pv, h0=h0, n=n: e.tensor_tensor(out=dr[:, h0:h0 + n], in0=pv[:, :, 128], in1=rr[:, h0:h0 + n], op=ALU.mult),
                                r=[R_ps[bN[bk]], R["rr"]], w=[R["dr"]])
                              V(lambda e, pv=pv, h0=h0, n=n: e.tensor_tensor(out=sm["hf"][:, h0 * 128:(h0 + n) * 128].rearrange("p (h e) -> p h e", h=n), in0=pv[:, :, 0:128],
                                                                           in1=rr[:, h0:h0 + n].unsqueeze(2).broadcast_to([128, n, 128]), op=ALU.mult),
                                r=[R_ps[bN[bk]], R["rr"]], w=[R["hf"]])
                          V(lambda e: e.tensor_tensor(out=sm["sqf"].rearrange("p (h e) -> p h e", h=HG), in0=fbl[:, NG + hs * 128: NG + (hs + HG) * 128].rearrange("p (h e) -> p h e", h=HG),
                                                      in1=dr[:].unsqueeze(2).broadcast_to([128, HG, 128]), op=ALU.mult), r=[R["fbl"], R["dr"]], w=[R["sqf"]])
                          V(lambda e: e.tensor_tensor(out=sm["hf"], in0=sm["hf"], in1=sm["sqf"], op=ALU.add), r=[R["hf"], R["sqf"]], w=[R["hf"]])
                          V(lambda e: e.tensor_tensor(out=sm["sqf"], in0=sm["hf"], in1=sm["hf"], op=ALU.mult), r=[R["hf"]], w=[R["sqf"]])
                          V(lambda e: e.tensor_reduce(out=ss[:], in_=sm["sqf"].rearrange("p (h e) -> p h e", h=HG), axis=AX.X, op=ALU.add), r=[R["sqf"]], w=[R["ss"]])
                          V(lambda e: e.tensor_scalar(out=ss[:], in0=ss[:], scalar1=1.0 / 128, scalar2=EPS, op0=ALU.mult, op1=ALU.add), r=[R["ss"]], w=[R["ss"]])
                          A(lambda e: e.activation(out=ss[:], in_=ss[:], func=AF.Sqrt), r=[R["ss"]], w=[R["ss"]])
                          V(lambda e: e.reciprocal(out=ss[:], in_=ss[:]), r=[R["ss"]], w=[R["ss"]])
                          V(lambda e: e.tensor_tensor(out=sm["ytok"].rearrange("p (h e) -> p h e", h=HG), in0=sm["hf"].rearrange("p (h e) -> p h e", h=HG),
                                                      in1=ss[:].unsqueeze(2).broadcast_to([128, HG, 128]), op=ALU.mult), r=[R["hf"], R["ss"]], w=[R["ytok"]])
                          bY = nb()

                          def fY(e, bY=bY):
                              for hl in range(HG):
                                  ins = e.transpose(out=psb[bY][:, hl * 128:(hl + 1) * 128], in_=sm["ytok"][:, hl * 128:(hl + 1) * 128], identity=identb[:])
                              return ins
                          T(fY, r=[R["ytok"], R["const"]], w=[R_ps[bY]])
                          go = lp + c.o_ghm + hs
                          V(lambda e, bY=bY, c0=c0: e.tensor_tensor(out=sm["sqf"].rearrange("p (h e) -> p h e", h=HG), in0=psb[bY][:, 0:HG * 128].rearrange("p (h e) -> p h e", h=HG),
                                                                  in1=sgo[:, 0:HG, c0:c0 + 128], op=ALU.mult), r=[R_ps[bY], R["sgo"]], w=[R["sqf"]])
                          V(lambda e, go=go, c0=c0: e.tensor_tensor(out=m_t[:, hs:hs + HG, c0:c0 + 128], in0=sm["sqf"].rearrange("p (h e) -> p h e", h=HG),
                                                                  in1=pp[:, go:go + HG].unsqueeze(2).broadcast_to([128, HG, 128]), op=ALU.mult),
                            r=[R["sqf"], R["pp"]], w=R_m[hs:hs + HG])
                          bD = [nb() for _ in range(nbk)]

                          def fD(e, bD=bD, bi=bi):
                              for hl in range(HG):
                                  bk, off = nd_slot(hl)
                                  ins = e.matmul(ps[bD[bk]][:, off:off + 129], lhsT=sm["ktok"][:, hl * 128:(hl + 1) * 128], rhs=vx[:, bi, hs + hl, :], start=True, stop=True)
                              return ins
                          T(fD, r=[R["ktok"], R["vx"]], w=[R_ps[x] for x in bD])
                          for bk in range(nbk):
                              h0 = bk * 3
                              n = min(3, HG - h0)
                              pv = ps[bD[bk]][:, 0:n * 129].rearrange("p (h e) -> p h e", h=n)
                              V(lambda e, pv=pv, h0=h0, n=n: e.tensor_tensor(out=Cf[:, hs + h0:hs + h0 + n, :], in0=pv, in1=Cf[:, hs + h0:hs + h0 + n, :], op=ALU.add),
                                r=[R_ps[bD[bk]], R["Cf"]], w=[R["Cf"]])
                          V(lambda e, bi=bi: e.tensor_tensor(out=Cf[:, hs:hs + HG, :], in0=Cf[:, hs:hs + HG, :],
                                                           in1=ebl[:, bi, hs:hs + HG].unsqueeze(2).broadcast_to([128, HG, 129]), op=ALU.mult), r=[R["Cf"], R["ebl"]], w=[R["Cf"]])
                          A(lambda e: e.activation(out=Cb[:, hs:hs + HG, :], in_=Cf[:, hs:hs + HG, :], func=AF.Copy), r=[R["Cf"]], w=[R["Cb"]])

                  def fox_stream(st_):
                      SB = POOLS["F%d" % st_][0:2]
                      FO = POOLS["F%d" % st_][2]
                      halves = [(sqb[st_], R_sqb[st_]), (PTc[:, st_ * 512:(st_ + 1) * 512], R["PTc%d" % st_])]
                      rr1_, ss1_, of_, ya_ = rr1s[st_], ss1s[st_], ofs[st_], yas[st_]
                      R_rr1, R_ss1, R_of, R_ya = R["rr1_%d" % st_], R["ss1_%d" % st_], R_ofs[st_], R["ya_%d" % st_]
                      it = -1
                      for bi in range(NB):
                          gi = t * NB + bi
                          nk = gi + 1
                          ng = (nk + 3) // 4
                          for ha in range(HA):
                              it += 1
                              if it % 2 != st_:
                                  continue

                              def slot(kb):
                                  hv = halves[(kb // 4) % 2][0]
                                  return hv[:, (kb % 4) * 128:(kb % 4 + 1) * 128]

                              def scores(g, bi=bi, ha=ha, gi=gi, nk=nk):
                                  bank = SB[g % 2]

                                  def f(e):
                                      for kb in range(g * 4, min(nk, g * 4 + 4)):
                                          o = ps[bank][:, (kb % 4) * 128:(kb % 4 + 1) * 128]
                                          e.matmul(o, lhsT=KT[:, ha, kb * 128:(kb + 1) * 128], rhs=qTa[:, ha, bi * 128:(bi + 1) * 128], start=True, stop=False)
                                          last = kb == gi
                                          ins = e.matmul(o, lhsT=sel2[ha][:], rhs=negF2[:, gi * 128:(gi + 1) * 128], start=False, stop=not last)
                                          if last:
                                              ins = e.matmul(o, lhsT=identb[:], rhs=maskT[:], start=False, stop=True)
                                      return ins
                                  T(f, r=[R["qTa"], R["KT"], R["negF2"], R["const"]], w=[R_ps[bank]])

                              def exps(g, ha=ha, nk=nk):
                                  bank = SB[g % 2]
                                  hres = halves[g % 2][1]
                                  for kb in range(g * 4, min(nk, g * 4 + 4)):
                                      A(lambda e, kb=kb: e.activation(out=slot(kb), in_=ps[bank][:, (kb % 4) * 128:(kb % 4 + 1) * 128], func=AF.Exp, bias=nFtok[:, kb, ha:ha + 1]),
                                        r=[R_ps[bank], R["nFt"]], w=[hres])

                              def pv(g, ha=ha, nk=nk):
                                  hres = halves[g % 2][1]

                                  def f(e):
                                      for kb in range(g * 4, min(nk, g * 4 + 4)):
                                          ins = e.matmul(ps[FO][:, 0:129], lhsT=slot(kb), rhs=Vext[:, kb, ha, :], start=(kb == 0), stop=(kb == nk - 1))
                                      return ins
                                  T(f, r=[hres, R["Vext"]], w=[R_ps[FO]])

                              scores(0)
                              exps(0)
                              for g in range(1, ng):
                                  scores(g)
                                  exps(g)
                                  pv(g - 1)
                              pv(ng - 1)
                              V(lambda e: e.reciprocal(out=rr1_[:], in_=ps[FO][:, 128:129]), r=[R_ps[FO]], w=[R_rr1])
                              bvo = NG + DM + ha * 128
                              V(lambda e, bvo=bvo: e.scalar_tensor_tensor(out=of_, in0=ps[FO][:, 0:128], scalar=rr1_[:, 0:1], in1=fbl[:, bvo:bvo + 128], op0=ALU.mult, op1=ALU.add),
                                r=[R_ps[FO], R_rr1, R["fbl"]], w=[R_of])
                              V(lambda e: e.tensor_tensor(out=ya_[:], in0=of_, in1=of_, op=ALU.mult), r=[R_of], w=[R_ya])
                              V(lambda e: e.tensor_reduce(out=ss1_[:], in_=ya_[:], axis=AX.X, op=ALU.add), r=[R_ya], w=[R_ss1])
                              V(lambda e: e.tensor_scalar(out=ss1_[:], in0=ss1_[:], scalar1=1.0 / 128, scalar2=EPS, op0=ALU.mult, op1=ALU.add), r=[R_ss1], w=[R_ss1])
                              A(lambda e: e.activation(out=ss1_[:], in_=ss1_[:], func=AF.Sqrt), r=[R_ss1], w=[R_ss1])
                              V(lambda e: e.reciprocal(out=ss1_[:], in_=ss1_[:]), r=[R_ss1], w=[R_ss1])
                              V(lambda e: e.tensor_scalar(out=ya_[:], in0=of_, scalar1=ss1_[:, 0:1], scalar2=None, op0=ALU.mult), r=[R_of, R_ss1], w=[R_ya])
                              T(lambda e: e.transpose(out=psb[FO][:, 0:128], in_=ya_[:], identity=identb[:]), r=[R_ya, R["const"]], w=[R_ps[FO]])
                              go = lp + c.o_gha + ha
                              A(lambda e, go=go, ha=ha, bi=bi: e.activation(out=m_t[:, HM + ha, bi * 128:(bi + 1) * 128], in_=psb[FO][:, 0:128], func=AF.Identity, scale=pp[:, go:go + 1]),
                                r=[R_ps[FO], R["pp"]], w=[R_m[HM + ha]])

                  def conv_all():
                      wo = lp + c.o_cdw
                      cmean = rs[:]
                      cm2 = sg[:, 0, :]
                      csq = sg[:, 1, :]
                      for cc in range(CC):
                          bC = nb()
                          for tap in range(31):
                              k = dg_ctr["C"] % 4
                              dg_ctr["C"] += 1
                              wc = wo + tap * CC + cc
                              V(lambda e, k=k, wc=wc: e.tensor_scalar(out=dgC[k][:], in0=identb[:], scalar1=pp[:, wc:wc + 1], scalar2=None, op0=ALU.mult),
                                r=[R["const"], R["pp"]], w=[R_dgC[k]])
                              T(lambda e, k=k, tap=tap, cc=cc, bC=bC: e.matmul(ps[bC][:, :], lhsT=dgC[k][:], rhs=y0[:, cc, tap:tap + TT], start=(tap == 0), stop=(tap == 30)),
                                r=[R_dgC[k], R["y0"]], w=[R_ps[bC]])
                          bo_ = lp + c.o_cdb + cc
                          A(lambda e, cc=cc, bC=bC, bo_=bo_: e.activation(out=cvt[cc][:], in_=ps[bC][:, :], func=AF.Identity, bias=pp[:, bo_:bo_ + 1]),
                            r=[R_ps[bC], R["pp"]], w=[R_cv[cc]])
                          A(lambda e, cc=cc: e.activation(out=y0[:, cc, 0:30], in_=y0[:, cc, TT:TT + 30], func=AF.Copy), r=[R["y0"]], w=[R["y0"]])
                      bM = nb()
                      for cc in range(CC):
                          T(lambda e, cc=cc: e.matmul(ps[bM][:, :], lhsT=ones_f[:], rhs=cvt[cc][:], start=(cc == 0), stop=(cc == CC - 1)), r=[R_cv[cc], R["const"]], w=[R_ps[bM]])
                      V(lambda e: e.tensor_scalar(out=cmean, in0=ps[bM][:, :], scalar1=1.0 / DC, scalar2=None, op0=ALU.mult), r=[R_ps[bM]], w=[R["rs"]])
                      bQ = nb()
                      for cc in range(CC):
                          A(lambda e, cc=cc: e.activation(out=csq, in_=cvt[cc][:], func=AF.Square), r=[R_cv[cc]], w=[R["sg"]])
                          T(lambda e, cc=cc: e.matmul(ps[bQ][:, :], lhsT=ones_f[:], rhs=csq, start=(cc == 0), stop=(cc == CC - 1)), r=[R["sg"], R["const"]], w=[R_ps[bQ]])
                      V(lambda e: e.tensor_tensor(out=cm2, in0=cmean, in1=cmean, op=ALU.mult), r=[R["rs"]], w=[R["sg"]])
                      V(lambda e: e.scalar_tensor_tensor(out=cm2, in0=ps[bQ][:, :], scalar=1.0 / DC, in1=cm2, op0=ALU.mult, op1=ALU.subtract), r=[R_ps[bQ], R["sg"]], w=[R["sg"]])
                      V(lambda e: e.tensor_scalar(out=cm2, in0=cm2, scalar1=EPS, scalar2=None, op0=ALU.add), r=[R["sg"]], w=[R["sg"]])
                      A(lambda e: e.activation(out=cm2, in_=cm2, func=AF.Sqrt), r=[R["sg"]], w=[R["sg"]])
                      V(lambda e: e.reciprocal(out=cm2, in_=cm2), r=[R["sg"]], w=[R["sg"]])
                      for cc in range(CC):
                          V(lambda e, cc=cc: e.tensor_tensor(out=cvt[cc][:], in0=cvt[cc][:], in1=cmean, op=ALU.subtract), r=[R_cv[cc], R["rs"]], w=[R_cv[cc]])
                          V(lambda e, cc=cc: e.tensor_tensor(out=cvt[cc][:], in0=cvt[cc][:], in1=cm2, op=ALU.mult), r=[R_cv[cc], R["sg"]], w=[R_cv[cc]])
                          go = lp + c.o_clg + cc
                          bo = lp + c.o_clb + cc
                          A(lambda e, cc=cc, go=go, bo=bo: e.activation(out=m_t[:, HM + HA + cc, :], in_=cvt[cc][:], func=AF.Silu, scale=pp[:, go:go + 1], bias=pp[:, bo:bo + 1]),
                            r=[R_cv[cc], R["pp"]], w=[R_m[HM + HA + cc]])

                  hb = HG // 2
                  pre = list(enumerate(c.in_blocks[:c.n_pre_blocks]))

                  def pre_chain():
                      set_pool("P")
                      for bidx_, (kind, idx, c0) in pre:
                          if kind == "vm":
                              continue
                          slot = load_w(wsc_in[l][bidx_], R_win[l])
                          if kind == "va":
                              do_tm(kind, idx, slot)
                          else:
                              do_fm(kind, idx, slot, 0, split=4)

                  if INTERLEAVE:
                      p.run_chains([gates_chain, pre_chain], (1, 1))
                  else:
                      gates_chain()
                      pre_chain()
                  set_pool("all")
                  for bidx_, (kind, idx, c0) in pre:
                      if kind == "vm":
                          slot = load_w(wsc_in[l][bidx_], R_win[l])
                          do_tm(kind, idx, slot)
                  ckpt('proj')

                  def m_chain():
                      set_pool("M")
                      bi_ = c.n_pre_blocks
                      for (kind, idx, c0) in c.in_blocks[c.n_pre_blocks:]:
                          slot = load_w(wsc_in[l][bi_], R_win[l])
                          bi_ += 1
                          g = idx // hb
                          do_fm(kind, idx, slot, g, split=4)
                          if kind == "om" and (idx % hb) == hb - 1:
                              mlstm_group(g)

                  def c_chain():
                      set_pool("C")
                      conv_all()

                  if INTERLEAVE:
                      p.run_chains([lambda: fox_stream(0), lambda: fox_stream(1), m_chain, c_chain], CH_W)
                  else:
                      m_chain()
                      c_chain()
                      fox_stream(0)
                      fox_stream(1)
                  set_pool("all")
                  ckpt('conv')
                  set_pool("S")
                  BN = 7
                  for j in range(NBO):
                      slot = load_w(wsc_out[l][j], R_wout[l])
                      for cb in range(2):
                          b = nb()
                          fm_group(slot, cb, m_t, R_m, b)
                          dblk = 2 * j + cb
                          V(lambda e, b=b, dblk=dblk: e.tensor_tensor(out=x_t[:, dblk, :], in0=x_t[:, dblk, :], in1=ps[b][:, :], op=ALU.add), r=[R_ps[b], R_x[dblk]], w=[R_x[dblk]])
                          sq = sqb[dblk % 2]
                          A(lambda e, dblk=dblk, sq=sq: e.activation(out=sq[:], in_=x_t[:, dblk, :], func=AF.Square), r=[R_x[dblk]], w=[R_sqb[dblk % 2]])
                          go_ = lp + c.o_gffn + dblk
                          V(lambda e, dblk=dblk, go_=go_: e.tensor_scalar(out=h_t[:, dblk, :], in0=x_t[:, dblk, :], scalar1=pp[:, go_:go_ + 1], scalar2=None, op0=ALU.mult),
                            r=[R_x[dblk], R["pp"]], w=[R_h[dblk]])
                          if dblk >= 1:
                              d1 = dblk - 1
                              T(lambda e, d1=d1: e.matmul(ps[BN][:, :], lhsT=ones_b[:], rhs=sqb[d1 % 2][:], start=(d1 == 0), stop=False),
                                r=[R_sqb[d1 % 2], R["const"]], w=[R_ps[BN]])
                  T(lambda e: e.matmul(ps[BN][:, :], lhsT=ones_b[:], rhs=sqb[(KC - 1) % 2][:], start=(KC == 1), stop=True),
                    r=[R_sqb[(KC - 1) % 2], R["const"]], w=[R_ps[BN]])
                  set_pool("all")
                  ckpt('stageC')
                  V(lambda e: e.tensor_scalar(out=rs[:], in0=ps[BN][:, :], scalar1=1.0 / D, scalar2=EPS, op0=ALU.mult, op1=ALU.add), r=[R_ps[BN]], w=[R["rs"]])
                  A(lambda e: e.activation(out=rs[:], in_=rs[:], func=AF.Sqrt), r=[R["rs"]], w=[R["rs"]])
                  V(lambda e: e.reciprocal(out=rs[:], in_=rs[:]), r=[R["rs"]], w=[R["rs"]])
                  nid = V(lambda e: e.tensor_tensor(out=rs[:], in0=rs[:], in1=rs[:], op=ALU.mult), r=[R["rs"]], w=[R["rs"]])
                  if s == 0 and l + 1 < NL and t < 4:
                      co = cast_ops(l + 1)
                      sel = co[t:t + 1] if NT >= 4 else (co if t == 0 else [])
                      for (f, n), res in sel:
                          p.dma("gpsimd", f, writes=[res], ndma=n, after=[nid])
                  kk = 0
                  for qd in range(4):
                      for j in range(NBO):
                          slot = load_w(wsc_up[l][qd * NBO + j], R_wup[l])
                          for cb in range(2):
                              fcl = 2 * j + cb
                              b = nb()
                              fm_group(slot, cb, h_t, R_h, b)
                              ri = kk % 2
                              kk += 1
                              A(lambda e, b=b, ri=ri: e.activation(out=rl[ri][:], in_=ps[b][:, :], func=AF.Relu), r=[R_ps[b]], w=[R_rl[ri]])
                              V(lambda e, ri=ri: e.tensor_tensor(out=rl[ri][:], in0=rl[ri][:], in1=rl[ri][:], op=ALU.mult), r=[R_rl[ri]], w=[R_rl[ri]])
                              V(lambda e, ri=ri, fcl=fcl: e.tensor_tensor(out=m_t[:, fcl, :], in0=rl[ri][:], in1=rs[:], op=ALU.mult), r=[R_rl[ri], R["rs"]], w=[R_m[fcl]])
                      for j in range(NBO):
                          slot = load_w(wsc_dn[l][qd * NBO + j], R_wdn[l])
                          for cb in range(2):
                              dblk = 2 * j + cb
                              b = nb()
                              fm_group(slot, cb, m_t, R_m, b)
                              V(lambda e, b=b, dblk=dblk: e.tensor_tensor(out=x_t[:, dblk, :], in0=x_t[:, dblk, :], in1=ps[b][:, :], op=ALU.add), r=[R_ps[b], R_x[dblk]], w=[R_x[dblk]])

                  ckpt('ffn')
                  if l < NL - 1:
                      for kc in range(KC):
                          p.dma(XQ, lambda e, sm_, xi=xi, kc=kc: e.dma_start(out=xs_d[xi][:, kc, :], in_=x_t[:, kc, :]).then_inc(sm_, 16), reads=[R_x[kc]], writes=[R_xs[xi][kc]])
                  else:
                      rmsnorm_h(l, c.o_gfin, inplace_f32=True)
                      k = 0
                      for bi in range(NB):
                          for dq in range(D // 512):
                              sa, sr = stgs[k % NSTG]
                              k += 1
                              b = nb()

                              def fto(e, b=b, dq=dq, bi=bi):
                                  for i4 in range(4):
                                      ins = e.transpose(out=ps[b][:, i4 * 128:(i4 + 1) * 128], in_=x_t[:, dq * 4 + i4, bi * 128:(bi + 1) * 128], identity=identf[:])
                                  return ins
                              T(fto, r=R_x[dq * 4:(dq + 1) * 4] + [R["const"]], w=[R_ps[b]])
                              if k % 2 == 0:
                                  V(lambda e, b=b, sa=sa: e.tensor_copy(out=sa, in_=ps[b][:, :]), r=[R_ps[b]], w=[sr])
                              else:
                                  A(lambda e, b=b, sa=sa: e.activation(out=sa, in_=ps[b][:, :], func=AF.Copy), r=[R_ps[b]], w=[sr])
                              dst = y_d[s, tok0 + bi * 128: tok0 + (bi + 1) * 128, dq * 512:(dq + 1) * 512]
                              p.dma("sync", lambda e, sm_, sa=sa, dst=dst: e.dma_start(out=dst, in_=sa).then_inc(sm_, 16), reads=[sr], is_output=True)

    except _Stop:
        pass
    p.emit()
    st.close()
    return nc, p, dbg_out


_CACHE = {}


def kernel(**inputs):
    cfg = Cfg()
    ncores = 8
    pp, fb = pack_params(cfg, inputs)
    nc, prog, _ = build(cfg)
    x = np.ascontiguousarray(inputs["x"], dtype=np.float32)
    in_maps = []
    for ci in range(ncores):
        in_maps.append({
            "x": np.ascontiguousarray(x[ci * cfg.NSEQ:(ci + 1) * cfg.NSEQ]),
            "w_in": inputs["w_in"], "w_out": inputs["w_out"], "w_up": inputs["w_up"], "w_down": inputs["w_down"],
            "pp": pp, "fb": fb,
        })
    res = run_bass_kernel_spmd(nc, in_maps, core_ids=list(range(ncores)))
    out = np.concatenate([np.asarray(r["y"]) for r in res.results], axis=0)
    return out.astype(np.float32, copy=False)
```
